# Optimizing a Trainium2 kernel written in Bass

```python
import math
import jax, jax.numpy as jnp
from jax import lax
import numpy as np


D_MODEL = 1024
BATCH = 8
SEQ = 4096
DEPTH = 1

A_HEADS = 8
A_HEAD_DIM = 64
A_WIDTH = A_HEADS * A_HEAD_DIM
IDX_HEADS = 16
IDX_DIM = 32
TOPK_MAX = 256
QBLK = 128
REL_BUCKETS = 32
REL_MAX_DIST = 128
G_HEADS = 4
G_DK = 64
G_DV = 128
G_KW = G_HEADS * G_DK
G_VW = G_HEADS * G_DV
G_RANK = 16
G_TAU = 16.0
G_CHUNK = 64
N_GROUPS = 4
EXPERTS_PER_GROUP = 8
N_EXPERTS = N_GROUPS * EXPERTS_PER_GROUP
EXPERT_TOP_K = 2
D_EXPERT = 256
DN_ALPHA = (2.0 * DEPTH) ** 0.25
DN_BETA = (8.0 * DEPTH) ** -0.25
LN_EPS = 1e-5

SPLIT_SIZES = (A_WIDTH, A_WIDTH, A_WIDTH, IDX_HEADS * IDX_DIM, IDX_DIM, IDX_HEADS,
               G_KW, G_KW, G_VW, G_VW, G_RANK, D_MODEL, D_MODEL)
VALUE_SEGMENTS = (2, 8)
SPLIT_OFFSETS = tuple(int(v) for v in np.cumsum(SPLIT_SIZES)[:-1])
D_IN_PROJ = int(sum(SPLIT_SIZES))

kernel_name = "hybrid_dsa_gla_hmoe_deepnorm_adaln"


def layer_norm(x, g=None, b=None):
    xf = x.astype(jnp.float32)
    mu = jnp.mean(xf, axis=-1, keepdims=True)
    var = jnp.mean(jnp.square(xf - mu), axis=-1, keepdims=True)
    y = (xf - mu) * lax.rsqrt(var + LN_EPS)
    if g is not None:
        y = y * g.astype(jnp.float32) + b.astype(jnp.float32)
    return y.astype(x.dtype)


def modulate(x, shift, scale):
    return layer_norm(x) * (1.0 + scale[:, None, :]) + shift[:, None, :]


def t5_bucket(dist):
    max_exact = REL_BUCKETS // 2
    d_f = jnp.maximum(dist, 1).astype(jnp.float32)
    large = max_exact + (jnp.log(d_f / max_exact) / math.log(REL_MAX_DIST / max_exact)
                         * (REL_BUCKETS - max_exact)).astype(jnp.int32)
    large = jnp.minimum(large, REL_BUCKETS - 1)
    return jnp.where(dist < max_exact, dist, large)


def dsa_sparse_attention(q, k, v, iq, ik, iw, rel_bias):
    B, S, H, Dh = q.shape
    topk = min(TOPK_MAX, S // 4)
    n_blk = S // QBLK
    scale = Dh ** -0.5
    iw = iw * (IDX_HEADS ** -0.5 * IDX_DIM ** -0.5)
    key_pos = jnp.arange(S, dtype=jnp.int32)
    gather = jax.vmap(lambda arr, idx: arr[idx])

    def block(i):
        q0 = i * QBLK
        qb = lax.dynamic_slice_in_dim(q, q0, QBLK, axis=1)
        iqb = lax.dynamic_slice_in_dim(iq, q0, QBLK, axis=1)
        iwb = lax.dynamic_slice_in_dim(iw, q0, QBLK, axis=1)
        q_pos = q0 + jnp.arange(QBLK, dtype=jnp.int32)
        idx_logits = jnp.einsum('bqhd,bsd->bqhs', iqb, ik)
        score = jnp.einsum('bqh,bqhs->bqs', iwb, jax.nn.relu(idx_logits)).astype(jnp.float32)
        causal = key_pos[None, :] <= q_pos[:, None]
        score = jnp.where(causal[None], score, -jnp.inf)
        _, sel = lax.top_k(score, topk)
        valid = sel <= q_pos[None, :, None]
        k_sel = gather(k, sel)
        v_sel = gather(v, sel)
        logits = jnp.einsum('bqhd,bqkhd->bhqk', qb, k_sel).astype(jnp.float32) * scale
        bucket = t5_bucket(jnp.maximum(q_pos[None, :, None] - sel, 0))
        bias = rel_bias[bucket]
        logits = logits + jnp.transpose(bias, (0, 3, 1, 2)).astype(jnp.float32)
        logits = jnp.where(valid[:, None], logits, -jnp.inf)
        p = jax.nn.softmax(logits, axis=-1).astype(v.dtype)
        return jnp.einsum('bhqk,bqkhd->bqhd', p, v_sel)

    out = lax.map(block, jnp.arange(n_blk, dtype=jnp.int32))
    return jnp.transpose(out, (1, 0, 2, 3, 4)).reshape(B, S, H, Dh)


def gla_chunked(q, k, v, log_g):
    B, S, H, Dk = q.shape
    Dv = v.shape[-1]
    C = G_CHUNK
    N = S // C

    def chunks(t):
        return jnp.transpose(t.astype(jnp.float32).reshape(B, N, C, H, -1), (0, 3, 1, 2, 4))

    qc = chunks(q) * (Dk ** -0.5)
    kc = chunks(k)
    vc = chunks(v)
    b = jnp.cumsum(chunks(log_g), axis=3)
    b_last = b[:, :, :, -1:, :]
    q_in = qc * jnp.exp(b)
    k_st = kc * jnp.exp(b_last - b)
    q_rel = qc * jnp.exp(b - b_last)
    tril = jnp.tril(jnp.ones((C, C), dtype=bool))
    att = jnp.einsum('bhncd,bhnjd->bhncj', q_rel, k_st)
    att = jnp.where(tril, att, 0.0)
    o_intra = jnp.einsum('bhncj,bhnje->bhnce', att, vc)
    u = jnp.einsum('bhncd,bhnce->bhnde', k_st, vc)
    decay = jnp.exp(b_last[:, :, :, 0, :])

    def step(state, inp):
        dec, uu = inp
        return dec[..., None] * state + uu, state

    _, s_prev = lax.scan(step, jnp.zeros((B, H, Dk, Dv), jnp.float32),
                         (jnp.moveaxis(decay, 2, 0), jnp.moveaxis(u, 2, 0)))
    s_prev = jnp.moveaxis(s_prev, 0, 2)
    o_inter = jnp.einsum('bhncd,bhnde->bhnce', q_in, s_prev)
    o = o_intra + o_inter
    return jnp.transpose(o, (0, 2, 3, 1, 4)).reshape(B, S, H, Dv)


def hier_moe(h, w_rg, b_rg, w_re, b_re, w1, w3, w2):
    B, S, D = h.shape
    t = h.reshape(B * S, D)
    g_prob = jax.nn.softmax((t @ w_rg + b_rg).astype(jnp.float32), axis=-1)
    g_w, g_idx = lax.top_k(g_prob, 1)
    e_logits = (t @ w_re + b_re).astype(jnp.float32).reshape(B * S, N_GROUPS, EXPERTS_PER_GROUP)
    g_onehot = jax.nn.one_hot(g_idx[:, 0], N_GROUPS, dtype=jnp.float32)
    e_in_group = jnp.einsum('tg,tge->te', g_onehot, e_logits)
    e_top, e_idx = lax.top_k(e_in_group, EXPERT_TOP_K)
    e_w = jax.nn.softmax(e_top, axis=-1) * g_w
    expert_id = g_idx * EXPERTS_PER_GROUP + e_idx
    gate = jnp.sum(jax.nn.one_hot(expert_id, N_EXPERTS, dtype=jnp.float32) * e_w[..., None], axis=1)
    gate = gate.astype(h.dtype)
    y = jnp.zeros_like(t)
    for e in range(N_EXPERTS):
        hid = jax.nn.silu(t @ w1[e]) * (t @ w3[e])
        y = y + gate[:, e:e + 1] * (hid @ w2[e])
    return y.reshape(B, S, D)


def setup_inputs(seed: int = 0) -> dict:
    key = jax.random.key(seed)
    ks = jax.random.split(key, 24)
    f32 = jnp.float32
    D = D_MODEL
    nrm = lambda k, shape, s: jax.random.normal(k, shape, f32) * s
    col_scale = jnp.concatenate([jnp.full((n,), DN_BETA if i in VALUE_SEGMENTS else 1.0, f32)
                                 for i, n in enumerate(SPLIT_SIZES)])
    return {
        'x': nrm(ks[0], (BATCH, SEQ, D), 1.0),
        'c': nrm(ks[1], (BATCH, D), 1.0),
        'rel_bias': nrm(ks[2], (REL_BUCKETS, A_HEADS), 0.5),
        'w_ada': nrm(ks[3], (DEPTH, D, 6 * D), 0.5 * D ** -0.5),
        'b_ada': nrm(ks[4], (DEPTH, 6 * D), 0.01),
        'w_in': nrm(ks[5], (DEPTH, D, D_IN_PROJ), D ** -0.5) * col_scale,
        'gla_w_gate': nrm(ks[6], (DEPTH, G_RANK, G_KW), G_RANK ** -0.5),
        'gla_b_gate': nrm(ks[7], (DEPTH, G_KW), 0.1),
        'gla_norm_g': 1.0 + nrm(ks[8], (DEPTH, G_VW), 0.02),
        'w_branch_a': nrm(ks[9], (DEPTH, A_WIDTH, D), A_WIDTH ** -0.5 * DN_BETA),
        'w_branch_b': nrm(ks[10], (DEPTH, G_VW, D), G_VW ** -0.5 * DN_BETA),
        'w_out': nrm(ks[11], (DEPTH, D, D), D ** -0.5 * DN_BETA),
        'ln1_g': 1.0 + nrm(ks[12], (DEPTH, D), 0.02),
        'ln1_b': nrm(ks[13], (DEPTH, D), 0.02),
        'w_router_group': nrm(ks[14], (DEPTH, D, N_GROUPS), D ** -0.5),
        'b_router_group': nrm(ks[15], (DEPTH, N_GROUPS), 0.01),
        'w_router_expert': nrm(ks[16], (DEPTH, D, N_EXPERTS), D ** -0.5),
        'b_router_expert': nrm(ks[17], (DEPTH, N_EXPERTS), 0.01),
        'w_exp_gate': nrm(ks[18], (DEPTH, N_EXPERTS, D, D_EXPERT), D ** -0.5),
        'w_exp_up': nrm(ks[19], (DEPTH, N_EXPERTS, D, D_EXPERT), D ** -0.5),
        'w_exp_down': nrm(ks[20], (DEPTH, N_EXPERTS, D_EXPERT, D), D_EXPERT ** -0.5 * DN_BETA),
        'ln2_g': 1.0 + nrm(ks[21], (DEPTH, D), 0.02),
        'ln2_b': nrm(ks[22], (DEPTH, D), 0.02),
    }


def reference(x, c, rel_bias, w_ada, b_ada, w_in, gla_w_gate, gla_b_gate, gla_norm_g,
              w_branch_a, w_branch_b, w_out, ln1_g, ln1_b, w_router_group, b_router_group,
              w_router_expert, b_router_expert, w_exp_gate, w_exp_up, w_exp_down, ln2_g, ln2_b):
    B, S, D = x.shape
    cond = jax.nn.silu(c)
    for l in range(DEPTH):
        ada = cond @ w_ada[l] + b_ada[l]
        sh1, sc1, gt1, sh2, sc2, gt2 = jnp.split(ada, 6, axis=-1)
        h = modulate(x, sh1, sc1)
        proj = h @ w_in[l]
        (aq, ak, av, iq, ik, iw, gq, gk, gv, gr, glr, gate_a, gate_b) = jnp.split(proj, SPLIT_OFFSETS, axis=-1)
        o_a = dsa_sparse_attention(aq.reshape(B, S, A_HEADS, A_HEAD_DIM),
                                   ak.reshape(B, S, A_HEADS, A_HEAD_DIM),
                                   av.reshape(B, S, A_HEADS, A_HEAD_DIM),
                                   iq.reshape(B, S, IDX_HEADS, IDX_DIM), ik, iw,
                                   rel_bias).reshape(B, S, A_WIDTH)
        log_g = jax.nn.log_sigmoid((glr @ gla_w_gate[l] + gla_b_gate[l]).astype(jnp.float32)) / G_TAU
        o_b = gla_chunked(gq.reshape(B, S, G_HEADS, G_DK), gk.reshape(B, S, G_HEADS, G_DK),
                          gv.reshape(B, S, G_HEADS, G_DV), log_g.reshape(B, S, G_HEADS, G_DK))
        o_b = (layer_norm(o_b) * gla_norm_g[l].reshape(G_HEADS, G_DV).astype(jnp.float32)).astype(x.dtype)
        o_b = (o_b * jax.nn.silu(gr.reshape(B, S, G_HEADS, G_DV))).reshape(B, S, G_VW)
        merged = (jax.nn.sigmoid(gate_a) * (o_a @ w_branch_a[l])
                  + jax.nn.sigmoid(gate_b) * (o_b @ w_branch_b[l]))
        y = merged @ w_out[l]
        x = layer_norm(DN_ALPHA * x + gt1[:, None, :] * y, ln1_g[l], ln1_b[l])
        h2 = modulate(x, sh2, sc2)
        f = hier_moe(h2, w_router_group[l], b_router_group[l], w_router_expert[l], b_router_expert[l],
                     w_exp_gate[l], w_exp_up[l], w_exp_down[l])
        x = layer_norm(DN_ALPHA * x + gt2[:, None, :] * f, ln2_g[l], ln2_b[l])
    return x
```

```python
import numpy as np
from contextlib import ExitStack
import concourse.bass as bass
import concourse.mybir as mybir
from concourse.bass_utils import run_bass_kernel_spmd

F32 = mybir.dt.float32
BF16 = mybir.dt.bfloat16
AF = mybir.ActivationFunctionType
ALU = mybir.AluOpType
AX = mybir.AxisListType

S = 4096
D = 1024
NT = S // 128
DP = 5696
O_AQ, O_AK, O_AV, O_IQ, O_IK, O_IW = 0, 512, 1024, 1536, 2048, 2080
O_GQ, O_GK, O_GV, O_GR, O_GLR, O_GA, O_GB = 2096, 2352, 2608, 3120, 3632, 3648, 4672
ALPHA = 2.0 ** 0.25
EPS = 1e-5
NEG = -1.0e30
TOKW = 4368
NFT = 14
NIT = 16
NT3 = NT
STOP = 0


class Sem:
    def __init__(self, h, is_dma=True):
        self.h = h
        self.val = 0
        self.is_dma = is_dma


class Buf:
    __slots__ = ("name", "last_w", "reads")

    def __init__(self, name):
        self.name = name
        self.last_w = None
        self.reads = {}


class Eng:
    def __init__(self, name, obj, sem):
        self.name = name
        self.obj = obj
        self.sem = sem
        self.seen = {}


class K:
    def __init__(self, nc, es):
        self.nc = nc
        self.es = es
        self.nsem = 0
        self.sems = []
        self.engs = {}
        for name, obj in (("pe", nc.tensor), ("act", nc.scalar), ("dve", nc.vector),
                          ("pool", nc.gpsimd), ("sp", nc.sync)):
            self.engs[name] = Eng(name, obj, self.new_sem("e_" + name))
            self.engs[name].sem.is_dma = False

    def new_sem(self, name):
        h = self.es.enter_context(self.nc.semaphore("s%d_%s" % (self.nsem, name)))
        self.nsem += 1
        s = Sem(h)
        self.sems.append(s)
        return s

    def _needs(self, E, r, w, skip_self=False):
        needs = {}

        def need(dep):
            if dep is None:
                return
            s, v = dep
            if skip_self and s is E.sem:
                return
            if s.is_dma:
                v = s.val
            if E.seen.get(s, 0) >= v:
                return
            if needs.get(s, 0) < v:
                needs[s] = v

        for b in r:
            need(b.last_w)
        for b in w:
            need(b.last_w)
            for s, v in b.reads.items():
                need((s, v))
        return needs

    def op(self, eng, fn, r=(), w=(), inc=True):
        E = self.engs[eng]
        needs = self._needs(E, r, w, skip_self=(eng == "pe"))
        items = list(needs.items())
        for s, v in items[:-1]:
            E.obj.wait_ge(s.h, v)
        ins = fn(E.obj)
        if items:
            s, v = items[-1]
            ins._wait_ge(s.h, v)
        for s, v in items:
            E.seen[s] = v
        if inc:
            E.sem.val += 1
            ins.then_inc(E.sem.h, 1)
            stamp = E.sem.val
        else:
            stamp = E.sem.val + 1
        for b in r:
            if b.reads.get(E.sem, 0) < stamp:
                b.reads[E.sem] = stamp
        for b in w:
            b.last_w = (E.sem, stamp)
            b.reads = {}
        return ins

    def dma(self, q, out, in_, sem, r=(), w=()):
        E = self.engs[q]
        needs = self._needs(E, r, w)
        for s, v in needs.items():
            E.obj.wait_ge(s.h, v)
            E.seen[s] = v
        ins = E.obj.dma_start(out=out, in_=in_)
        sem.val += 16
        ins.then_inc(sem.h, 16)
        for b in r:
            if b.reads.get(sem, 0) < sem.val:
                b.reads[sem] = sem.val
        for b in w:
            b.last_w = (sem, sem.val)
            b.reads = {}
        return ins

    def barrier(self):
        for E in self.engs.values():
            for s in self.sems:
                if s is E.sem:
                    continue
                if s.val > E.seen.get(s, 0):
                    E.obj.wait_ge(s.h, s.val)
                    E.seen[s] = s.val

    def final_wait(self, q, sems):
        E = self.engs[q]
        for s in sems:
            E.obj.wait_ge(s.h, s.val)


def build_nc(debug=None):
    nc = bass.Bass("TRN2", target_bir_lowering=False)
    dbg = {}

    def din(name, shape, dt=F32):
        return nc.dram_tensor(name, list(shape), dt, kind="ExternalInput").ap()

    x_d = din("x", [S, D])
    c_d = din("c_pl", [128, 8])
    w_ada_d = din("w_ada", [D, 6 * D])
    b_ada_pl_d = din("b_ada_pl", [128, 48])
    b_ada_row_d = din("b_ada_row", [1, 6 * D])
    w_in_d = din("w_in", [D, DP])
    rel_bias_d = din("rel_bias", [32, 8])
    ohpad_d = din("ohpad", [32, 384])
    gla_wg_d = din("gla_wg", [16, 256])
    gla_bg_d = din("gla_bg", [1, 256])
    gla_g_d = din("gla_g", [1, 512])
    out_d = nc.dram_tensor("out", [S, D], F32, kind="ExternalOutput").ap()
    w_ba_d = din("w_ba", [512, D])
    w_bb_d = din("w_bb", [512, D])
    w_o_d = din("w_o", [D, D])
    ln1_g_d = din("ln1_g", [1, D]); ln1_b_d = din("ln1_b", [1, D])
    ln2_g_d = din("ln2_g", [1, D]); ln2_b_d = din("ln2_b", [1, D])
    w_r_d = din("w_r", [D, 36]); b_r_d = din("b_r", [1, 36])
    w_eg_d = din("w_eg", [32, D, 256]); w_eu_d = din("w_eu", [32, D, 256]); w_ed_d = din("w_ed", [32, 256, D])
    x1_d = nc.dram_tensor("x1s", [S, D], F32, kind="Internal").ap()
    h2T_d = nc.dram_tensor("h2Ts", [8, 128, S], BF16, kind="Internal").ap()
    a2_d = nc.dram_tensor("a2", [8, 128, 384], F32, kind="Internal").ap()
    oaT_d = nc.dram_tensor("oaT", [4, 128, S], BF16, kind="Internal").ap()
    obT_d = nc.dram_tensor("obT", [4, 128, S], BF16, kind="Internal").ap()

    featT_d = nc.dram_tensor("featT", [NFT, 128, S], BF16, kind="Internal").ap()
    tok_d = nc.dram_tensor("tokm", [S, TOKW], BF16, kind="Internal").ap()

    if debug:
        dbg["hT"] = nc.dram_tensor("dbg_hT", [128, 8, S], BF16, kind="ExternalOutput").ap()
        dbg["featT"] = nc.dram_tensor("dbg_featT", [NFT, 128, S], BF16, kind="ExternalOutput").ap()
        dbg["tok"] = nc.dram_tensor("dbg_tok", [S, TOKW], BF16, kind="ExternalOutput").ap()
        dbg["ada"] = nc.dram_tensor("dbg_ada", [128, 32], F32, kind="ExternalOutput").ap()
        dbg["gt"] = nc.dram_tensor("dbg_gt", [128, 2048], F32, kind="ExternalOutput").ap()
        dbg["oaT"] = nc.dram_tensor("dbg_oaT", [4, 128, S], BF16, kind="ExternalOutput").ap()
        dbg["obT"] = nc.dram_tensor("dbg_obT", [4, 128, S], BF16, kind="ExternalOutput").ap()

    es = ExitStack()
    with es:
        k = K(nc, es)

        SB_LIMIT = 208 * 1024

        def _chk(name, t):
            m = nc.lookup_mloc(name)
            sz = 1
            for d_ in list(m.dims)[1:]:
                sz *= d_
            assert m.addr + sz <= SB_LIMIT, ("SBUF overflow", name, m.addr, sz)
            return t

        uid = [0]

        def sb(name, shape, dt):
            uid[0] += 1
            name = "%s_u%d" % (name, uid[0])
            return _chk(name, es.enter_context(nc.sbuf_tensor(name, list(shape), dt)))

        cur = [None]

        def lsb(name, shape, dt):
            uid[0] += 1
            name = "%s_u%d" % (name, uid[0])
            return _chk(name, cur[0].enter_context(nc.sbuf_tensor(name, list(shape), dt)))

        def phase_begin():
            cur[0] = ExitStack()

        def phase_end():
            k.barrier()
            cur[0].close()
            cur[0] = None

        def pst(name, shape, dt):
            return es.enter_context(nc.psum_tensor(name, list(shape), dt))

        ident = sb("ident", [128, 128], BF16)
        identf = sb("identf", [128, 128], F32)
        hT = sb("hT", [128, 8, S], BF16)
        adaP = sb("adaP", [128, 32], F32)
        gtB = sb("gtB", [128, 2048], F32)
        B_ident = Buf("ident")
        B_hT = [Buf("hT%d" % i) for i in range(NT)]
        B_adaP = Buf("adaP")
        B_gtB = Buf("gtB")

        ps = [pst("ps%d" % i, [128, 512], F32) for i in range(8)]
        B_ps = [Buf("ps%d" % i) for i in range(8)]

        k.op("pool", lambda e: e.memset(identf[:], 0.0), w=[B_ident])
        k.op("pool", lambda e: e.affine_select(out=identf[:], in_=identf[:], pattern=[[-1, 128]],
                                               compare_op=ALU.not_equal, fill=1.0, base=0,
                                               channel_multiplier=1), r=[B_ident], w=[B_ident])
        k.op("pool", lambda e: e.tensor_copy(out=ident[:], in_=identf[:]), r=[B_ident], w=[B_ident])

        ones1 = sb("ones1", [1, 128], BF16)
        phase_begin()
        c_sb = lsb("c_sb", [128, 8], F32)
        cond = lsb("cond", [128, 8], BF16)
        condB = lsb("condB", [128, 8, 128], BF16)
        bpl = lsb("bpl", [128, 48], F32)
        brow = lsb("brow", [1, 6 * D], F32)
        browb = lsb("browb", [1, 6 * D], BF16)
        wada = [lsb("wada%d" % i, [128, 8, 1024], BF16) for i in range(2)]
        B_c = Buf("c"); B_cond = Buf("cond"); B_bpl = Buf("bpl"); B_brow = Buf("brow")
        B_wada = [Buf("wada0"), Buf("wada1")]
        s_misc = k.new_sem("misc")
        s_wada = [k.new_sem("wada0"), k.new_sem("wada1")]
        k.dma("sp", c_sb[:], c_d[:, :], s_misc, w=[B_c])
        k.dma("sp", bpl[:], b_ada_pl_d[:, :], k.new_sem("misc2"), w=[B_bpl])
        k.dma("sp", brow[:], b_ada_row_d[:, :], k.new_sem("misc3"), w=[B_brow])
        k.op("act", lambda e: e.activation(out=cond[:], in_=c_sb[:], func=AF.Silu), r=[B_c], w=[B_cond])
        k.op("dve", lambda e: e.tensor_copy(out=condB[:], in_=cond[:].unsqueeze(2).broadcast_to([128, 8, 128])),
             r=[B_cond], w=[B_cond])
        k.op("dve", lambda e: e.tensor_copy(out=browb[:], in_=brow[:]), r=[B_brow], w=[B_brow])
        k.op("dve", lambda e: e.memset(ones1[:], 1.0), w=[B_brow])
        w_ada_v = w_ada_d.rearrange("(k p) j -> p k j", p=128)
        for pc in range(6):
            sl = pc % 2
            k.dma("pool", wada[sl][:], w_ada_v[:, :, pc * 1024:(pc + 1) * 1024], s_wada[sl], w=[B_wada[sl]])
            if pc in (2, 5):
                g = 0 if pc == 2 else 1
                for half in range(2):
                    bank = 2 + half
                    for kk in range(8):
                        k.op("pe", lambda e, kk=kk, half=half, bank=bank: e.matmul(
                            ps[bank][:], lhsT=condB[:, kk, :], rhs=wada[sl][:, kk, half * 512:(half + 1) * 512],
                            start=(kk == 0), stop=False), r=[B_cond, B_wada[sl]], w=[B_ps[bank]], inc=False)
                    k.op("pe", lambda e, half=half, bank=bank: e.matmul(
                        ps[bank][:], lhsT=ones1[:, :], rhs=browb[:, pc * 1024 + half * 512: pc * 1024 + (half + 1) * 512],
                        start=False, stop=True), r=[B_brow], w=[B_ps[bank]])
                    k.op("act", lambda e, half=half, bank=bank, g=g: e.activation(
                        out=gtB[:, g * 1024 + half * 512: g * 1024 + (half + 1) * 512], in_=ps[bank][:], func=AF.Identity),
                        r=[B_ps[bank]], w=[B_gtB])
            else:
                slot = {0: 0, 1: 1, 3: 2, 4: 3}[pc]
                for jc in range(8):
                    col = slot * 8 + jc
                    for kk in range(8):
                        k.op("pe", lambda e, kk=kk, jc=jc, col=col: e.matmul(
                            ps[0][:, col:col + 1], lhsT=wada[sl][:, kk, jc * 128:(jc + 1) * 128], rhs=cond[:, kk:kk + 1],
                            start=(kk == 0), stop=(kk == 7)), r=[B_cond, B_wada[sl]], w=[B_ps[0]], inc=(kk == 7))
                add1 = 1.0 if pc in (1, 4) else 0.0
                k.op("dve", lambda e, slot=slot, pc=pc, add1=add1: e.scalar_tensor_tensor(
                    out=adaP[:, slot * 8:(slot + 1) * 8], in0=ps[0][:, slot * 8:(slot + 1) * 8], scalar=add1,
                    in1=bpl[:, pc * 8:(pc + 1) * 8], op0=ALU.add, op1=ALU.add),
                    r=[B_ps[0], B_bpl], w=[B_adaP])

        phase_end()
        LN = {}

        def alloc_ln(n=2, nxn=None):
            nxn = n if nxn is None else nxn
            LN["xn"] = [lsb("xn%d" % i, [128, D], BF16) for i in range(nxn)]
            LN["st6"] = [lsb("st6_%d" % i, [128, 2, 6], F32) for i in range(n)]
            LN["mv"] = [lsb("mv%d" % i, [128, 4], F32) for i in range(n)]
            LN["B_xn"] = [Buf("xn%d" % i) for i in range(nxn)]
            LN["B_st"] = [Buf("st%d" % i) for i in range(n)]
            LN["B_mv"] = [Buf("mv%d" % i) for i in range(n)]

        phase_begin()
        NXS = 4
        xs = [lsb("xs%d" % i, [128, D], F32) for i in range(NXS)]
        B_xs = [Buf("xs%d" % i) for i in range(NXS)]
        s_xs = [k.new_sem("xs%d" % i) for i in range(NXS)]
        alloc_ln()
        psT = [ps[4][:].bitcast(BF16), ps[5][:].bitcast(BF16)]

        def ln_stats(xt, Bx, sl):
            st6, mv, B_st, B_mv = LN["st6"], LN["mv"], LN["B_st"], LN["B_mv"]
            for hh in range(2):
                k.op("dve", lambda e, hh=hh: e.bn_stats(out=st6[sl][:, hh, :], in_=xt[:, hh * 512:(hh + 1) * 512]),
                     r=[Bx], w=[B_st[sl]])
            k.op("dve", lambda e: e.bn_aggr(out=mv[sl][:, 0:2], in_=st6[sl][:].rearrange("p a b -> p (a b)")),
                 r=[B_st[sl]], w=[B_mv[sl]])
            k.op("act", lambda e: e.activation(out=mv[sl][:, 2:3], in_=mv[sl][:, 1:2], func=AF.Sqrt, bias=EPS),
                 r=[B_mv[sl]], w=[B_mv[sl]])
            k.op("dve", lambda e: e.reciprocal(out=mv[sl][:, 2:3], in_=mv[sl][:, 2:3]), r=[B_mv[sl]], w=[B_mv[sl]])
            k.op("dve", lambda e: e.scalar_tensor_tensor(out=mv[sl][:, 3:4], in0=mv[sl][:, 0:1], scalar=-1.0,
                                                         in1=mv[sl][:, 2:3], op0=ALU.mult, op1=ALU.mult),
                 r=[B_mv[sl]], w=[B_mv[sl]])

        def to_featT(i, xt, Bx, dst, Bdst, col0, scol, bcol, bank=None, slo=0):
            sl = i % 2 + slo
            xn, mv, B_xn, B_mv = LN["xn"], LN["mv"], LN["B_xn"], LN["B_mv"]
            ln_stats(xt, Bx, sl)
            yield
            k.op("act", lambda e: e.activation(out=xn[i % 2][:], in_=xt[:], func=AF.Identity,
                                               scale=mv[sl][:, 2:3], bias=mv[sl][:, 3:4]),
                 r=[Bx, B_mv[sl]], w=[B_xn[i % 2]])
            yield
            if bank is None:
                bank = 4 + i % 2
            psT_ = ps[bank][:].bitcast(BF16)
            for kk in range(8):
                k.op("pe", lambda e, kk=kk: e.transpose(out=psT_[:, kk * 128:(kk + 1) * 128],
                                                        in_=xn[i % 2][:, kk * 128:(kk + 1) * 128], identity=ident[:]),
                     r=[B_xn[i % 2], B_ident], w=[B_ps[bank]], inc=(kk == 7))
            for kk in range(8):
                if kk % 2 == 0:
                    k.op("act", lambda e, kk=kk: e.activation(
                        out=dst[:, kk, col0:col0 + 128], in_=psT_[:, kk * 128:(kk + 1) * 128], func=AF.Identity,
                        scale=adaP[:, scol + kk:scol + kk + 1], bias=adaP[:, bcol + kk:bcol + kk + 1]),
                        r=[B_ps[bank], B_adaP], w=[Bdst])
                else:
                    k.op("dve", lambda e, kk=kk: e.tensor_scalar(
                        out=dst[:, kk, col0:col0 + 128], in0=psT_[:, kk * 128:(kk + 1) * 128],
                        scalar1=adaP[:, scol + kk:scol + kk + 1], scalar2=adaP[:, bcol + kk:bcol + kk + 1],
                        op0=ALU.mult, op1=ALU.add), r=[B_ps[bank], B_adaP], w=[Bdst])

        def run_il(gens):
            live = list(gens)
            while live:
                for g in list(live):
                    try:
                        next(g)
                    except StopIteration:
                        live.remove(g)

        gs_ = []
        for i in range(NT + 2):
            if i < NT:
                sl = i % NXS
                k.dma("sp", xs[sl][:], x_d[i * 128:(i + 1) * 128, :], s_xs[sl], w=[B_xs[sl]])
                g = to_featT(i, xs[sl], B_xs[sl], hT, B_hT[i], i * 128, 8, 0)
                next(g)
                gs_.append(g)
            if 1 <= i <= NT:
                next(gs_[i - 1])
            if i >= 2:
                for _ in gs_[i - 2]:
                    pass

        if debug:
            s_dbg = k.new_sem("dbg")
            k.dma("sp", dbg["hT"][:, :, :], hT[:], s_dbg, r=B_hT)
            k.dma("sp", dbg["ada"][:, :], adaP[:], s_dbg, r=[B_adaP])
            k.dma("sp", dbg["gt"][:, :], gtB[:], s_dbg, r=[B_gtB])

        phase_end()
        fm = []
        for j in range(4): fm.append((j, O_AQ + j * 128, 128))
        for j in range(4): fm.append((4 + j, O_AK + j * 128, 128))
        fm.append((8, O_IK, 32))
        for j in range(2): fm.append((9 + j, O_GQ + j * 128, 128))
        for j in range(2): fm.append((11 + j, O_GK + j * 128, 128))
        fm.append((13, O_GLR, 16))
        tm = [(0, O_AV, 512), (512, O_IQ, 512), (1024, O_GV, 512), (1536, O_GR, 512), (2048, (O_GK, O_IW), 272),
              (2320, O_GA, 512), (2832, O_GA + 512, 512), (3344, O_GB, 512), (3856, O_GB + 512, 512)]
        w_in_v = w_in_d.rearrange("(k p) j -> p k j", p=128)
        vstack = ExitStack()
        v_aug = _chk("v_aug_p", vstack.enter_context(nc.sbuf_tensor("v_aug_p", [128, NT, 8, 65], BF16)))
        B_vt = [Buf("v_aug%d" % i) for i in range(NT)]
        k.op("pool", lambda e: e.memset(v_aug[:], 1.0), w=B_vt)
        phase_begin()
        wfm = [lsb("wfm%d" % i, [128, 8, 128], BF16) for i in range(2)]
        B_wfm = [Buf("wfm0"), Buf("wfm1")]
        s_wfm = [k.new_sem("wfm0"), k.new_sem("wfm1")]
        stg = [lsb("stg%d" % i, [128, S], BF16) for i in range(2)]
        B_stg = [Buf("stg0"), Buf("stg1")]
        s_stg = [k.new_sem("stg0"), k.new_sem("stg1")]
        B_featT = [Buf("featT%d" % i) for i in range(NFT)]
        ev = 0
        for n, (fi, c0, ncol) in enumerate(fm):
            sl = n % 2
            if ncol == 128:
                k.dma("pool", wfm[sl][:], w_in_v[:, :, c0:c0 + 128], s_wfm[sl], w=[B_wfm[sl]])
                M = 128
            elif ncol == 32:
                for rep in range(4):
                    k.dma("pool", wfm[sl][:, :, rep * 32:(rep + 1) * 32], w_in_v[:, :, c0:c0 + 32], s_wfm[sl], w=[B_wfm[sl]])
                M = 128
            else:
                k.dma("pool", wfm[sl][:, :, 0:16], w_in_v[:, :, c0:c0 + 16], s_wfm[sl], w=[B_wfm[sl]])
                M = 16
            for tb in range(8):
                bank = tb % 4
                for kk in range(8):
                    k.op("pe", lambda e, kk=kk, tb=tb, bank=bank, M=M: e.matmul(
                        ps[bank][0:M, :], lhsT=wfm[sl][:, kk, 0:M], rhs=hT[:, kk, tb * 512:(tb + 1) * 512],
                        start=(kk == 0), stop=(kk == 7)),
                        r=[B_wfm[sl]] + B_hT[tb * 4:(tb + 1) * 4], w=[B_ps[bank]], inc=(kk == 7))
                if ev % 2 == 0:
                    k.op("act", lambda e, tb=tb, bank=bank, M=M: e.activation(
                        out=stg[sl][0:M, tb * 512:(tb + 1) * 512], in_=ps[bank][0:M, :], func=AF.Identity),
                        r=[B_ps[bank]], w=[B_stg[sl]])
                else:
                    k.op("dve", lambda e, tb=tb, bank=bank, M=M: e.tensor_copy(
                        out=stg[sl][0:M, tb * 512:(tb + 1) * 512], in_=ps[bank][0:M, :]),
                        r=[B_ps[bank]], w=[B_stg[sl]])
                ev += 1
            k.dma("sp", featT_d[fi, 0:M, :], stg[sl][0:M, :], s_stg[sl], r=[B_stg[sl]], w=[B_featT[fi]])

        wtm = [lsb("wtm%d" % i, [128, 8, 512], BF16) for i in range(2)]
        B_wtm = [Buf("wtm0"), Buf("wtm1")]
        s_wtm = [k.new_sem("wtm0"), k.new_sem("wtm1")]
        NTS = 8
        tstg = [lsb("tstg%d" % i, [128, 512], BF16) for i in range(NTS)]
        B_tstg = [Buf("tstg%d" % i) for i in range(NTS)]
        s_tstg = [k.new_sem("tstg%d" % i) for i in range(NTS)]
        B_tok = Buf("tok")
        cnt = 0

        def load_wtm(n):
            t0_, c0_, ncol_ = tm[n]
            if isinstance(c0_, tuple):
                k.dma("pool", wtm[n % 2][:, :, 0:256], w_in_v[:, :, c0_[0]:c0_[0] + 256], s_wtm[n % 2], w=[B_wtm[n % 2]])
                k.dma("pool", wtm[n % 2][:, :, 256:272], w_in_v[:, :, c0_[1]:c0_[1] + 16], s_wtm[n % 2], w=[B_wtm[n % 2]])
            else:
                k.dma("pool", wtm[n % 2][:, :, 0:ncol_], w_in_v[:, :, c0_:c0_ + ncol_], s_wtm[n % 2], w=[B_wtm[n % 2]])

        load_wtm(0)
        for n, (t0, c0, ncol) in enumerate(tm):
            sl = n % 2
            if n + 1 < len(tm):
                load_wtm(n + 1)
            for i in range(NT):
                bank = i % 4
                for kk in range(8):
                    k.op("pe", lambda e, kk=kk, i=i, bank=bank: e.matmul(
                        ps[bank][:, 0:ncol], lhsT=hT[:, kk, i * 128:(i + 1) * 128], rhs=wtm[sl][:, kk, 0:ncol],
                        start=(kk == 0), stop=(kk == 7)),
                        r=[B_wtm[sl], B_hT[i]], w=[B_ps[bank]], inc=(kk == 7))
                if n == 0:
                    v_out = v_aug[:, i, :, 0:64]
                    v_in = ps[bank][:, 0:512].rearrange("p (h d) -> p h d", h=8)
                    if cnt % 2 == 0:
                        k.op("act", lambda e, v_out=v_out, v_in=v_in: e.activation(out=v_out, in_=v_in, func=AF.Identity),
                             r=[B_ps[bank]], w=[B_vt[i]])
                    else:
                        k.op("dve", lambda e, v_out=v_out, v_in=v_in: e.tensor_copy(out=v_out, in_=v_in),
                             r=[B_ps[bank]], w=[B_vt[i]])
                    cnt += 1
                    continue
                ts = cnt % NTS
                if cnt % 2 == 0:
                    k.op("act", lambda e, bank=bank, ts=ts: e.activation(
                        out=tstg[ts][:, 0:ncol], in_=ps[bank][:, 0:ncol], func=AF.Identity),
                        r=[B_ps[bank]], w=[B_tstg[ts]])
                else:
                    k.op("dve", lambda e, bank=bank, ts=ts: e.tensor_copy(
                        out=tstg[ts][:, 0:ncol], in_=ps[bank][:, 0:ncol]),
                        r=[B_ps[bank]], w=[B_tstg[ts]])
                k.dma("sp" if cnt % 2 == 0 else "pool", tok_d[i * 128:(i + 1) * 128, t0:t0 + ncol], tstg[ts][:, 0:ncol], s_tstg[ts],
                      r=[B_tstg[ts]], w=[B_tok])
                cnt += 1

        phase_end()
        phase_begin()
        BIG = hT
        kT = BIG[:, 0:4, :]
        ikT = BIG[:, 4, :]
        A = BIG[:, 5:7, :].rearrange("p a b -> p (a b)").bitcast(F32)
        msk = BIG[:, 7, :]
        B_kc = Buf("kcache"); B_A = Buf("A"); B_msk = Buf("msk")
        B_v = Buf("v_aug")
        maskT0 = lsb("maskT", [128, NT, 128], BF16)
        NBTh = lsb("NBTh", [128, 2, 8, 128], BF16)
        NBTl = lsb("NBTl", [128, 2, 8, 128], BF16)
        cB = lsb("cB", [128, 8], F32)
        c8 = lsb("c8", [128, 8], F32)
        B_NBT = Buf("NBT")
        tri_in = lsb("tri_in", [128, 128], BF16)
        tri_st = lsb("tri_st", [128, 128], BF16)
        trif = lsb("trif", [128, 128], F32)
        B_tri = Buf("tri")
        wg = lsb("wg", [16, 256], BF16)
        bg = lsb("bg", [1, 256], BF16)
        wgf = lsb("wgf", [16, 256], F32)
        bgf = lsb("bgf", [1, 256], F32)
        gB = lsb("gB", [128, 512], F32)
        B_gc = Buf("glaconst")
        pw = lsb("pw", [128, NIT + 1], F32)
        B_pw = Buf("pw")
        s_c = k.new_sem("p3c")
        s_c2 = k.new_sem("p3c2")
        psb = [p[:].bitcast(BF16) for p in ps]

        s_cp = k.new_sem("p3cp")
        for j in range(4):
            k.dma(("sp", "act", "pool", "act")[j], kT[:, j, :], featT_d[4 + j, :, :], s_cp if j == 2 else s_c, r=[B_featT[4 + j]], w=[B_kc])
        k.dma("sp", ikT, featT_d[8, :, :], s_c, r=[B_featT[8]], w=[B_kc])
        k.op("pool", lambda e: e.memset(trif[:], 1.0), w=[B_tri])
        k.op("pool", lambda e: e.affine_select(out=trif[:], in_=trif[:], pattern=[[1, 128]], compare_op=ALU.is_ge,
                                               fill=0.0, base=0, channel_multiplier=-1), r=[B_tri], w=[B_tri])
        k.op("pool", lambda e: e.tensor_copy(out=tri_in[:], in_=trif[:]), r=[B_tri], w=[B_tri])
        k.op("pool", lambda e: e.memset(trif[:], 1.0), r=[B_tri], w=[B_tri])
        k.op("pool", lambda e: e.affine_select(out=trif[:], in_=trif[:], pattern=[[-1, 128]], compare_op=ALU.is_gt,
                                               fill=0.0, base=0, channel_multiplier=1), r=[B_tri], w=[B_tri])
        k.op("pool", lambda e: e.tensor_copy(out=tri_st[:], in_=trif[:]), r=[B_tri], w=[B_tri])
        for t in range(NIT + 1):
            k.op("pool", lambda e, t=t: e.memset(pw[:, t:t + 1], 2.0 ** -(t + 1)), w=[B_pw])
        k.dma("sp", wgf[:], gla_wg_d[:, :], s_c, w=[B_gc])
        k.dma("sp", bgf[:], gla_bg_d[:, :], s_c, w=[B_gc])
        k.dma("sp", gB[:], gla_g_d[0:1, :].broadcast_to([128, 512]), s_c, w=[B_gc])
        k.op("dve", lambda e: e.tensor_copy(out=wg[:], in_=wgf[:]), r=[B_gc], w=[B_gc])
        k.op("dve", lambda e: e.tensor_copy(out=bg[:], in_=bgf[:]), r=[B_gc], w=[B_gc])
        ph2 = ExitStack()
        rb = ph2.enter_context(nc.sbuf_tensor("rb", [32, 8], F32))
        rbB = ph2.enter_context(nc.sbuf_tensor("rbB", [32, 8, 128], F32))
        oh = ph2.enter_context(nc.sbuf_tensor("oh", [32, 384], F32))
        Rrep = ph2.enter_context(nc.sbuf_tensor("Rrep", [128, 8, 384], F32))
        NBT = ph2.enter_context(nc.sbuf_tensor("NBTf", [128, 2, 8, 128], F32))
        B_rb = Buf("rb"); B_Rrep = Buf("Rrep"); B_A2 = Buf("A2")
        k.dma("sp", rb[:], rel_bias_d[:, :], s_c2, w=[B_rb])
        k.dma("sp", oh[:], ohpad_d[:, :], s_c2, w=[B_rb])
        k.op("dve", lambda e: e.tensor_copy(out=rbB[:], in_=rb[:].unsqueeze(2).broadcast_to([32, 8, 128])), r=[B_rb], w=[B_rb])
        for h in range(8):
            bank = h % 2
            k.op("pe", lambda e, h=h, bank=bank: e.matmul(ps[bank][:, 0:384], lhsT=rbB[:, h, :], rhs=oh[:, :], start=True, stop=True),
                 r=[B_rb], w=[B_ps[bank]])
            k.op("act", lambda e, h=h, bank=bank: e.activation(out=Rrep[:, h, :], in_=ps[bank][:, 0:384], func=AF.Identity),
                 r=[B_ps[bank]], w=[B_Rrep])
        k.dma("sp", a2_d.rearrange("h p m -> p h m"), Rrep[:], s_c2, r=[B_Rrep], w=[B_A2])
        for t in range(2):
            src = bass.AP(tensor=a2_d.tensor, offset=128 + 128 * t, ap=[[383, 128], [128 * 384, 8], [1, 128]])
            k.dma("sp", NBT[:, t, :, :], src, s_c2, r=[B_A2], w=[B_NBT])
        k.op("dve", lambda e: e.tensor_copy(out=c8[:], in_=Rrep[:, :, 383]), r=[B_Rrep], w=[B_NBT])
        k.op("dve", lambda e: e.tensor_scalar(out=cB[:], in0=c8[:], scalar1=0.125, scalar2=None, op0=ALU.mult), r=[B_NBT], w=[B_NBT])
        for t in range(2):
            for h in range(8):
                k.op("dve", lambda e, t=t, h=h: e.tensor_scalar(out=NBT[:, t, h, :], in0=NBT[:, t, h, :], scalar1=c8[:, h:h + 1],
                                                                scalar2=None, op0=ALU.subtract), r=[B_NBT], w=[B_NBT])
        k.op("dve", lambda e: e.tensor_copy(out=NBTh[:].rearrange("p a b c -> p (a b c)"), in_=NBT[:].rearrange("p a b c -> p (a b c)")),
             r=[B_NBT], w=[B_NBT])
        k.op("dve", lambda e: e.tensor_tensor(out=NBTl[:].rearrange("p a b c -> p (a b c)"), in0=NBT[:].rearrange("p a b c -> p (a b c)"),
                                              in1=NBTh[:].rearrange("p a b c -> p (a b c)"), op=ALU.subtract), r=[B_NBT], w=[B_NBT])
        k.barrier()
        ph2.close()

        nt3 = NT3
        maskT = [maskT0, lsb("maskT1", [128, NT, 128], BF16)]
        B_maskT = [Buf("maskT0"), Buf("maskT1")]
        qT_t = [lsb("qT_t%d" % i, [128, 4, 128], BF16) for i in range(2)]
        gT_t = [lsb("gT_t%d" % i, [128, 4, 128], BF16) for i in range(2)]
        glr_t = [lsb("glr_t%d" % i, [16, 128], BF16) for i in range(2)]
        tok_t = [lsb("tok_t%d" % i, [128, 1808], BF16) for i in range(2)]
        B_ld = [Buf("ld0"), Buf("ld1")]
        s_ld = [k.new_sem("ld0"), k.new_sem("ld1")]
        wabs = lsb("wabs", [128, 16], F32)
        sgn = lsb("sgn", [128, 16], F32)
        sgnD = lsb("sgnD", [128, 16, 128], BF16)
        iqs = lsb("iqs", [128, 512], BF16)
        iqT = lsb("iqT", [128, 4, 128], BF16)
        B_iq = Buf("iq"); B_iqT = Buf("iqT"); B_sgnD = Buf("sgnD")
        NRH = 8
        Rh = [lsb("Rh%d" % i, [128, 512], BF16) for i in range(NRH)]
        B_Rh = [Buf("Rh%d" % i) for i in range(NRH)]
        bis = lsb("bis", [128, 8], F32)
        nmid = lsb("nmid", [128, NIT + 1], F32)
        whs = lsb("whs", [128, NIT + 1], F32)
        whs2 = lsb("whs2", [128, NIT + 1], F32)
        mid = lsb("mid", [128, NIT + 1], F32)
        B_bis = Buf("bis"); B_bisA = Buf("bisA"); B_bisD = Buf("bisD"); B_msk2 = Buf("msk2")
        expS = [lsb("expS%d" % i, [128, 512], BF16) for i in range(2)]
        B_expS = [Buf("expS0"), Buf("expS1")]
        PT = [lsb("PT%d" % i, [128, 512], BF16) for i in range(2)]
        B_PT = [Buf("PT0"), Buf("PT1")]
        rec = lsb("rec", [128, 8], F32)
        o_a = lsb("o_a", [128, 8, 64], BF16)
        o_aT = [lsb("o_aT%d" % i, [128, 4, 128], BF16) for i in range(2)]
        B_oa = Buf("o_a"); B_oaT = [Buf("o_aT0"), Buf("o_aT1")]
        s_oaT = [k.new_sem("oaT0"), k.new_sem("oaT1")]
        B_oaTd = Buf("oaTd"); B_obTd = Buf("obTd")
        ge_ = lsb("g_e", [128, 256], F32)
        lgf = lsb("g_lgf", [128, 256], F32)
        lgh = lsb("g_lgh", [128, 256], BF16)
        lgl = lsb("g_lgl", [128, 256], BF16)
        ET = lsb("g_ET", [128, 3, 256], F32)
        E2 = lsb("g_E2", [128, 256], F32)
        qin = lsb("g_qin", [128, 2, 128], BF16)
        kst = lsb("g_kst", [128, 2, 128], BF16)
        qrl = lsb("g_qrl", [128, 2, 128], BF16)
        kstk = lsb("g_kstk", [128, 256], BF16)
        attT = lsb("g_attT", [128, 4, 128], BF16)
        Sst = lsb("g_S", [128, 2, 128], F32)
        Sb = lsb("g_Sb", [128, 2, 128], BF16)
        gst = lsb("g_st", [128, 4, 6], F32)
        gmv = lsb("g_mv", [128, 4, 2], F32)
        grs = lsb("g_rs", [128, 8], F32)
        on = lsb("g_on", [128, 512], F32)
        sg = lsb("g_sg", [128, 512], F32)
        ob = lsb("g_ob", [128, 512], BF16)
        o_bT = [lsb("o_bT%d" % i, [128, 4, 128], BF16) for i in range(2)]
        B_g1 = Buf("g1"); B_lg = Buf("lg"); B_ET = Buf("ET"); B_gq = Buf("gq"); B_att = Buf("att")
        B_S = Buf("S"); B_Sb = Buf("Sb"); B_gs = Buf("gs"); B_on = Buf("on"); B_ob = Buf("ob")
        B_obT = [Buf("o_bT0"), Buf("o_bT1")]
        s_obT = [k.new_sem("obT0"), k.new_sem("obT1")]
        k.op("dve", lambda e: e.memset(Sst[:], 0.0), w=[B_S])
        k.op("dve", lambda e: e.memset(Sb[:], 0.0), w=[B_Sb])
        WC = (16.0 ** -0.5) * (32.0 ** -0.5)
        evc = [0]

        def stage_idx(i):
            sl = i % 2
            n = (i + 1) * 128
            k.dma("sp", qT_t[sl][:], featT_d[0:4, :, i * 128:(i + 1) * 128].rearrange("c p t -> p c t"), s_ld[sl],
                  r=B_featT[0:4], w=[B_ld[sl]])
            k.dma("sp", gT_t[sl][:], featT_d[9:13, :, i * 128:(i + 1) * 128].rearrange("c p t -> p c t"), s_ld[sl],
                  r=B_featT[9:13], w=[B_ld[sl]])
            k.dma("sp", glr_t[sl][:], featT_d[13, 0:16, i * 128:(i + 1) * 128], s_ld[sl], r=[B_featT[13]], w=[B_ld[sl]])
            k.dma("sp", tok_t[sl][:], tok_d[i * 128:(i + 1) * 128, 512:2320], s_ld[sl], r=[B_tok], w=[B_ld[sl]])
            tk = tok_t[sl]
            iq_v = tk[:, 0:512]; iw_v = tk[:, 1792:1808]
            k.op("act", lambda e: e.activation(out=wabs[:], in_=iw_v, func=AF.Abs, scale=WC), r=[B_ld[sl]], w=[B_iq])
            k.op("dve", lambda e: e.tensor_scalar(out=sgn[:], in0=iw_v, scalar1=0.0, scalar2=2.0, op0=ALU.is_gt, op1=ALU.mult),
                 r=[B_ld[sl]], w=[B_iq])
            k.op("dve", lambda e: e.tensor_scalar(out=sgn[:], in0=sgn[:], scalar1=-1.0, scalar2=None, op0=ALU.add), r=[B_iq], w=[B_iq])
            k.op("dve", lambda e: e.tensor_tensor(out=sgnD[:], in0=ident[:].unsqueeze(1).broadcast_to([128, 16, 128]),
                                                  in1=sgn[:].unsqueeze(2).broadcast_to([128, 16, 128]), op=ALU.mult),
                 r=[B_iq, B_ident], w=[B_sgnD])
            k.op("dve", lambda e: e.tensor_tensor(out=iqs[:].rearrange("p (h d) -> p h d", h=16),
                                                  in0=iq_v.rearrange("p (h d) -> p h d", h=16),
                                                  in1=wabs[:].unsqueeze(2).broadcast_to([128, 16, 32]), op=ALU.mult),
                 r=[B_ld[sl], B_iq], w=[B_iq])
            for c in range(4):
                k.op("pe", lambda e, c=c: e.transpose(out=psb[3][:, c * 128:(c + 1) * 128], in_=iqs[:, c * 128:(c + 1) * 128],
                                                      identity=ident[:]), r=[B_iq, B_ident], w=[B_ps[3]], inc=(c == 3))
            k.op("act", lambda e: e.activation(out=iqT[:].rearrange("p c t -> p (c t)"), in_=psb[3][:, 0:512], func=AF.Identity),
                 r=[B_ps[3]], w=[B_iqT])
            nkb = (i + 4) // 4
            for kb in range(nkb):
                k0 = kb * 512
                nk = min(512, n - k0)
                sbank = 4 + kb % 2
                pend = []
                for c in range(4):
                    for hh in range(4):
                        pb = 32 * hh
                        k.op("pe", lambda e, c=c, pb=pb, hh=hh: e.matmul(
                            ps[hh][:, 0:nk], lhsT=iqT[pb:pb + 32, c, :], rhs=ikT[pb:pb + 32, k0:k0 + nk], start=True, stop=True,
                            tile_position=(pb, 0)), r=[B_iqT, B_kc], w=[B_ps[hh]])
                    for (ph_, prs) in pend:
                        k.op("pe", lambda e, ph_=ph_, prs=prs: e.matmul(ps[sbank][:, 0:nk], lhsT=sgnD[:, ph_, :], rhs=Rh[prs][:, 0:nk],
                                                                        start=(ph_ == 0), stop=False),
                             r=[B_sgnD, B_Rh[prs]], w=[B_ps[sbank]], inc=False)
                    pend = []
                    for hh in range(4):
                        h = c * 4 + hh
                        rs = (c % 2) * 4 + hh
                        if hh < 3:
                            k.op("act", lambda e, hh=hh, rs=rs: e.activation(out=Rh[rs][:, 0:nk], in_=ps[hh][:, 0:nk], func=AF.Relu),
                                 r=[B_ps[hh]], w=[B_Rh[rs]])
                        else:
                            k.op("dve", lambda e, hh=hh, rs=rs: e.tensor_scalar(out=Rh[rs][:, 0:nk], in0=ps[hh][:, 0:nk], scalar1=0.0,
                                                                                scalar2=None, op0=ALU.max), r=[B_ps[hh]], w=[B_Rh[rs]])
                        pend.append((h, rs))
                for (ph_, prs) in pend:
                    k.op("pe", lambda e, ph_=ph_, prs=prs: e.matmul(ps[sbank][:, 0:nk], lhsT=sgnD[:, ph_, :], rhs=Rh[prs][:, 0:nk],
                                                                    start=False, stop=(ph_ == 15)),
                         r=[B_sgnD, B_Rh[prs]], w=[B_ps[sbank]], inc=(ph_ == 15))
                if kb % 2 == 0:
                    k.op("act", lambda e, sbank=sbank: e.activation(out=A[:, k0:k0 + nk], in_=ps[sbank][:, 0:nk], func=AF.Identity),
                         r=[B_ps[sbank]], w=[B_A])
                else:
                    k.op("dve", lambda e, sbank=sbank: e.tensor_copy(out=A[:, k0:k0 + nk], in_=ps[sbank][:, 0:nk]),
                         r=[B_ps[sbank]], w=[B_A])

        def stage_bis(i):
            n = (i + 1) * 128
            mp = i % 2
            k.op("pool", lambda e: e.affine_select(out=A[:, i * 128:n], in_=A[:, i * 128:n], pattern=[[-1, 128]],
                                                   compare_op=ALU.is_ge, fill=NEG, base=0, channel_multiplier=1),
                 r=[B_A], w=[B_A])
            if i < 2:
                k.op("dve", lambda e: e.memset(bis[:, 0:1], -1.0e29), w=[B_bis])
            else:
                k.op("dve", lambda e: e.tensor_reduce(out=bis[:, 5:6], in_=A[:, 0:i * 128], axis=AX.X, op=ALU.max,
                                                      apply_absolute_value=True), r=[B_A], w=[B_bis])
                k.op("dve", lambda e: e.tensor_reduce(out=bis[:, 4:5], in_=A[:, 0:n], axis=AX.X, op=ALU.max),
                     r=[B_A], w=[B_bis])
                yield
                k.op("dve", lambda e: e.scalar_tensor_tensor(out=bis[:, 4:5], in0=bis[:, 4:5], scalar=2.0, in1=bis[:, 5:6],
                                                             op0=ALU.add, op1=ALU.add), r=[B_bis], w=[B_bis])
                k.op("dve", lambda e: e.tensor_scalar(out=whs[:], in0=pw[:], scalar1=bis[:, 4:5], scalar2=None, op0=ALU.mult),
                     r=[B_bis, B_pw], w=[B_bis])
                k.op("dve", lambda e: e.tensor_scalar(out=whs2[:], in0=whs[:], scalar1=2.0, scalar2=None, op0=ALU.mult),
                     r=[B_bis], w=[B_bis])
                k.op("dve", lambda e: e.scalar_tensor_tensor(out=nmid[:, 0:1], in0=bis[:, 5:6], scalar=1.0, in1=whs[:, 0:1],
                                                             op0=ALU.add, op1=ALU.subtract), r=[B_bis], w=[B_bis])
                k.op("dve", lambda e: e.tensor_scalar(out=mid[:, 0:1], in0=nmid[:, 0:1], scalar1=-1.0, scalar2=None, op0=ALU.mult),
                     r=[B_bis], w=[B_bis])
                for t in range(NIT):
                    k.op("dve", lambda e, t=t: e.tensor_scalar(out=msk[:, 0:n], in0=A[:, 0:n], scalar1=mid[:, t:t + 1], scalar2=None,
                                                               op0=ALU.is_ge, op1=ALU.add, accum_out=bis[:, 3:4]),
                         r=[B_A, B_bis], w=[B_msk, B_bis])
                    k.op("dve", lambda e, t=t: e.tensor_scalar(out=bis[:, 7:8], in0=bis[:, 3:4], scalar1=255.5, scalar2=whs2[:, t + 1:t + 2],
                                                               op0=ALU.is_ge, op1=ALU.mult), r=[B_bis], w=[B_bis])
                    k.op("dve", lambda e, t=t: e.scalar_tensor_tensor(out=mid[:, t + 1:t + 2], in0=bis[:, 7:8], scalar=mid[:, t:t + 1],
                                                                      in1=whs[:, t + 1:t + 2], op0=ALU.add, op1=ALU.subtract),
                         r=[B_bis], w=[B_bis])
                    yield
                k.op("dve", lambda e: e.tensor_tensor(out=bis[:, 0:1], in0=mid[:, NIT:NIT + 1], in1=whs[:, NIT:NIT + 1], op=ALU.subtract),
                     r=[B_bis], w=[B_bis])
            k.op("dve", lambda e: e.tensor_scalar(out=msk[:, 0:n], in0=A[:, 0:n], scalar1=bis[:, 0:1], scalar2=None, op0=ALU.is_ge),
                 r=[B_A, B_bis], w=[B_msk, B_msk2])
            yield
            for g0 in range(0, i + 1, 8):
                g1 = min(g0 + 8, i + 1)
                for j in range(g0, g1):
                    k.op("pe", lambda e, j=j, g0=g0: e.transpose(out=psb[3][:, (j - g0) * 128:(j - g0 + 1) * 128],
                                                                 in_=msk[:, j * 128:(j + 1) * 128], identity=ident[:]),
                         r=[B_msk, B_ident], w=[B_ps[3]], inc=(j == g1 - 1))
                k.op("act", lambda e, g0=g0, g1=g1: e.activation(out=maskT[mp][:, g0:g1, :].rearrange("p j q -> p (j q)"),
                                                                 in_=psb[3][:, 0:(g1 - g0) * 128], func=AF.Identity),
                     r=[B_ps[3]], w=[B_maskT[mp]])
                yield

        def stage_attn(i):
            sl = i % 2
            mp = i % 2
            cntS = 0

            def emit_pv(h, g0, g1, es_):
                obank = 4 + h // 4
                for j in range(g0, g1):
                    k.op("pe", lambda e, j=j: e.matmul(
                        ps[obank][:, (h % 4) * 65:(h % 4) * 65 + 65], lhsT=PT[es_][:, (j - g0) * 128:(j - g0 + 1) * 128],
                        rhs=v_aug[:, j, h, :], start=(j == 0), stop=(j == i)),
                        r=[B_PT[es_], B_v], w=[B_ps[obank]], inc=(j == g1 - 1))

            prev = None
            for h in range(8):
                c = h // 2; pb = 64 * (h % 2)
                for g0 in range(0, i + 1, 4):
                    g1 = min(g0 + 4, i + 1)
                    ng = g1 - g0
                    bank = cntS % 3
                    es_ = cntS % 2
                    cntS += 1
                    for j in range(g0, g1):
                        near = j >= i - 1
                        k.op("pe", lambda e, j=j, g0=g0, bank=bank, near=near: e.matmul(
                            ps[bank][:, (j - g0) * 128:(j - g0 + 1) * 128], lhsT=kT[pb:pb + 64, c, j * 128:(j + 1) * 128],
                            rhs=qT_t[sl][pb:pb + 64, c, :], start=True, stop=(not near)),
                            r=[B_kc, B_ld[sl]], w=[B_ps[bank]], inc=(j == g1 - 1 and not near))
                        if near:
                            t = 0 if j == i else 1
                            k.op("pe", lambda e, j=j, g0=g0, bank=bank, t=t: e.matmul(
                                ps[bank][:, (j - g0) * 128:(j - g0 + 1) * 128], lhsT=ident[:, :], rhs=NBTh[:, t, h, :],
                                start=False, stop=False), r=[B_ident, B_NBT], w=[B_ps[bank]], inc=False)
                            k.op("pe", lambda e, j=j, g0=g0, bank=bank, t=t: e.matmul(
                                ps[bank][:, (j - g0) * 128:(j - g0 + 1) * 128], lhsT=ident[:, :], rhs=NBTl[:, t, h, :],
                                start=False, stop=True), r=[B_ident, B_NBT], w=[B_ps[bank]], inc=(j == g1 - 1))
                    k.op("act", lambda e, bank=bank, es_=es_, ng=ng: e.activation(
                        out=expS[es_][:, 0:ng * 128], in_=ps[bank][:, 0:ng * 128], func=AF.Exp, scale=0.125, bias=cB[:, h:h + 1]),
                        r=[B_ps[bank], B_NBT], w=[B_expS[es_]])
                    k.op("pool", lambda e, es_=es_, ng=ng, g0=g0, g1=g1: e.tensor_tensor(
                        out=PT[es_][:, 0:ng * 128], in0=expS[es_][:, 0:ng * 128],
                        in1=maskT[mp][:, g0:g1, :].rearrange("p j q -> p (j q)"), op=ALU.mult),
                        r=[B_expS[es_], B_maskT[mp]], w=[B_PT[es_]])
                    if prev is not None:
                        emit_pv(*prev)
                    prev = (h, g0, g1, es_)
                    yield
            emit_pv(*prev)
            for hb in range(2):
                pv = ps[4 + hb][:, 0:260].rearrange("p (h d) -> p h d", h=4)
                k.op("dve", lambda e, hb=hb, pv=pv: e.reciprocal(out=rec[:, hb * 4:(hb + 1) * 4], in_=pv[:, :, 64]),
                     r=[B_ps[4 + hb]], w=[B_oa])
                k.op("dve", lambda e, hb=hb, pv=pv: e.tensor_tensor(
                    out=o_a[:, hb * 4:(hb + 1) * 4, :], in0=pv[:, :, 0:64],
                    in1=rec[:, hb * 4:(hb + 1) * 4].unsqueeze(2).broadcast_to([128, 4, 64]), op=ALU.mult),
                    r=[B_ps[4 + hb], B_oa], w=[B_oa])
            oa2 = o_a[:].rearrange("p h d -> p (h d)")
            for c in range(4):
                k.op("pe", lambda e, c=c: e.transpose(out=psb[3][:, c * 128:(c + 1) * 128], in_=oa2[:, c * 128:(c + 1) * 128],
                                                      identity=ident[:]), r=[B_oa, B_ident], w=[B_ps[3]], inc=(c == 3))
            k.op("act", lambda e: e.activation(out=o_aT[sl][:].rearrange("p c t -> p (c t)"), in_=psb[3][:, 0:512], func=AF.Identity),
                 r=[B_ps[3]], w=[B_oaT[sl]])
            k.dma("pool", oaT_d[:, :, i * 128:(i + 1) * 128].rearrange("c p t -> p c t"), o_aT[sl][:], s_oaT[sl],
                  r=[B_oaT[sl]], w=[B_oaTd])
            yield

        def stage_gla(i):
            sl = i % 2
            tk = tok_t[sl]
            gv_v = tk[:, 512:1024]; gr_v = tk[:, 1024:1536]; gk_v = tk[:, 1536:1792]
            gq_v = gT_t[sl][:, 0:2, :]
            gkT_v = gT_t[sl][:, 2:4, :]
            k.op("pe", lambda e: e.matmul(ps[6][:, 0:256], lhsT=glr_t[sl][:, :], rhs=wg[:, :], start=True, stop=False),
                 r=[B_ld[sl], B_gc], w=[B_ps[6]], inc=False)
            k.op("pe", lambda e: e.matmul(ps[6][:, 0:256], lhsT=ones1[:, :], rhs=bg[:, :], start=False, stop=True),
                 r=[B_gc], w=[B_ps[6]])
            k.op("act", lambda e: e.activation(out=ge_[:], in_=ps[6][:, 0:256], func=AF.Exp, scale=-1.0), r=[B_ps[6]], w=[B_g1])
            k.op("act", lambda e: e.activation(out=ge_[:], in_=ge_[:], func=AF.Ln, bias=1.0), r=[B_g1], w=[B_g1])
            k.op("dve", lambda e: e.tensor_scalar(out=lgf[:], in0=ge_[:], scalar1=-1.0 / 16.0, scalar2=None, op0=ALU.mult),
                 r=[B_g1], w=[B_lg])
            k.op("dve", lambda e: e.tensor_copy(out=lgh[:], in_=lgf[:]), r=[B_lg], w=[B_lg])
            k.op("dve", lambda e: e.tensor_tensor(out=lgl[:], in0=lgf[:], in1=lgh[:], op=ALU.subtract), r=[B_lg], w=[B_lg])
            yield
            for fc in range(2):
                for pi, part in enumerate((lgh, lgl)):
                    k.op("pe", lambda e, fc=fc, part=part, pi=pi: e.matmul(
                        ps[7][:, fc * 128:(fc + 1) * 128], lhsT=part[:, fc * 128:(fc + 1) * 128], rhs=tri_in[:, :],
                        start=(pi == 0), stop=(pi == 1)), r=[B_lg, B_tri], w=[B_ps[7]], inc=False)
                for pi, part in enumerate((lgh, lgl)):
                    k.op("pe", lambda e, fc=fc, part=part, pi=pi: e.matmul(
                        ps[7][:, 256 + fc * 128:256 + (fc + 1) * 128], lhsT=part[:, fc * 128:(fc + 1) * 128], rhs=tri_st[:, :],
                        start=(pi == 0), stop=(pi == 1)), r=[B_lg, B_tri], w=[B_ps[7]], inc=False)
            for pi, part in enumerate((lgh, lgl)):
                k.op("pe", lambda e, part=part, pi=pi: e.matmul(
                    ps[6][:, 256:512], lhsT=tri_st[:, :], rhs=part[:, :], start=(pi == 0), stop=(pi == 1)),
                    r=[B_lg, B_tri], w=[B_ps[6], B_ps[7]], inc=(pi == 1))
            k.op("act", lambda e: e.activation(out=ET[:, 0, :], in_=ps[7][:, 0:256], func=AF.Exp), r=[B_ps[7]], w=[B_ET])
            k.op("act", lambda e: e.activation(out=ET[:, 1, :], in_=ps[7][:, 256:512], func=AF.Exp), r=[B_ps[7]], w=[B_ET])
            k.op("act", lambda e: e.activation(out=ET[:, 2, :], in_=ps[7][:, 256:512], func=AF.Exp, scale=-1.0), r=[B_ps[7]], w=[B_ET])
            k.op("act", lambda e: e.activation(out=E2[:], in_=ps[6][:, 256:512], func=AF.Exp), r=[B_ps[6]], w=[B_ET])
            yield
            gq2 = gq_v.rearrange("p c t -> p (c t)")
            gk2 = gkT_v.rearrange("p c t -> p (c t)")
            k.op("dve", lambda e: e.scalar_tensor_tensor(out=qin[:].rearrange("p c t -> p (c t)"), in0=gq2, scalar=0.125, in1=ET[:, 0, :],
                                                         op0=ALU.mult, op1=ALU.mult), r=[B_ld[sl], B_ET], w=[B_gq])
            k.op("dve", lambda e: e.tensor_tensor(out=kst[:].rearrange("p c t -> p (c t)"), in0=gk2, in1=ET[:, 1, :], op=ALU.mult),
                 r=[B_ld[sl], B_ET], w=[B_gq])
            k.op("dve", lambda e: e.scalar_tensor_tensor(out=qrl[:].rearrange("p c t -> p (c t)"), in0=gq2, scalar=0.125, in1=ET[:, 2, :],
                                                         op0=ALU.mult, op1=ALU.mult), r=[B_ld[sl], B_ET], w=[B_gq])
            k.op("dve", lambda e: e.tensor_tensor(out=kstk[:], in0=gk_v, in1=E2[:], op=ALU.mult), r=[B_ld[sl], B_ET], w=[B_gq])
            yield
            for h in range(4):
                fc = h // 2; pb = 64 * (h % 2)
                abank = 6 if h % 2 == 0 else 7
                k.op("pe", lambda e, h=h, fc=fc, pb=pb, abank=abank: e.matmul(
                    ps[abank][:, fc * 128:(fc + 1) * 128], lhsT=kst[pb:pb + 64, fc, :],
                    rhs=qrl[pb:pb + 64, fc, :], start=True, stop=True),
                    r=[B_gq], w=[B_ps[abank]])
            for h in range(4):
                fc = h // 2
                abank = 6 if h % 2 == 0 else 7
                k.op("dve", lambda e, h=h, fc=fc, abank=abank: e.tensor_tensor(
                    out=attT[:, h, :], in0=ps[abank][:, fc * 128:(fc + 1) * 128], in1=tri_in[:, :], op=ALU.mult),
                    r=[B_ps[abank], B_tri], w=[B_att])
            yield
            for h in range(4):
                fc = h // 2; pb = 64 * (h % 2)
                k.op("pe", lambda e, h=h: e.matmul(ps[7][:, h * 128:(h + 1) * 128], lhsT=attT[:, h, :], rhs=gv_v[:, h * 128:(h + 1) * 128],
                                                   start=True, stop=False), r=[B_att, B_ld[sl]], w=[B_ps[7]], inc=False)
                k.op("pe", lambda e, h=h, fc=fc, pb=pb: e.matmul(ps[7][:, h * 128:(h + 1) * 128], lhsT=qin[pb:pb + 64, fc, :],
                                                                 rhs=Sb[pb:pb + 64, fc, :], start=False, stop=True),
                     r=[B_gq, B_Sb], w=[B_ps[7]], inc=(h == 3))
            yield
            for fc in range(2):
                k.op("pe", lambda e, fc=fc: e.matmul(ps[6][:, fc * 256:(fc + 1) * 256], lhsT=kstk[:, fc * 128:(fc + 1) * 128],
                                                     rhs=gv_v[:, fc * 256:(fc + 1) * 256], start=True, stop=True),
                     r=[B_gq, B_ld[sl]], w=[B_ps[6]], inc=(fc == 1))
            for fc in range(2):
                for hh in range(2):
                    pb = 64 * hh
                    k.op("dve", lambda e, fc=fc, hh=hh, pb=pb: e.scalar_tensor_tensor(
                        out=Sst[pb:pb + 64, fc, :], in0=Sst[pb:pb + 64, fc, :], scalar=ET[pb:pb + 64, 0, fc * 128 + 127:fc * 128 + 128],
                        in1=ps[6][pb:pb + 64, fc * 256 + hh * 128:fc * 256 + (hh + 1) * 128], op0=ALU.mult, op1=ALU.add),
                        r=[B_ET, B_ps[6], B_S], w=[B_S])
            k.op("act", lambda e: e.activation(out=Sb[:].rearrange("p c t -> p (c t)"), in_=Sst[:].rearrange("p c t -> p (c t)"),
                                               func=AF.Identity), r=[B_S], w=[B_Sb])
            yield
            for h in range(4):
                k.op("dve", lambda e, h=h: e.bn_stats(out=gst[:, h, :], in_=ps[7][:, h * 128:(h + 1) * 128]), r=[B_ps[7]], w=[B_gs])
            for h in range(4):
                k.op("dve", lambda e, h=h: e.bn_aggr(out=gmv[:, h, :], in_=gst[:, h, :]), r=[B_gs], w=[B_gs])
            k.op("act", lambda e: e.activation(out=grs[:, 0:4], in_=gmv[:, :, 1], func=AF.Sqrt, bias=EPS), r=[B_gs], w=[B_gs])
            k.op("dve", lambda e: e.reciprocal(out=grs[:, 0:4], in_=grs[:, 0:4]), r=[B_gs], w=[B_gs])
            k.op("dve", lambda e: e.scalar_tensor_tensor(out=grs[:, 4:8], in0=gmv[:, :, 0], scalar=-1.0, in1=grs[:, 0:4],
                                                         op0=ALU.mult, op1=ALU.mult), r=[B_gs], w=[B_gs])
            for h in range(4):
                k.op("act", lambda e, h=h: e.activation(out=on[:, h * 128:(h + 1) * 128], in_=ps[7][:, h * 128:(h + 1) * 128],
                                                        func=AF.Identity, scale=grs[:, h:h + 1], bias=grs[:, 4 + h:5 + h]),
                     r=[B_ps[7], B_gs], w=[B_on])
            k.op("act", lambda e: e.activation(out=sg[:], in_=gr_v, func=AF.Silu), r=[B_ld[sl]], w=[B_ob])
            k.op("dve", lambda e: e.tensor_tensor(out=on[:], in0=on[:], in1=gB[:], op=ALU.mult), r=[B_on, B_gc], w=[B_on])
            k.op("dve", lambda e: e.tensor_tensor(out=ob[:], in0=on[:], in1=sg[:], op=ALU.mult), r=[B_on, B_ob], w=[B_ob])
            for c in range(4):
                k.op("pe", lambda e, c=c: e.transpose(out=psb[3][:, 512 + c * 128:512 + (c + 1) * 128], in_=ob[:, c * 128:(c + 1) * 128],
                                                      identity=ident[:]), r=[B_ob, B_ident], w=[B_ps[3]], inc=(c == 3))
            k.op("act", lambda e: e.activation(out=o_bT[sl][:].rearrange("p c t -> p (c t)"), in_=psb[3][:, 512:1024], func=AF.Identity),
                 r=[B_ps[3]], w=[B_obT[sl]])
            k.dma("pool", obT_d[:, :, i * 128:(i + 1) * 128].rearrange("c p t -> p c t"), o_bT[sl][:], s_obT[sl],
                  r=[B_obT[sl]], w=[B_obTd])

            yield

        def run_interleaved(gens, weights):
            live = [[g, w] for g, w in zip(gens, weights)]
            while live:
                for ent in list(live):
                    g, w = ent
                    for _ in range(w):
                        try:
                            next(g)
                        except StopIteration:
                            live.remove(ent)
                            break

        for step in range(nt3 + 1):
            if step < nt3:
                stage_idx(step)
            gens = []; wts = []
            if step < nt3:
                gens.append(stage_bis(step)); wts.append(1)
                gens.append(stage_gla(step)); wts.append(1)
            if step >= 1:
                gens.append(stage_attn(step - 1)); wts.append(3)
            run_interleaved(gens, wts)
        phase_end()
        vstack.close()
        if debug:
            s_dbg2 = k.new_sem("dbg2")
            k.dma("sp", dbg["oaT"][:, :, :], oaT_d[:, :, :], s_dbg2)
            k.dma("sp", dbg["obT"][:, :, :], obT_d[:, :, :], s_dbg2)
            k.final_wait("sp", [s_dbg2])
        if STOP == 20:
            return nc
        phase_begin()
        h2T = hT
        B_h2T = [Buf("h2T%d" % i) for i in range(NT)]
        w_ba = lsb("w_ba_s", [128, 4, 1024], BF16)
        w_bb = lsb("w_bb_s", [128, 4, 1024], BF16)
        w_o = lsb("w_o_s", [128, 8, 1024], BF16)
        g1B = lsb("g1B", [128, 1024], F32)
        b1B = lsb("b1B", [128, 1024], F32)
        B_w3b = Buf("w3b")
        s_w3b = k.new_sem("w3b")
        k.dma("pool", w_ba[:], w_ba_d.rearrange("(k p) j -> p k j", p=128), s_w3b, w=[B_w3b])
        k.dma("pool", w_bb[:], w_bb_d.rearrange("(k p) j -> p k j", p=128), s_w3b, w=[B_w3b])
        k.dma("pool", w_o[:], w_o_d.rearrange("(k p) j -> p k j", p=128), s_w3b, w=[B_w3b])
        s_w3c = k.new_sem("w3c")
        k.dma("sp", g1B[:], ln1_g_d[0:1, :].broadcast_to([128, 1024]), s_w3c, w=[B_w3b])
        k.dma("sp", b1B[:], ln1_b_d[0:1, :].broadcast_to([128, 1024]), s_w3c, w=[B_w3b])
        k.barrier()
        xs = [lsb("xs%d" % i, [128, D], F32) for i in range(2)]
        B_xs = [Buf("xs0"), Buf("xs1")]
        s_xs = [k.new_sem("xsb0"), k.new_sem("xsb1")]
        alloc_ln(4, nxn=2)
        gts = [lsb("gts%d" % i, [128, 2048], BF16) for i in range(2)]
        oaL = [lsb("oaL%d" % i, [128, 4, 128], BF16) for i in range(2)]
        obL = [lsb("obL%d" % i, [128, 4, 128], BF16) for i in range(2)]
        B_l3 = [Buf("l3_0"), Buf("l3_1")]
        s_l3 = [k.new_sem("l3_0"), k.new_sem("l3_1")]
        sga = lsb("sga", [128, 1024], BF16)
        sgb = lsb("sgb", [128, 1024], BF16)
        m1 = lsb("m1", [128, 1024], F32)
        m2 = lsb("m2", [128, 1024], F32)
        mrg = lsb("mrg", [128, 1024], BF16)
        mrgT = lsb("mrgT", [128, 8, 128], BF16)
        vv = lsb("vv", [128, 1024], F32)
        x1t = [lsb("x1t%d" % i, [128, 1024], F32) for i in range(3)]
        B_sg = Buf("sg"); B_m1 = Buf("m1"); B_m2 = Buf("m2"); B_mrg = Buf("mrg"); B_mrgT = Buf("mrgT"); B_vv = Buf("vv")
        B_x1t = [Buf("x1t%d" % i) for i in range(3)]
        s_x1t = [k.new_sem("x1t%d" % i) for i in range(3)]
        B_x1d = [Buf("x1d%d" % i) for i in range(NT)]
        mrgT2 = [mrgT, lsb("mrgT1", [128, 8, 128], BF16)]
        B_mrgT2 = [B_mrgT, Buf("mrgT1")]

        def p3b_front(i):
            sl = i % 2
            k.dma("sp", gts[sl][:], tok_d[i * 128:(i + 1) * 128, 2320:4368], s_l3[sl], r=[B_tok], w=[B_l3[sl]])
            k.dma("sp", oaL[sl][:], oaT_d[:, :, i * 128:(i + 1) * 128].rearrange("c p t -> p c t"), s_l3[sl], r=[B_oaTd], w=[B_l3[sl]])
            k.dma("sp", obL[sl][:], obT_d[:, :, i * 128:(i + 1) * 128].rearrange("c p t -> p c t"), s_l3[sl], r=[B_obTd], w=[B_l3[sl]])
            k.dma("sp", xs[sl][:], x_d[i * 128:(i + 1) * 128, :], s_xs[sl], w=[B_xs[sl]])
            for br, (src, wt) in enumerate(((oaL[sl], w_ba), (obL[sl], w_bb))):
                for half in range(2):
                    bank = br * 2 + half
                    for kc in range(4):
                        k.op("pe", lambda e, src=src, wt=wt, kc=kc, half=half, bank=bank: e.matmul(
                            ps[bank][:, :], lhsT=src[:, kc, :], rhs=wt[:, kc, half * 512:(half + 1) * 512],
                            start=(kc == 0), stop=(kc == 3)), r=[B_l3[sl], B_w3b], w=[B_ps[bank]], inc=(kc == 3))
            k.op("act", lambda e: e.activation(out=sga[:], in_=gts[sl][:, 0:1024], func=AF.Sigmoid), r=[B_l3[sl]], w=[B_sg])
            k.op("act", lambda e: e.activation(out=sgb[:], in_=gts[sl][:, 1024:2048], func=AF.Sigmoid), r=[B_l3[sl]], w=[B_sg])
            yield
            for half in range(2):
                cs = slice(half * 512, (half + 1) * 512)
                k.op("dve", lambda e, half=half, cs=cs: e.tensor_tensor(out=m1[:, cs], in0=ps[half][:, :], in1=sga[:, cs], op=ALU.mult),
                     r=[B_ps[half], B_sg], w=[B_m1])
                k.op("dve", lambda e, half=half, cs=cs: e.tensor_tensor(out=m2[:, cs], in0=ps[2 + half][:, :], in1=sgb[:, cs], op=ALU.mult),
                     r=[B_ps[2 + half], B_sg], w=[B_m2])
            yield
            k.op("pool", lambda e: e.tensor_tensor(out=mrg[:], in0=m1[:], in1=m2[:], op=ALU.add), r=[B_m1, B_m2], w=[B_mrg])
            for kc in range(8):
                k.op("pe", lambda e, kc=kc: e.transpose(out=psb[6][:, kc * 128:(kc + 1) * 128], in_=mrg[:, kc * 128:(kc + 1) * 128],
                                                        identity=ident[:]), r=[B_mrg, B_ident], w=[B_ps[6]], inc=(kc == 7))
            k.op("act", lambda e: e.activation(out=mrgT2[sl][:].rearrange("p c t -> p (c t)"), in_=psb[6][:, :], func=AF.Identity),
                 r=[B_ps[6]], w=[B_mrgT2[sl]])
            yield

        def p3b_back(i):
            sl = i % 2
            for half in range(2):
                bank = 4 + half
                for kc in range(8):
                    k.op("pe", lambda e, kc=kc, half=half, bank=bank: e.matmul(
                        ps[bank][:, :], lhsT=mrgT2[sl][:, kc, :], rhs=w_o[:, kc, half * 512:(half + 1) * 512],
                        start=(kc == 0), stop=(kc == 7)), r=[B_mrgT2[sl], B_w3b], w=[B_ps[bank]], inc=(kc == 7))
            for half in range(2):
                cs = slice(half * 512, (half + 1) * 512)
                k.op("dve", lambda e, half=half, cs=cs: e.tensor_tensor(out=vv[:, cs], in0=ps[4 + half][:, :], in1=gtB[:, cs], op=ALU.mult),
                     r=[B_ps[4 + half], B_gtB], w=[B_vv])
            k.op("dve", lambda e: e.scalar_tensor_tensor(out=vv[:], in0=xs[sl][:], scalar=ALPHA, in1=vv[:], op0=ALU.mult, op1=ALU.add),
                 r=[B_xs[sl], B_vv], w=[B_vv])
            yield
            s3 = i % 3
            mv = LN["mv"]; B_mv = LN["B_mv"]
            ln_stats(vv, B_vv, sl)
            k.op("act", lambda e: e.activation(out=x1t[s3][:], in_=vv[:], func=AF.Identity, scale=mv[sl][:, 2:3], bias=mv[sl][:, 3:4]),
                 r=[B_vv, B_mv[sl]], w=[B_x1t[s3]])
            yield
            k.op("pool", lambda e: e.tensor_tensor(out=x1t[s3][:], in0=x1t[s3][:], in1=g1B[:], op=ALU.mult), r=[B_x1t[s3], B_w3b], w=[B_x1t[s3]])
            k.op("pool", lambda e: e.tensor_tensor(out=x1t[s3][:], in0=x1t[s3][:], in1=b1B[:], op=ALU.add), r=[B_x1t[s3], B_w3b], w=[B_x1t[s3]])
            k.dma("pool", x1_d[i * 128:(i + 1) * 128, :], x1t[s3][:], s_x1t[s3], r=[B_x1t[s3]], w=[B_x1d[i]])
            yield

        def p3b_feat(i):
            s3 = i % 3
            yield from to_featT(i, x1t[s3], B_x1t[s3], h2T, B_h2T[i], i * 128, 24, 16, bank=7, slo=2)
            yield

        def run_il(gens):
            live = list(gens)
            while live:
                for g in list(live):
                    try:
                        next(g)
                    except StopIteration:
                        live.remove(g)

        for step in range(NT + 2):
            gens = []
            if step < NT:
                gens.append(p3b_front(step))
            if 1 <= step <= NT:
                gens.append(p3b_back(step - 1))
            if 2 <= step:
                gens.append(p3b_feat(step - 2))
            run_il(gens)
        s_h2 = k.new_sem("h2d")
        B_h2d = Buf("h2d")
        k.dma("sp", h2T_d.rearrange("k p t -> p k t"), h2T[:], s_h2, r=B_h2T, w=[B_h2d])
        if debug:
            dbg["x1"] = nc.dram_tensor("dbg_x1", [S, D], F32, kind="ExternalOutput").ap()
            k.barrier()
            k.dma("sp", dbg["x1"][:, :], x1_d[:, :], s_h2, r=B_x1d)
        phase_end()
        if STOP == 21:
            return nc
        phase_begin()
        HT = 2048
        h2h = lsb("h2h", [128, 8, HT], BF16)
        B_h2h = Buf("h2h")
        s_h2h = k.new_sem("h2h")
        acc = hT[:].rearrange("p a b -> p (a b)").bitcast(F32).rearrange("p (t c) -> p t c", c=1024)
        B_acc = [Buf("acc%d" % i) for i in range(HT // 128)]
        wr = lsb("wr", [128, 8, 36], BF16)
        brr = lsb("brr", [1, 36], BF16)
        brf = lsb("brf", [1, 36], F32)
        g2B = lsb("g2B", [128, 1024], F32)
        b2B = lsb("b2B", [128, 1024], F32)
        B_wr = Buf("wr")
        s_wr = k.new_sem("wr")
        k.dma("pool", wr[:], w_r_d.rearrange("(k p) j -> p k j", p=128), s_wr, w=[B_wr])
        s_wr2 = k.new_sem("wr2")
        k.dma("sp", brf[:], b_r_d[:, :], s_wr2, w=[B_wr])
        k.dma("sp", g2B[:], ln2_g_d[0:1, :].broadcast_to([128, 1024]), s_wr2, w=[B_wr])
        k.dma("sp", b2B[:], ln2_b_d[0:1, :].broadcast_to([128, 1024]), s_wr2, w=[B_wr])
        k.op("dve", lambda e: e.tensor_copy(out=brr[:], in_=brf[:]), r=[B_wr], w=[B_wr])
        k.barrier()
        gates = lsb("gates", [128, HT // 128, 32], F32)
        B_gates = Buf("gates")
        B_rt = Buf("rt")
        TH_ = HT // 128
        L3 = lsb("r_L3", [128, TH_, 36], F32)
        r_mx = lsb("r_mx", [128, TH_], F32); r_gw = lsb("r_gw", [128, TH_], F32)
        r_m1 = lsb("r_m1", [128, TH_], F32); r_m2 = lsb("r_m2", [128, TH_], F32)
        r_d = lsb("r_d", [128, TH_], F32); r_w1 = lsb("r_w1", [128, TH_], F32); r_w2 = lsb("r_w2", [128, TH_], F32)
        r_oh4 = lsb("r_oh4", [128, TH_, 4], F32); r_ex4 = lsb("r_ex4", [128, TH_, 4], F32)
        r_t32 = lsb("r_t32", [128, TH_, 4, 8], F32)
        r_eig = lsb("r_eig", [128, TH_, 8], F32); r_e2 = lsb("r_e2", [128, TH_, 8], F32)
        r_eq1 = lsb("r_eq1", [128, TH_, 8], F32); r_eq2 = lsb("r_eq2", [128, TH_, 8], F32)
        w1s = [lsb("w1s%d" % i, [128, 8, 256], BF16) for i in range(2)]
        w3s = [lsb("w3s%d" % i, [128, 8, 256], BF16) for i in range(2)]
        w2s = [lsb("w2s%d" % i, [128, 2, 1024], BF16) for i in range(2)]
        B_ws = [Buf("ws0"), Buf("ws1")]
        s_ws = [k.new_sem("ws0"), k.new_sem("ws1")]
        sgl = [lsb("sgl%d" % i, [128, 2, 512], BF16) for i in range(2)]
        hid = [lsb("hid%d" % i, [128, 2, 512], BF16) for i in range(2)]
        B_sgl = [Buf("sgl0"), Buf("sgl1")]
        B_hid = [Buf("hid0"), Buf("hid1")]
        NTL = 3
        st6b = lsb("st6b", [128, HT // 128, 2, 6], F32)
        mvb = lsb("mvb", [128, HT // 128, 4], F32)
        B_mvt = [Buf("mvt%d" % i) for i in range(HT // 128)]
        x1l = [lsb("x1l%d" % i, [128, 1024], F32) for i in range(NTL)]
        B_x1l = [Buf("x1l%d" % i) for i in range(NTL)]
        s_x1l = [k.new_sem("x1l%d" % i) for i in range(NTL)]
        fo = [lsb("fo%d" % i, [128, 1024], F32) for i in range(NTL)]
        B_fo = [Buf("fo%d" % i) for i in range(NTL)]
        s_fo = [k.new_sem("fo%d" % i) for i in range(NTL)]
        NTH = HT // 128
        k.dma("sp", h2h[:], h2T_d[:, :, 0:HT].rearrange("k p t -> p k t"), s_h2h, r=[B_h2d], w=[B_h2h])
        for hf in range(S // HT):
            for ti in range(NTH):
                bank = ti // 8
                co = (ti % 8) * 36
                k.op("pe", lambda e, bank=bank, co=co: e.matmul(ps[bank][:, co:co + 36], lhsT=ones1[:, :], rhs=brr[:, :], start=True, stop=False),
                     r=[B_wr], w=[B_ps[bank]], inc=False)
                for kc in range(8):
                    k.op("pe", lambda e, kc=kc, ti=ti, bank=bank, co=co: e.matmul(
                        ps[bank][:, co:co + 36], lhsT=h2h[:, kc, ti * 128:(ti + 1) * 128], rhs=wr[:, kc, :],
                        start=False, stop=(kc == 7)), r=[B_h2h, B_wr], w=[B_ps[bank]], inc=(kc == 7))
            T_ = NTH
            for bank in range(2):
                k.op("dve", lambda e, bank=bank: e.tensor_copy(out=L3[:, bank * 8:(bank + 1) * 8, :].rearrange("p t c -> p (t c)"),
                                                               in_=ps[bank][:, 0:288]), r=[B_ps[bank]], w=[B_rt])
            lg4 = L3[:, :, 0:4]
            le = L3[:, :, 4:36].rearrange("p t (g e) -> p t g e", g=4)
            k.op("dve", lambda e: e.tensor_reduce(out=r_mx[:], in_=lg4, axis=AX.X, op=ALU.max), r=[B_rt], w=[B_rt])
            k.op("dve", lambda e: e.tensor_tensor(out=r_oh4[:], in0=lg4, in1=r_mx[:].unsqueeze(2).broadcast_to([128, T_, 4]), op=ALU.is_equal),
                 r=[B_rt], w=[B_rt])
            k.op("dve", lambda e: e.tensor_tensor(out=r_ex4[:], in0=lg4, in1=r_mx[:].unsqueeze(2).broadcast_to([128, T_, 4]), op=ALU.subtract),
                 r=[B_rt], w=[B_rt])
            k.op("act", lambda e: e.activation(out=r_ex4[:], in_=r_ex4[:], func=AF.Exp), r=[B_rt], w=[B_rt])
            k.op("dve", lambda e: e.tensor_reduce(out=r_gw[:], in_=r_ex4[:], axis=AX.X, op=ALU.add), r=[B_rt], w=[B_rt])
            k.op("dve", lambda e: e.reciprocal(out=r_gw[:], in_=r_gw[:]), r=[B_rt], w=[B_rt])
            k.op("dve", lambda e: e.tensor_tensor(out=r_t32[:], in0=le, in1=r_oh4[:].unsqueeze(3).broadcast_to([128, T_, 4, 8]), op=ALU.mult),
                 r=[B_rt], w=[B_rt])
            k.op("dve", lambda e: e.tensor_reduce(out=r_eig[:], in_=r_t32[:].rearrange("p t g e -> p t e g"), axis=AX.X, op=ALU.add),
                 r=[B_rt], w=[B_rt])
            k.op("dve", lambda e: e.tensor_reduce(out=r_m1[:], in_=r_eig[:], axis=AX.X, op=ALU.max), r=[B_rt], w=[B_rt])
            k.op("dve", lambda e: e.tensor_tensor(out=r_eq1[:], in0=r_eig[:], in1=r_m1[:].unsqueeze(2).broadcast_to([128, T_, 8]), op=ALU.is_equal),
                 r=[B_rt], w=[B_rt])
            k.op("dve", lambda e: e.scalar_tensor_tensor(out=r_e2[:], in0=r_eq1[:], scalar=-1.0e30, in1=r_eig[:], op0=ALU.mult, op1=ALU.add),
                 r=[B_rt], w=[B_rt])
            k.op("dve", lambda e: e.tensor_reduce(out=r_m2[:], in_=r_e2[:], axis=AX.X, op=ALU.max), r=[B_rt], w=[B_rt])
            k.op("dve", lambda e: e.tensor_tensor(out=r_eq2[:], in0=r_eig[:], in1=r_m2[:].unsqueeze(2).broadcast_to([128, T_, 8]), op=ALU.is_equal),
                 r=[B_rt], w=[B_rt])
            k.op("dve", lambda e: e.tensor_tensor(out=r_d[:], in0=r_m2[:], in1=r_m1[:], op=ALU.subtract), r=[B_rt], w=[B_rt])
            k.op("act", lambda e: e.activation(out=r_d[:], in_=r_d[:], func=AF.Exp), r=[B_rt], w=[B_rt])
            k.op("dve", lambda e: e.tensor_scalar(out=r_w1[:], in0=r_d[:], scalar1=1.0, scalar2=None, op0=ALU.add), r=[B_rt], w=[B_rt])
            k.op("dve", lambda e: e.reciprocal(out=r_w1[:], in_=r_w1[:]), r=[B_rt], w=[B_rt])
            k.op("dve", lambda e: e.tensor_tensor(out=r_w1[:], in0=r_w1[:], in1=r_gw[:], op=ALU.mult), r=[B_rt], w=[B_rt])
            k.op("dve", lambda e: e.tensor_tensor(out=r_w2[:], in0=r_w1[:], in1=r_d[:], op=ALU.mult), r=[B_rt], w=[B_rt])
            k.op("dve", lambda e: e.tensor_tensor(out=r_eq1[:], in0=r_eq1[:], in1=r_w1[:].unsqueeze(2).broadcast_to([128, T_, 8]), op=ALU.mult),
                 r=[B_rt], w=[B_rt])
            k.op("dve", lambda e: e.tensor_tensor(out=r_eq2[:], in0=r_eq2[:], in1=r_w2[:].unsqueeze(2).broadcast_to([128, T_, 8]), op=ALU.mult),
                 r=[B_rt], w=[B_rt])
            k.op("dve", lambda e: e.tensor_tensor(out=r_eq1[:], in0=r_eq1[:], in1=r_eq2[:], op=ALU.add), r=[B_rt], w=[B_rt])
            k.op("dve", lambda e: e.tensor_tensor(out=gates[:].rearrange("p t (g e) -> p t g e", g=4),
                                                  in0=r_oh4[:].unsqueeze(3).broadcast_to([128, T_, 4, 8]),
                                                  in1=r_eq1[:].unsqueeze(2).broadcast_to([128, T_, 4, 8]), op=ALU.mult),
                 r=[B_rt], w=[B_gates])
            ycnt = [0]

            def emit_H(ex, tb, ws, hs):
                for which, wsrc in enumerate((w1s[ws], w3s[ws])):
                    for fcn in range(2):
                        bank = which * 2 + fcn
                        for kc in range(8):
                            k.op("pe", lambda e, wsrc=wsrc, fcn=fcn, kc=kc, bank=bank: e.matmul(
                                ps[bank][:, :], lhsT=wsrc[:, kc, fcn * 128:(fcn + 1) * 128], rhs=h2h[:, kc, tb * 512:(tb + 1) * 512],
                                start=(kc == 0), stop=(kc == 7)), r=[B_ws[ws], B_h2h], w=[B_ps[bank]], inc=(kc == 7))
                for fcn in range(2):
                    k.op("act", lambda e, fcn=fcn: e.activation(out=sgl[hs][:, fcn, :], in_=ps[fcn][:, :], func=AF.Silu),
                         r=[B_ps[fcn]], w=[B_sgl[hs]])
                for fcn in range(2):
                    k.op("dve", lambda e, fcn=fcn: e.tensor_tensor(out=hid[hs][:, fcn, :], in0=ps[2 + fcn][:, :], in1=sgl[hs][:, fcn, :],
                                                                   op=ALU.mult), r=[B_ps[2 + fcn], B_sgl[hs]], w=[B_hid[hs]])

            def emit_Y(ex, tb, ws, hs):
                for t4 in range(4):
                    ti = tb * 4 + t4
                    yb = 4 + 2 * (ycnt[0] % 2)
                    ycnt[0] += 1
                    for h2_ in range(2):
                        for fcn in range(2):
                            k.op("pe", lambda e, h2_=h2_, fcn=fcn, t4=t4, yb=yb: e.matmul(
                                ps[yb + h2_][:, :], lhsT=hid[hs][:, fcn, t4 * 128:(t4 + 1) * 128], rhs=w2s[ws][:, fcn, h2_ * 512:(h2_ + 1) * 512],
                                start=(fcn == 0), stop=(fcn == 1)), r=[B_hid[hs], B_ws[ws]], w=[B_ps[yb + h2_]], inc=(fcn == 1))
                    for h2_ in range(2):
                        cs = slice(h2_ * 512, (h2_ + 1) * 512)
                        if ex == 0:
                            k.op("dve", lambda e, h2_=h2_, cs=cs, ti=ti, yb=yb: e.tensor_scalar(
                                out=acc[:, ti, cs], in0=ps[yb + h2_][:, :], scalar1=gates[:, ti, ex:ex + 1], scalar2=None, op0=ALU.mult),
                                r=[B_ps[yb + h2_], B_gates], w=[B_acc[ti]])
                        else:
                            k.op("dve", lambda e, h2_=h2_, cs=cs, ti=ti, yb=yb: e.scalar_tensor_tensor(
                                out=acc[:, ti, cs], in0=ps[yb + h2_][:, :], scalar=gates[:, ti, ex:ex + 1], in1=acc[:, ti, cs],
                                op0=ALU.mult, op1=ALU.add), r=[B_ps[yb + h2_], B_gates, B_acc[ti]], w=[B_acc[ti]])

            prev = None
            hcnt = 0
            for ex in range(32):
                ws = ex % 2
                k.dma("pool", w1s[ws][:], w_eg_d[ex].rearrange("(k p) f -> p k f", p=128), s_ws[ws], w=[B_ws[ws]])
                k.dma("pool", w3s[ws][:], w_eu_d[ex].rearrange("(k p) f -> p k f", p=128), s_ws[ws], w=[B_ws[ws]])
                k.dma("pool", w2s[ws][:], w_ed_d[ex].rearrange("(k p) j -> p k j", p=128), s_ws[ws], w=[B_ws[ws]])
                for tb in range(HT // 512):
                    hs = hcnt % 2
                    hcnt += 1
                    emit_H(ex, tb, ws, hs)
                    if prev is not None:
                        emit_Y(*prev)
                    prev = (ex, tb, ws, hs)
            emit_Y(*prev)
            if hf + 1 < S // HT:
                k.dma("sp", h2h[:], h2T_d[:, :, (hf + 1) * HT:(hf + 2) * HT].rearrange("k p t -> p k t"), s_h2h, r=[B_h2d], w=[B_h2h])
            for ti in range(NTH):
                i = hf * NTH + ti
                sl = ti % NTL
                k.dma("sp", x1l[sl][:], x1_d[i * 128:(i + 1) * 128, :], s_x1l[sl], r=[B_x1d[i]], w=[B_x1l[sl]])
                k.op("pool", lambda e, ti=ti: e.tensor_tensor(out=acc[:, ti, :], in0=acc[:, ti, :], in1=gtB[:, 1024:2048], op=ALU.mult),
                     r=[B_acc[ti], B_gtB], w=[B_acc[ti]])
                k.op("dve", lambda e, ti=ti, sl=sl: e.scalar_tensor_tensor(out=acc[:, ti, :], in0=x1l[sl][:], scalar=ALPHA, in1=acc[:, ti, :],
                                                                           op0=ALU.mult, op1=ALU.add), r=[B_x1l[sl], B_acc[ti]], w=[B_acc[ti]])
                for hh in range(2):
                    k.op("dve", lambda e, ti=ti, hh=hh: e.bn_stats(out=st6b[:, ti, hh, :], in_=acc[:, ti, hh * 512:(hh + 1) * 512]),
                         r=[B_acc[ti]], w=[B_mvt[ti]])
                k.op("dve", lambda e, ti=ti: e.bn_aggr(out=mvb[:, ti, 0:2], in_=st6b[:, ti, :, :].rearrange("p a b -> p (a b)")),
                     r=[B_mvt[ti]], w=[B_mvt[ti]])
            k.op("act", lambda e: e.activation(out=mvb[:, :, 2], in_=mvb[:, :, 1], func=AF.Sqrt, bias=EPS), r=B_mvt, w=B_mvt)
            k.op("dve", lambda e: e.reciprocal(out=mvb[:, :, 2], in_=mvb[:, :, 2]), r=B_mvt, w=B_mvt)
            k.op("dve", lambda e: e.scalar_tensor_tensor(out=mvb[:, :, 3], in0=mvb[:, :, 0], scalar=-1.0, in1=mvb[:, :, 2],
                                                         op0=ALU.mult, op1=ALU.mult), r=B_mvt, w=B_mvt)
            for ti in range(NTH):
                i = hf * NTH + ti
                k.op("act", lambda e, ti=ti: e.activation(out=acc[:, ti, :], in_=acc[:, ti, :], func=AF.Identity, scale=mvb[:, ti, 2:3],
                                                          bias=mvb[:, ti, 3:4]), r=[B_acc[ti], B_mvt[ti]], w=[B_acc[ti]])
                k.op("dve", lambda e, ti=ti: e.tensor_tensor(out=acc[:, ti, :], in0=acc[:, ti, :], in1=g2B[:], op=ALU.mult),
                     r=[B_acc[ti], B_wr], w=[B_acc[ti]])
                k.op("pool", lambda e, ti=ti: e.tensor_tensor(out=acc[:, ti, :], in0=acc[:, ti, :], in1=b2B[:], op=ALU.add),
                     r=[B_acc[ti], B_wr], w=[B_acc[ti]])
                k.dma("pool", out_d[i * 128:(i + 1) * 128, :], acc[:, ti, :], s_fo[ti % NTL], r=[B_acc[ti]])
        k.barrier()
        k.final_wait("sp", s_fo)
        cur[0].close()
        cur[0] = None
    return nc


def t5_bucket_np(d):
    d = np.asarray(d)
    max_exact = 16
    d_f = np.maximum(d, 1).astype(np.float32)
    large = max_exact + (np.log(d_f / max_exact) / np.log(128 / max_exact) * (32 - max_exact)).astype(np.int32)
    large = np.minimum(large, 31)
    return np.where(d < max_exact, d, large)


def make_ohpad():
    oh = np.zeros((32, 384), np.float32)
    d = np.arange(256)
    b = t5_bucket_np(d)
    oh[b, 128 + d] = 8.0
    return oh


def prep_inputs(inputs, b):
    f = np.float32
    c = np.ascontiguousarray(inputs["c"][b].reshape(8, 128).T.astype(f))
    b_ada = inputs["b_ada"][0]
    m = {
        "x": np.ascontiguousarray(inputs["x"][b]),
        "c_pl": c,
        "w_ada": np.ascontiguousarray(inputs["w_ada"][0]),
        "b_ada_pl": np.ascontiguousarray(b_ada.reshape(48, 128).T),
        "b_ada_row": np.ascontiguousarray(b_ada.reshape(1, -1)),
        "w_in": np.ascontiguousarray(inputs["w_in"][0]),
        "rel_bias": np.ascontiguousarray(inputs["rel_bias"].astype(f)),
        "ohpad": make_ohpad(),
        "gla_wg": np.ascontiguousarray(inputs["gla_w_gate"][0]),
        "gla_bg": np.ascontiguousarray(inputs["gla_b_gate"][0].reshape(1, -1)),
        "gla_g": np.ascontiguousarray(inputs["gla_norm_g"][0].reshape(1, -1)),
        "w_ba": np.ascontiguousarray(inputs["w_branch_a"][0]),
        "w_bb": np.ascontiguousarray(inputs["w_branch_b"][0]),
        "w_o": np.ascontiguousarray(inputs["w_out"][0]),
        "ln1_g": np.ascontiguousarray(inputs["ln1_g"][0].reshape(1, -1)),
        "ln1_b": np.ascontiguousarray(inputs["ln1_b"][0].reshape(1, -1)),
        "ln2_g": np.ascontiguousarray(inputs["ln2_g"][0].reshape(1, -1)),
        "ln2_b": np.ascontiguousarray(inputs["ln2_b"][0].reshape(1, -1)),
        "w_r": np.ascontiguousarray(np.concatenate([inputs["w_router_group"][0], inputs["w_router_expert"][0]], axis=1)),
        "b_r": np.ascontiguousarray(np.concatenate([inputs["b_router_group"][0], inputs["b_router_expert"][0]]).reshape(1, -1)),
        "w_eg": np.ascontiguousarray(inputs["w_exp_gate"][0]),
        "w_eu": np.ascontiguousarray(inputs["w_exp_up"][0]),
        "w_ed": np.ascontiguousarray(inputs["w_exp_down"][0]),
    }
    return m


def kernel(**inputs):
    nc = build_nc()
    in_maps = [prep_inputs(inputs, b) for b in range(8)]
    res = run_bass_kernel_spmd(nc, in_maps, core_ids=list(range(8)))
    return np.stack([r["out"] for r in res.results], axis=0)
```

```python
import numpy as np
from contextlib import ExitStack
import concourse.bass as bass
import concourse.mybir as mybir
from concourse.bass_utils import run_bass_kernel_spmd

F32 = mybir.dt.float32
BF16 = mybir.dt.bfloat16
AF = mybir.ActivationFunctionType
ALU = mybir.AluOpType
AX = mybir.AxisListType

S = 4096
D = 1024
NT = S // 128
DP = 5696
O_AQ, O_AK, O_AV, O_IQ, O_IK, O_IW = 0, 512, 1024, 1536, 2048, 2080
O_GQ, O_GK, O_GV, O_GR, O_GLR, O_GA, O_GB = 2096, 2352, 2608, 3120, 3632, 3648, 4672
ALPHA = 2.0 ** 0.25
EPS = 1e-5
NEG = -1.0e30
TOKW = 4368
NFT = 14
NIT = 16
NT3 = NT
STOP = 0


class Sem:
    def __init__(self, h, is_dma=True):
        self.h = h
        self.val = 0
        self.is_dma = is_dma


class Buf:
    __slots__ = ("name", "last_w", "reads")

    def __init__(self, name):
        self.name = name
        self.last_w = None
        self.reads = {}


class Eng:
    def __init__(self, name, obj, sem):
        self.name = name
        self.obj = obj
        self.sem = sem
        self.seen = {}


class K:
    def __init__(self, nc, es):
        self.nc = nc
        self.es = es
        self.nsem = 0
        self.sems = []
        self.engs = {}
        for name, obj in (("pe", nc.tensor), ("act", nc.scalar), ("dve", nc.vector),
                          ("pool", nc.gpsimd), ("sp", nc.sync)):
            self.engs[name] = Eng(name, obj, self.new_sem("e_" + name))
            self.engs[name].sem.is_dma = False

    def new_sem(self, name):
        h = self.es.enter_context(self.nc.semaphore("s%d_%s" % (self.nsem, name)))
        self.nsem += 1
        s = Sem(h)
        self.sems.append(s)
        return s

    def _needs(self, E, r, w, skip_self=False):
        needs = {}

        def need(dep):
            if dep is None:
                return
            s, v = dep
            if skip_self and s is E.sem:
                return
            if s.is_dma:
                v = s.val
            if E.seen.get(s, 0) >= v:
                return
            if needs.get(s, 0) < v:
                needs[s] = v

        for b in r:
            need(b.last_w)
        for b in w:
            need(b.last_w)
            for s, v in b.reads.items():
                need((s, v))
        return needs

    def op(self, eng, fn, r=(), w=(), inc=True):
        E = self.engs[eng]
        needs = self._needs(E, r, w, skip_self=(eng == "pe"))
        items = list(needs.items())
        for s, v in items[:-1]:
            E.obj.wait_ge(s.h, v)
        ins = fn(E.obj)
        if items:
            s, v = items[-1]
            ins._wait_ge(s.h, v)
        for s, v in items:
            E.seen[s] = v
        if inc:
            E.sem.val += 1
            ins.then_inc(E.sem.h, 1)
            stamp = E.sem.val
        else:
            stamp = E.sem.val + 1
        for b in r:
            if b.reads.get(E.sem, 0) < stamp:
                b.reads[E.sem] = stamp
        for b in w:
            b.last_w = (E.sem, stamp)
            b.reads = {}
        return ins

    def dma(self, q, out, in_, sem, r=(), w=()):
        E = self.engs[q]
        needs = self._needs(E, r, w)
        for s, v in needs.items():
            E.obj.wait_ge(s.h, v)
            E.seen[s] = v
        ins = E.obj.dma_start(out=out, in_=in_)
        sem.val += 16
        ins.then_inc(sem.h, 16)
        for b in r:
            if b.reads.get(sem, 0) < sem.val:
                b.reads[sem] = sem.val
        for b in w:
            b.last_w = (sem, sem.val)
            b.reads = {}
        return ins

    def barrier(self):
        for E in self.engs.values():
            for s in self.sems:
                if s is E.sem:
                    continue
                if s.val > E.seen.get(s, 0):
                    E.obj.wait_ge(s.h, s.val)
                    E.seen[s] = s.val

    def final_wait(self, q, sems):
        E = self.engs[q]
        for s in sems:
            E.obj.wait_ge(s.h, s.val)


def build_nc(debug=None):
    nc = bass.Bass("TRN2", target_bir_lowering=False)
    dbg = {}

    def din(name, shape, dt=F32):
        return nc.dram_tensor(name, list(shape), dt, kind="ExternalInput").ap()

    x_d = din("x", [S, D])
    c_d = din("c_pl", [128, 8])
    w_ada_d = din("w_ada", [D, 6 * D])
    b_ada_pl_d = din("b_ada_pl", [128, 48])
    b_ada_row_d = din("b_ada_row", [1, 6 * D])
    w_in_d = din("w_in", [D, DP])
    rel_bias_d = din("rel_bias", [32, 8])
    ohpad_d = din("ohpad", [32, 384])
    gla_wg_d = din("gla_wg", [16, 256])
    gla_bg_d = din("gla_bg", [1, 256])
    gla_g_d = din("gla_g", [1, 512])
    out_d = nc.dram_tensor("out", [S, D], F32, kind="ExternalOutput").ap()
    w_ba_d = din("w_ba", [512, D])
    w_bb_d = din("w_bb", [512, D])
    w_o_d = din("w_o", [D, D])
    ln1_g_d = din("ln1_g", [1, D]); ln1_b_d = din("ln1_b", [1, D])
    ln2_g_d = din("ln2_g", [1, D]); ln2_b_d = din("ln2_b", [1, D])
    w_r_d = din("w_r", [D, 36]); b_r_d = din("b_r", [1, 36])
    w_eg_d = din("w_eg", [32, D, 256]); w_eu_d = din("w_eu", [32, D, 256]); w_ed_d = din("w_ed", [32, 256, D])
    x1_d = nc.dram_tensor("x1s", [S, D], F32, kind="Internal").ap()
    h2T_d = nc.dram_tensor("h2Ts", [8, 128, S], BF16, kind="Internal").ap()
    a2_d = nc.dram_tensor("a2", [8, 128, 384], F32, kind="Internal").ap()
    oaT_d = nc.dram_tensor("oaT", [4, 128, S], BF16, kind="Internal").ap()
    obT_d = nc.dram_tensor("obT", [4, 128, S], BF16, kind="Internal").ap()

    featT_d = nc.dram_tensor("featT", [NFT, 128, S], BF16, kind="Internal").ap()
    tok_d = nc.dram_tensor("tokm", [S, TOKW], BF16, kind="Internal").ap()

    if debug:
        dbg["hT"] = nc.dram_tensor("dbg_hT", [128, 8, S], BF16, kind="ExternalOutput").ap()
        dbg["featT"] = nc.dram_tensor("dbg_featT", [NFT, 128, S], BF16, kind="ExternalOutput").ap()
        dbg["tok"] = nc.dram_tensor("dbg_tok", [S, TOKW], BF16, kind="ExternalOutput").ap()
        dbg["ada"] = nc.dram_tensor("dbg_ada", [128, 32], F32, kind="ExternalOutput").ap()
        dbg["gt"] = nc.dram_tensor("dbg_gt", [128, 2048], F32, kind="ExternalOutput").ap()
        dbg["oaT"] = nc.dram_tensor("dbg_oaT", [4, 128, S], BF16, kind="ExternalOutput").ap()
        dbg["obT"] = nc.dram_tensor("dbg_obT", [4, 128, S], BF16, kind="ExternalOutput").ap()

    es = ExitStack()
    with es:
        k = K(nc, es)

        SB_LIMIT = 208 * 1024

        def _chk(name, t):
            m = nc.lookup_mloc(name)
            sz = 1
            for d_ in list(m.dims)[1:]:
                sz *= d_
            assert m.addr + sz <= SB_LIMIT, ("SBUF overflow", name, m.addr, sz)
            return t

        uid = [0]

        def sb(name, shape, dt):
            uid[0] += 1
            name = "%s_u%d" % (name, uid[0])
            return _chk(name, es.enter_context(nc.sbuf_tensor(name, list(shape), dt)))

        cur = [None]

        def lsb(name, shape, dt):
            uid[0] += 1
            name = "%s_u%d" % (name, uid[0])
            return _chk(name, cur[0].enter_context(nc.sbuf_tensor(name, list(shape), dt)))

        def phase_begin():
            cur[0] = ExitStack()

        def phase_end():
            k.barrier()
            cur[0].close()
            cur[0] = None

        def pst(name, shape, dt):
            return es.enter_context(nc.psum_tensor(name, list(shape), dt))

        ident = sb("ident", [128, 128], BF16)
        identf = sb("identf", [128, 128], F32)
        hT = sb("hT", [128, 8, S], BF16)
        adaP = sb("adaP", [128, 32], F32)
        gtB = sb("gtB", [128, 2048], F32)
        B_ident = Buf("ident")
        B_hT = [Buf("hT%d" % i) for i in range(NT)]
        B_adaP = Buf("adaP")
        B_gtB = Buf("gtB")

        ps = [pst("ps%d" % i, [128, 512], F32) for i in range(8)]
        B_ps = [Buf("ps%d" % i) for i in range(8)]

        k.op("pool", lambda e: e.memset(identf[:], 0.0), w=[B_ident])
        k.op("pool", lambda e: e.affine_select(out=identf[:], in_=identf[:], pattern=[[-1, 128]],
                                               compare_op=ALU.not_equal, fill=1.0, base=0,
                                               channel_multiplier=1), r=[B_ident], w=[B_ident])
        k.op("pool", lambda e: e.tensor_copy(out=ident[:], in_=identf[:]), r=[B_ident], w=[B_ident])

        ones1 = sb("ones1", [1, 128], BF16)
        phase_begin()
        c_sb = lsb("c_sb", [128, 8], F32)
        cond = lsb("cond", [128, 8], BF16)
        condB = lsb("condB", [128, 8, 128], BF16)
        bpl = lsb("bpl", [128, 48], F32)
        brow = lsb("brow", [1, 6 * D], F32)
        browb = lsb("browb", [1, 6 * D], BF16)
        wada = [lsb("wada%d" % i, [128, 8, 1024], BF16) for i in range(2)]
        B_c = Buf("c"); B_cond = Buf("cond"); B_bpl = Buf("bpl"); B_brow = Buf("brow")
        B_wada = [Buf("wada0"), Buf("wada1")]
        s_misc = k.new_sem("misc")
        s_wada = [k.new_sem("wada0"), k.new_sem("wada1")]
        k.dma("sp", c_sb[:], c_d[:, :], s_misc, w=[B_c])
        k.dma("sp", bpl[:], b_ada_pl_d[:, :], k.new_sem("misc2"), w=[B_bpl])
        k.dma("sp", brow[:], b_ada_row_d[:, :], k.new_sem("misc3"), w=[B_brow])
        k.op("act", lambda e: e.activation(out=cond[:], in_=c_sb[:], func=AF.Silu), r=[B_c], w=[B_cond])
        k.op("dve", lambda e: e.tensor_copy(out=condB[:], in_=cond[:].unsqueeze(2).broadcast_to([128, 8, 128])),
             r=[B_cond], w=[B_cond])
        k.op("dve", lambda e: e.tensor_copy(out=browb[:], in_=brow[:]), r=[B_brow], w=[B_brow])
        k.op("dve", lambda e: e.memset(ones1[:], 1.0), w=[B_brow])
        w_ada_v = w_ada_d.rearrange("(k p) j -> p k j", p=128)
        for pc in range(6):
            sl = pc % 2
            k.dma("pool", wada[sl][:], w_ada_v[:, :, pc * 1024:(pc + 1) * 1024], s_wada[sl], w=[B_wada[sl]])
            if pc in (2, 5):
                g = 0 if pc == 2 else 1
                for half in range(2):
                    bank = 2 + half
                    for kk in range(8):
                        k.op("pe", lambda e, kk=kk, half=half, bank=bank: e.matmul(
                            ps[bank][:], lhsT=condB[:, kk, :], rhs=wada[sl][:, kk, half * 512:(half + 1) * 512],
                            start=(kk == 0), stop=False), r=[B_cond, B_wada[sl]], w=[B_ps[bank]], inc=False)
                    k.op("pe", lambda e, half=half, bank=bank: e.matmul(
                        ps[bank][:], lhsT=ones1[:, :], rhs=browb[:, pc * 1024 + half * 512: pc * 1024 + (half + 1) * 512],
                        start=False, stop=True), r=[B_brow], w=[B_ps[bank]])
                    k.op("act", lambda e, half=half, bank=bank, g=g: e.activation(
                        out=gtB[:, g * 1024 + half * 512: g * 1024 + (half + 1) * 512], in_=ps[bank][:], func=AF.Identity),
                        r=[B_ps[bank]], w=[B_gtB])
            else:
                slot = {0: 0, 1: 1, 3: 2, 4: 3}[pc]
                for jc in range(8):
                    col = slot * 8 + jc
                    for kk in range(8):
                        k.op("pe", lambda e, kk=kk, jc=jc, col=col: e.matmul(
                            ps[0][:, col:col + 1], lhsT=wada[sl][:, kk, jc * 128:(jc + 1) * 128], rhs=cond[:, kk:kk + 1],
                            start=(kk == 0), stop=(kk == 7)), r=[B_cond, B_wada[sl]], w=[B_ps[0]], inc=(kk == 7))
                add1 = 1.0 if pc in (1, 4) else 0.0
                k.op("dve", lambda e, slot=slot, pc=pc, add1=add1: e.scalar_tensor_tensor(
                    out=adaP[:, slot * 8:(slot + 1) * 8], in0=ps[0][:, slot * 8:(slot + 1) * 8], scalar=add1,
                    in1=bpl[:, pc * 8:(pc + 1) * 8], op0=ALU.add, op1=ALU.add),
                    r=[B_ps[0], B_bpl], w=[B_adaP])

        phase_end()
        LN = {}

        def alloc_ln(n=2, nxn=None):
            nxn = n if nxn is None else nxn
            LN["xn"] = [lsb("xn%d" % i, [128, D], BF16) for i in range(nxn)]
            LN["st6"] = [lsb("st6_%d" % i, [128, 2, 6], F32) for i in range(n)]
            LN["mv"] = [lsb("mv%d" % i, [128, 4], F32) for i in range(n)]
            LN["B_xn"] = [Buf("xn%d" % i) for i in range(nxn)]
            LN["B_st"] = [Buf("st%d" % i) for i in range(n)]
            LN["B_mv"] = [Buf("mv%d" % i) for i in range(n)]

        phase_begin()
        NXS = 4
        xs = [lsb("xs%d" % i, [128, D], F32) for i in range(NXS)]
        B_xs = [Buf("xs%d" % i) for i in range(NXS)]
        s_xs = [k.new_sem("xs%d" % i) for i in range(NXS)]
        alloc_ln()
        psT = [ps[4][:].bitcast(BF16), ps[5][:].bitcast(BF16)]

        def ln_stats(xt, Bx, sl):
            st6, mv, B_st, B_mv = LN["st6"], LN["mv"], LN["B_st"], LN["B_mv"]
            for hh in range(2):
                k.op("dve", lambda e, hh=hh: e.bn_stats(out=st6[sl][:, hh, :], in_=xt[:, hh * 512:(hh + 1) * 512]),
                     r=[Bx], w=[B_st[sl]])
            k.op("dve", lambda e: e.bn_aggr(out=mv[sl][:, 0:2], in_=st6[sl][:].rearrange("p a b -> p (a b)")),
                 r=[B_st[sl]], w=[B_mv[sl]])
            k.op("act", lambda e: e.activation(out=mv[sl][:, 2:3], in_=mv[sl][:, 1:2], func=AF.Sqrt, bias=EPS),
                 r=[B_mv[sl]], w=[B_mv[sl]])
            k.op("dve", lambda e: e.reciprocal(out=mv[sl][:, 2:3], in_=mv[sl][:, 2:3]), r=[B_mv[sl]], w=[B_mv[sl]])
            k.op("dve", lambda e: e.scalar_tensor_tensor(out=mv[sl][:, 3:4], in0=mv[sl][:, 0:1], scalar=-1.0,
                                                         in1=mv[sl][:, 2:3], op0=ALU.mult, op1=ALU.mult),
                 r=[B_mv[sl]], w=[B_mv[sl]])

        def to_featT(i, xt, Bx, dst, Bdst, col0, scol, bcol, bank=None, slo=0):
            sl = i % 2 + slo
            xn, mv, B_xn, B_mv = LN["xn"], LN["mv"], LN["B_xn"], LN["B_mv"]
            ln_stats(xt, Bx, sl)
            yield
            k.op("act", lambda e: e.activation(out=xn[i % 2][:], in_=xt[:], func=AF.Identity,
                                               scale=mv[sl][:, 2:3], bias=mv[sl][:, 3:4]),
                 r=[Bx, B_mv[sl]], w=[B_xn[i % 2]])
            yield
            if bank is None:
                bank = 4 + i % 2
            psT_ = ps[bank][:].bitcast(BF16)
            for kk in range(8):
                k.op("pe", lambda e, kk=kk: e.transpose(out=psT_[:, kk * 128:(kk + 1) * 128],
                                                        in_=xn[i % 2][:, kk * 128:(kk + 1) * 128], identity=ident[:]),
                     r=[B_xn[i % 2], B_ident], w=[B_ps[bank]], inc=(kk == 7))
            for kk in range(8):
                if kk % 2 == 0:
                    k.op("act", lambda e, kk=kk: e.activation(
                        out=dst[:, kk, col0:col0 + 128], in_=psT_[:, kk * 128:(kk + 1) * 128], func=AF.Identity,
                        scale=adaP[:, scol + kk:scol + kk + 1], bias=adaP[:, bcol + kk:bcol + kk + 1]),
                        r=[B_ps[bank], B_adaP], w=[Bdst])
                else:
                    k.op("dve", lambda e, kk=kk: e.tensor_scalar(
                        out=dst[:, kk, col0:col0 + 128], in0=psT_[:, kk * 128:(kk + 1) * 128],
                        scalar1=adaP[:, scol + kk:scol + kk + 1], scalar2=adaP[:, bcol + kk:bcol + kk + 1],
                        op0=ALU.mult, op1=ALU.add), r=[B_ps[bank], B_adaP], w=[Bdst])

        def run_il(gens):
            live = list(gens)
            while live:
                for g in list(live):
                    try:
                        next(g)
                    except StopIteration:
                        live.remove(g)

        gs_ = []
        for i in range(NT + 2):
            if i < NT:
                sl = i % NXS
                k.dma("sp", xs[sl][:], x_d[i * 128:(i + 1) * 128, :], s_xs[sl], w=[B_xs[sl]])
                g = to_featT(i, xs[sl], B_xs[sl], hT, B_hT[i], i * 128, 8, 0)
                next(g)
                gs_.append(g)
            if 1 <= i <= NT:
                next(gs_[i - 1])
            if i >= 2:
                for _ in gs_[i - 2]:
                    pass

        if debug:
            s_dbg = k.new_sem("dbg")
            k.dma("sp", dbg["hT"][:, :, :], hT[:], s_dbg, r=B_hT)
            k.dma("sp", dbg["ada"][:, :], adaP[:], s_dbg, r=[B_adaP])
            k.dma("sp", dbg["gt"][:, :], gtB[:], s_dbg, r=[B_gtB])

        phase_end()
        fm = []
        for j in range(4): fm.append((j, O_AQ + j * 128, 128))
        for j in range(4): fm.append((4 + j, O_AK + j * 128, 128))
        fm.append((8, O_IK, 32))
        for j in range(2): fm.append((9 + j, O_GQ + j * 128, 128))
        for j in range(2): fm.append((11 + j, O_GK + j * 128, 128))
        fm.append((13, O_GLR, 16))
        tm = [(0, O_AV, 512), (512, O_IQ, 512), (1024, O_GV, 512), (1536, O_GR, 512), (2048, (O_GK, O_IW), 272),
              (2320, O_GA, 512), (2832, O_GA + 512, 512), (3344, O_GB, 512), (3856, O_GB + 512, 512)]
        w_in_v = w_in_d.rearrange("(k p) j -> p k j", p=128)
        vstack = ExitStack()
        v_aug = _chk("v_aug_p", vstack.enter_context(nc.sbuf_tensor("v_aug_p", [128, NT, 8, 65], BF16)))
        B_vt = [Buf("v_aug%d" % i) for i in range(NT)]
        k.op("pool", lambda e: e.memset(v_aug[:], 1.0), w=B_vt)
        phase_begin()
        wfm = [lsb("wfm%d" % i, [128, 8, 128], BF16) for i in range(2)]
        B_wfm = [Buf("wfm0"), Buf("wfm1")]
        s_wfm = [k.new_sem("wfm0"), k.new_sem("wfm1")]
        stg = [lsb("stg%d" % i, [128, S], BF16) for i in range(2)]
        B_stg = [Buf("stg0"), Buf("stg1")]
        s_stg = [k.new_sem("stg0"), k.new_sem("stg1")]
        B_featT = [Buf("featT%d" % i) for i in range(NFT)]
        ev = 0
        for n, (fi, c0, ncol) in enumerate(fm):
            sl = n % 2
            if ncol == 128:
                k.dma("pool", wfm[sl][:], w_in_v[:, :, c0:c0 + 128], s_wfm[sl], w=[B_wfm[sl]])
                M = 128
            elif ncol == 32:
                for rep in range(4):
                    k.dma("pool", wfm[sl][:, :, rep * 32:(rep + 1) * 32], w_in_v[:, :, c0:c0 + 32], s_wfm[sl], w=[B_wfm[sl]])
                M = 128
            else:
                k.dma("pool", wfm[sl][:, :, 0:16], w_in_v[:, :, c0:c0 + 16], s_wfm[sl], w=[B_wfm[sl]])
                M = 16
            for tb in range(8):
                bank = tb % 4
                for kk in range(8):
                    k.op("pe", lambda e, kk=kk, tb=tb, bank=bank, M=M: e.matmul(
                        ps[bank][0:M, :], lhsT=wfm[sl][:, kk, 0:M], rhs=hT[:, kk, tb * 512:(tb + 1) * 512],
                        start=(kk == 0), stop=(kk == 7)),
                        r=[B_wfm[sl]] + B_hT[tb * 4:(tb + 1) * 4], w=[B_ps[bank]], inc=(kk == 7))
                if ev % 2 == 0:
                    k.op("act", lambda e, tb=tb, bank=bank, M=M: e.activation(
                        out=stg[sl][0:M, tb * 512:(tb + 1) * 512], in_=ps[bank][0:M, :], func=AF.Identity),
                        r=[B_ps[bank]], w=[B_stg[sl]])
                else:
                    k.op("dve", lambda e, tb=tb, bank=bank, M=M: e.tensor_copy(
                        out=stg[sl][0:M, tb * 512:(tb + 1) * 512], in_=ps[bank][0:M, :]),
                        r=[B_ps[bank]], w=[B_stg[sl]])
                ev += 1
            k.dma("sp", featT_d[fi, 0:M, :], stg[sl][0:M, :], s_stg[sl], r=[B_stg[sl]], w=[B_featT[fi]])

        tm2 = [[(O_AV, 512)],
               [(O_IQ, 512), (O_GV, 512)],
               [(O_GR, 512), ((O_GK, O_IW), 272)],
               [(O_GA, 512), (O_GA + 512, 512)],
               [(O_GB, 512), (O_GB + 512, 512)]]
        tm2_t0 = [None, 512, 1536, 2320, 3344]
        wtm = [lsb("wtm%d" % i, [128, 8, 1024], BF16) for i in range(2)]
        B_wtm = [Buf("wtm0"), Buf("wtm1")]
        s_wtm = [k.new_sem("wtm0"), k.new_sem("wtm1")]
        NTS = 8
        tstg = [lsb("tstg%d" % i, [128, 1024], BF16) for i in range(NTS)]
        B_tstgA = [Buf("tstgA%d" % i) for i in range(NTS)]
        B_tstgB = [Buf("tstgB%d" % i) for i in range(NTS)]
        s_tstg = [k.new_sem("tstg%d" % i) for i in range(NTS)]
        B_tok = Buf("tok")
        cnt = 0

        def load_wtm(n):
            for pi_, (c0_, ncol_) in enumerate(tm2[n]):
                o_ = pi_ * 512
                if isinstance(c0_, tuple):
                    k.dma("pool", wtm[n % 2][:, :, o_:o_ + 256], w_in_v[:, :, c0_[0]:c0_[0] + 256], s_wtm[n % 2], w=[B_wtm[n % 2]])
                    k.dma("pool", wtm[n % 2][:, :, o_ + 256:o_ + 272], w_in_v[:, :, c0_[1]:c0_[1] + 16], s_wtm[n % 2], w=[B_wtm[n % 2]])
                else:
                    k.dma("pool", wtm[n % 2][:, :, o_:o_ + ncol_], w_in_v[:, :, c0_:c0_ + ncol_], s_wtm[n % 2], w=[B_wtm[n % 2]])

        load_wtm(0)
        for n, parts in enumerate(tm2):
            sl = n % 2
            if n + 1 < len(tm2):
                load_wtm(n + 1)
            for i in range(NT):
                b0 = 2 * (i % 4)
                for pi_, (c0, ncol) in enumerate(parts):
                    bank = b0 + pi_
                    o_ = pi_ * 512
                    for kk in range(8):
                        k.op("pe", lambda e, kk=kk, i=i, bank=bank, o_=o_, ncol=ncol: e.matmul(
                            ps[bank][:, 0:ncol], lhsT=hT[:, kk, i * 128:(i + 1) * 128], rhs=wtm[sl][:, kk, o_:o_ + ncol],
                            start=(kk == 0), stop=(kk == 7)),
                            r=[B_wtm[sl], B_hT[i]], w=[B_ps[bank]], inc=(kk == 7))
                if n == 0:
                    v_out = v_aug[:, i, :, 0:64]
                    v_in = ps[b0][:, 0:512].rearrange("p (h d) -> p h d", h=8)
                    if cnt % 2 == 0:
                        k.op("act", lambda e, v_out=v_out, v_in=v_in: e.activation(out=v_out, in_=v_in, func=AF.Identity),
                             r=[B_ps[b0]], w=[B_vt[i]])
                    else:
                        k.op("dve", lambda e, v_out=v_out, v_in=v_in: e.tensor_copy(out=v_out, in_=v_in),
                             r=[B_ps[b0]], w=[B_vt[i]])
                    cnt += 1
                    continue
                ts = cnt % NTS
                nc1 = parts[1][1]
                k.op("act", lambda e, b0=b0, ts=ts: e.activation(out=tstg[ts][:, 0:512], in_=ps[b0][:, 0:512], func=AF.Identity),
                     r=[B_ps[b0]], w=[B_tstgA[ts]])
                k.op("dve", lambda e, b0=b0, ts=ts, nc1=nc1: e.tensor_copy(out=tstg[ts][:, 512:512 + nc1], in_=ps[b0 + 1][:, 0:nc1]),
                     r=[B_ps[b0 + 1]], w=[B_tstgB[ts]])
                t0 = tm2_t0[n]
                k.dma("sp" if cnt % 2 == 0 else "pool", tok_d[i * 128:(i + 1) * 128, t0:t0 + 512 + nc1], tstg[ts][:, 0:512 + nc1], s_tstg[ts],
                      r=[B_tstgA[ts], B_tstgB[ts]], w=[B_tok])
                cnt += 1

        phase_end()
        phase_begin()
        BIG = hT
        kT = BIG[:, 0:4, :]
        ikT = BIG[:, 4, :]
        A = BIG[:, 5:7, :].rearrange("p a b -> p (a b)").bitcast(F32)
        msk = BIG[:, 7, :]
        B_kc = Buf("kcache"); B_A = Buf("A"); B_msk = Buf("msk")
        B_v = Buf("v_aug")
        maskT0 = lsb("maskT", [128, NT, 128], BF16)
        NBTh = lsb("NBTh", [128, 2, 8, 128], BF16)
        NBTl = lsb("NBTl", [128, 2, 8, 128], BF16)
        cB = lsb("cB", [128, 8], F32)
        c8 = lsb("c8", [128, 8], F32)
        B_NBT = Buf("NBT")
        tri_in = lsb("tri_in", [128, 128], BF16)
        tri_st = lsb("tri_st", [128, 128], BF16)
        trif = lsb("trif", [128, 128], F32)
        B_tri = Buf("tri")
        wg = lsb("wg", [16, 256], BF16)
        bg = lsb("bg", [1, 256], BF16)
        wgf = lsb("wgf", [16, 256], F32)
        bgf = lsb("bgf", [1, 256], F32)
        gB = lsb("gB", [128, 512], F32)
        B_gc = Buf("glaconst")
        pw = lsb("pw", [128, NIT + 1], F32)
        B_pw = Buf("pw")
        s_c = k.new_sem("p3c")
        s_c2 = k.new_sem("p3c2")
        psb = [p[:].bitcast(BF16) for p in ps]

        s_cp = k.new_sem("p3cp")
        for j in range(4):
            k.dma(("sp", "act", "pool", "act")[j], kT[:, j, :], featT_d[4 + j, :, :], s_cp if j == 2 else s_c, r=[B_featT[4 + j]], w=[B_kc])
        k.dma("sp", ikT, featT_d[8, :, :], s_c, r=[B_featT[8]], w=[B_kc])
        k.op("pool", lambda e: e.memset(trif[:], 1.0), w=[B_tri])
        k.op("pool", lambda e: e.affine_select(out=trif[:], in_=trif[:], pattern=[[1, 128]], compare_op=ALU.is_ge,
                                               fill=0.0, base=0, channel_multiplier=-1), r=[B_tri], w=[B_tri])
        k.op("pool", lambda e: e.tensor_copy(out=tri_in[:], in_=trif[:]), r=[B_tri], w=[B_tri])
        k.op("pool", lambda e: e.memset(trif[:], 1.0), r=[B_tri], w=[B_tri])
        k.op("pool", lambda e: e.affine_select(out=trif[:], in_=trif[:], pattern=[[-1, 128]], compare_op=ALU.is_gt,
                                               fill=0.0, base=0, channel_multiplier=1), r=[B_tri], w=[B_tri])
        k.op("pool", lambda e: e.tensor_copy(out=tri_st[:], in_=trif[:]), r=[B_tri], w=[B_tri])
        for t in range(NIT + 1):
            k.op("pool", lambda e, t=t: e.memset(pw[:, t:t + 1], 2.0 ** -(t + 1)), w=[B_pw])
        k.dma("sp", wgf[:], gla_wg_d[:, :], s_c, w=[B_gc])
        k.dma("sp", bgf[:], gla_bg_d[:, :], s_c, w=[B_gc])
        k.dma("sp", gB[:], gla_g_d[0:1, :].broadcast_to([128, 512]), s_c, w=[B_gc])
        k.op("dve", lambda e: e.tensor_copy(out=wg[:], in_=wgf[:]), r=[B_gc], w=[B_gc])
        k.op("dve", lambda e: e.tensor_copy(out=bg[:], in_=bgf[:]), r=[B_gc], w=[B_gc])
        ph2 = ExitStack()
        rb = ph2.enter_context(nc.sbuf_tensor("rb", [32, 8], F32))
        rbB = ph2.enter_context(nc.sbuf_tensor("rbB", [32, 8, 128], F32))
        oh = ph2.enter_context(nc.sbuf_tensor("oh", [32, 384], F32))
        Rrep = ph2.enter_context(nc.sbuf_tensor("Rrep", [128, 8, 384], F32))
        NBT = ph2.enter_context(nc.sbuf_tensor("NBTf", [128, 2, 8, 128], F32))
        B_rb = Buf("rb"); B_Rrep = Buf("Rrep"); B_A2 = Buf("A2")
        k.dma("sp", rb[:], rel_bias_d[:, :], s_c2, w=[B_rb])
        k.dma("sp", oh[:], ohpad_d[:, :], s_c2, w=[B_rb])
        k.op("dve", lambda e: e.tensor_copy(out=rbB[:], in_=rb[:].unsqueeze(2).broadcast_to([32, 8, 128])), r=[B_rb], w=[B_rb])
        for h in range(8):
            bank = h % 2
            k.op("pe", lambda e, h=h, bank=bank: e.matmul(ps[bank][:, 0:384], lhsT=rbB[:, h, :], rhs=oh[:, :], start=True, stop=True),
                 r=[B_rb], w=[B_ps[bank]])
            k.op("act", lambda e, h=h, bank=bank: e.activation(out=Rrep[:, h, :], in_=ps[bank][:, 0:384], func=AF.Identity),
                 r=[B_ps[bank]], w=[B_Rrep])
        k.dma("sp", a2_d.rearrange("h p m -> p h m"), Rrep[:], s_c2, r=[B_Rrep], w=[B_A2])
        for t in range(2):
            src = bass.AP(tensor=a2_d.tensor, offset=128 + 128 * t, ap=[[383, 128], [128 * 384, 8], [1, 128]])
            k.dma("sp", NBT[:, t, :, :], src, s_c2, r=[B_A2], w=[B_NBT])
        k.op("dve", lambda e: e.tensor_copy(out=c8[:], in_=Rrep[:, :, 383]), r=[B_Rrep], w=[B_NBT])
        k.op("dve", lambda e: e.tensor_scalar(out=cB[:], in0=c8[:], scalar1=0.125, scalar2=None, op0=ALU.mult), r=[B_NBT], w=[B_NBT])
        for t in range(2):
            for h in range(8):
                k.op("dve", lambda e, t=t, h=h: e.tensor_scalar(out=NBT[:, t, h, :], in0=NBT[:, t, h, :], scalar1=c8[:, h:h + 1],
                                                                scalar2=None, op0=ALU.subtract), r=[B_NBT], w=[B_NBT])
        k.op("dve", lambda e: e.tensor_copy(out=NBTh[:].rearrange("p a b c -> p (a b c)"), in_=NBT[:].rearrange("p a b c -> p (a b c)")),
             r=[B_NBT], w=[B_NBT])
        k.op("dve", lambda e: e.tensor_tensor(out=NBTl[:].rearrange("p a b c -> p (a b c)"), in0=NBT[:].rearrange("p a b c -> p (a b c)"),
                                              in1=NBTh[:].rearrange("p a b c -> p (a b c)"), op=ALU.subtract), r=[B_NBT], w=[B_NBT])
        k.barrier()
        ph2.close()

        nt3 = NT3
        maskT = [maskT0, lsb("maskT1", [128, NT, 128], BF16)]
        B_maskT = [Buf("maskT0"), Buf("maskT1")]
        qT_t = [lsb("qT_t%d" % i, [128, 4, 128], BF16) for i in range(2)]
        gT_t = [lsb("gT_t%d" % i, [128, 4, 128], BF16) for i in range(2)]
        glr_t = [lsb("glr_t%d" % i, [16, 128], BF16) for i in range(2)]
        tok_t = [lsb("tok_t%d" % i, [128, 1808], BF16) for i in range(2)]
        B_ld = [Buf("ld0"), Buf("ld1")]
        s_ld = [k.new_sem("ld0"), k.new_sem("ld1")]
        wabs = lsb("wabs", [128, 16], F32)
        sgn = lsb("sgn", [128, 16], F32)
        sgnD = lsb("sgnD", [128, 16, 128], BF16)
        iqs = lsb("iqs", [128, 512], BF16)
        iqT = lsb("iqT", [128, 4, 128], BF16)
        B_iq = Buf("iq"); B_iqT = Buf("iqT"); B_sgnD = Buf("sgnD")
        NRH = 8
        Rh = [lsb("Rh%d" % i, [128, 512], BF16) for i in range(NRH)]
        B_Rh = [Buf("Rh%d" % i) for i in range(NRH)]
        bis = lsb("bis", [128, 8], F32)
        nmid = lsb("nmid", [128, NIT + 1], F32)
        whs = lsb("whs", [128, NIT + 1], F32)
        whs2 = lsb("whs2", [128, NIT + 1], F32)
        mid = lsb("mid", [128, NIT + 1], F32)
        B_bis = Buf("bis"); B_bisA = Buf("bisA"); B_bisD = Buf("bisD"); B_msk2 = Buf("msk2")
        expS = [lsb("expS%d" % i, [128, 512], BF16) for i in range(2)]
        B_expS = [Buf("expS0"), Buf("expS1")]
        PT = [lsb("PT%d" % i, [128, 512], BF16) for i in range(2)]
        B_PT = [Buf("PT0"), Buf("PT1")]
        rec = lsb("rec", [128, 8], F32)
        o_a = lsb("o_a", [128, 8, 64], BF16)
        o_aT = [lsb("o_aT%d" % i, [128, 4, 128], BF16) for i in range(2)]
        B_oa = Buf("o_a"); B_oaT = [Buf("o_aT0"), Buf("o_aT1")]
        s_oaT = [k.new_sem("oaT0"), k.new_sem("oaT1")]
        B_oaTd = Buf("oaTd"); B_obTd = Buf("obTd")
        ge_ = lsb("g_e", [128, 256], F32)
        lgf = lsb("g_lgf", [128, 256], F32)
        lgh = lsb("g_lgh", [128, 256], BF16)
        lgl = lsb("g_lgl", [128, 256], BF16)
        ET = lsb("g_ET", [128, 3, 256], F32)
        E2 = lsb("g_E2", [128, 256], F32)
        qin = lsb("g_qin", [128, 2, 128], BF16)
        kst = lsb("g_kst", [128, 2, 128], BF16)
        qrl = lsb("g_qrl", [128, 2, 128], BF16)
        kstk = lsb("g_kstk", [128, 256], BF16)
        attT = lsb("g_attT", [128, 4, 128], BF16)
        Sst = lsb("g_S", [128, 2, 128], F32)
        Sb = lsb("g_Sb", [128, 2, 128], BF16)
        gst = lsb("g_st", [128, 4, 6], F32)
        gmv = lsb("g_mv", [128, 4, 2], F32)
        grs = lsb("g_rs", [128, 8], F32)
        on = lsb("g_on", [128, 512], F32)
        sg = lsb("g_sg", [128, 512], F32)
        ob = lsb("g_ob", [128, 512], BF16)
        o_bT = [lsb("o_bT%d" % i, [128, 4, 128], BF16) for i in range(2)]
        B_g1 = Buf("g1"); B_lg = Buf("lg"); B_ET = Buf("ET"); B_gq = Buf("gq"); B_att = Buf("att")
        B_S = Buf("S"); B_Sb = Buf("Sb"); B_gs = Buf("gs"); B_on = Buf("on"); B_ob = Buf("ob")
        B_obT = [Buf("o_bT0"), Buf("o_bT1")]
        s_obT = [k.new_sem("obT0"), k.new_sem("obT1")]
        k.op("dve", lambda e: e.memset(Sst[:], 0.0), w=[B_S])
        k.op("dve", lambda e: e.memset(Sb[:], 0.0), w=[B_Sb])
        WC = (16.0 ** -0.5) * (32.0 ** -0.5)
        evc = [0]

        def stage_idx(i):
            sl = i % 2
            n = (i + 1) * 128
            k.dma("sp", qT_t[sl][:], featT_d[0:4, :, i * 128:(i + 1) * 128].rearrange("c p t -> p c t"), s_ld[sl],
                  r=B_featT[0:4], w=[B_ld[sl]])
            k.dma("sp", gT_t[sl][:], featT_d[9:13, :, i * 128:(i + 1) * 128].rearrange("c p t -> p c t"), s_ld[sl],
                  r=B_featT[9:13], w=[B_ld[sl]])
            k.dma("sp", glr_t[sl][:], featT_d[13, 0:16, i * 128:(i + 1) * 128], s_ld[sl], r=[B_featT[13]], w=[B_ld[sl]])
            k.dma("sp", tok_t[sl][:], tok_d[i * 128:(i + 1) * 128, 512:2320], s_ld[sl], r=[B_tok], w=[B_ld[sl]])
            tk = tok_t[sl]
            iq_v = tk[:, 0:512]; iw_v = tk[:, 1792:1808]
            k.op("act", lambda e: e.activation(out=wabs[:], in_=iw_v, func=AF.Abs, scale=WC), r=[B_ld[sl]], w=[B_iq])
            k.op("dve", lambda e: e.tensor_scalar(out=sgn[:], in0=iw_v, scalar1=0.0, scalar2=2.0, op0=ALU.is_gt, op1=ALU.mult),
                 r=[B_ld[sl]], w=[B_iq])
            k.op("dve", lambda e: e.tensor_scalar(out=sgn[:], in0=sgn[:], scalar1=-1.0, scalar2=None, op0=ALU.add), r=[B_iq], w=[B_iq])
            k.op("dve", lambda e: e.tensor_tensor(out=sgnD[:], in0=ident[:].unsqueeze(1).broadcast_to([128, 16, 128]),
                                                  in1=sgn[:].unsqueeze(2).broadcast_to([128, 16, 128]), op=ALU.mult),
                 r=[B_iq, B_ident], w=[B_sgnD])
            k.op("dve", lambda e: e.tensor_tensor(out=iqs[:].rearrange("p (h d) -> p h d", h=16),
                                                  in0=iq_v.rearrange("p (h d) -> p h d", h=16),
                                                  in1=wabs[:].unsqueeze(2).broadcast_to([128, 16, 32]), op=ALU.mult),
                 r=[B_ld[sl], B_iq], w=[B_iq])
            for c in range(4):
                k.op("pe", lambda e, c=c: e.transpose(out=psb[3][:, c * 128:(c + 1) * 128], in_=iqs[:, c * 128:(c + 1) * 128],
                                                      identity=ident[:]), r=[B_iq, B_ident], w=[B_ps[3]], inc=(c == 3))
            k.op("act", lambda e: e.activation(out=iqT[:].rearrange("p c t -> p (c t)"), in_=psb[3][:, 0:512], func=AF.Identity),
                 r=[B_ps[3]], w=[B_iqT])
            nkb = (i + 4) // 4
            for kb in range(nkb):
                k0 = kb * 512
                nk = min(512, n - k0)
                sbank = 4 + kb % 2
                pend = []
                for c in range(4):
                    for hh in range(4):
                        pb = 32 * hh
                        k.op("pe", lambda e, c=c, pb=pb, hh=hh: e.matmul(
                            ps[hh][:, 0:nk], lhsT=iqT[pb:pb + 32, c, :], rhs=ikT[pb:pb + 32, k0:k0 + nk], start=True, stop=True,
                            tile_position=(pb, 0)), r=[B_iqT, B_kc], w=[B_ps[hh]])
                    for (ph_, prs) in pend:
                        k.op("pe", lambda e, ph_=ph_, prs=prs: e.matmul(ps[sbank][:, 0:nk], lhsT=sgnD[:, ph_, :], rhs=Rh[prs][:, 0:nk],
                                                                        start=(ph_ == 0), stop=False),
                             r=[B_sgnD, B_Rh[prs]], w=[B_ps[sbank]], inc=False)
                    pend = []
                    for hh in range(4):
                        h = c * 4 + hh
                        rs = (c % 2) * 4 + hh
                        if hh < 3:
                            k.op("act", lambda e, hh=hh, rs=rs: e.activation(out=Rh[rs][:, 0:nk], in_=ps[hh][:, 0:nk], func=AF.Relu),
                                 r=[B_ps[hh]], w=[B_Rh[rs]])
                        else:
                            k.op("dve", lambda e, hh=hh, rs=rs: e.tensor_scalar(out=Rh[rs][:, 0:nk], in0=ps[hh][:, 0:nk], scalar1=0.0,
                                                                                scalar2=None, op0=ALU.max), r=[B_ps[hh]], w=[B_Rh[rs]])
                        pend.append((h, rs))
                for (ph_, prs) in pend:
                    k.op("pe", lambda e, ph_=ph_, prs=prs: e.matmul(ps[sbank][:, 0:nk], lhsT=sgnD[:, ph_, :], rhs=Rh[prs][:, 0:nk],
                                                                    start=False, stop=(ph_ == 15)),
                         r=[B_sgnD, B_Rh[prs]], w=[B_ps[sbank]], inc=(ph_ == 15))
                if kb % 2 == 0:
                    k.op("act", lambda e, sbank=sbank: e.activation(out=A[:, k0:k0 + nk], in_=ps[sbank][:, 0:nk], func=AF.Identity),
                         r=[B_ps[sbank]], w=[B_A])
                else:
                    k.op("dve", lambda e, sbank=sbank: e.tensor_copy(out=A[:, k0:k0 + nk], in_=ps[sbank][:, 0:nk]),
                         r=[B_ps[sbank]], w=[B_A])

        def stage_bis(i):
            n = (i + 1) * 128
            mp = i % 2
            k.op("pool", lambda e: e.affine_select(out=A[:, i * 128:n], in_=A[:, i * 128:n], pattern=[[-1, 128]],
                                                   compare_op=ALU.is_ge, fill=NEG, base=0, channel_multiplier=1),
                 r=[B_A], w=[B_A])
            if i < 2:
                k.op("dve", lambda e: e.memset(bis[:, 0:1], -1.0e29), w=[B_bis])
            else:
                k.op("dve", lambda e: e.tensor_reduce(out=bis[:, 5:6], in_=A[:, 0:i * 128], axis=AX.X, op=ALU.max,
                                                      apply_absolute_value=True), r=[B_A], w=[B_bis])
                k.op("dve", lambda e: e.tensor_reduce(out=bis[:, 4:5], in_=A[:, 0:n], axis=AX.X, op=ALU.max),
                     r=[B_A], w=[B_bis])
                yield
                k.op("dve", lambda e: e.scalar_tensor_tensor(out=bis[:, 4:5], in0=bis[:, 4:5], scalar=2.0, in1=bis[:, 5:6],
                                                             op0=ALU.add, op1=ALU.add), r=[B_bis], w=[B_bis])
                k.op("dve", lambda e: e.tensor_scalar(out=whs[:], in0=pw[:], scalar1=bis[:, 4:5], scalar2=None, op0=ALU.mult),
                     r=[B_bis, B_pw], w=[B_bis])
                k.op("dve", lambda e: e.tensor_scalar(out=whs2[:], in0=whs[:], scalar1=2.0, scalar2=None, op0=ALU.mult),
                     r=[B_bis], w=[B_bis])
                k.op("dve", lambda e: e.scalar_tensor_tensor(out=nmid[:, 0:1], in0=bis[:, 5:6], scalar=1.0, in1=whs[:, 0:1],
                                                             op0=ALU.add, op1=ALU.subtract), r=[B_bis], w=[B_bis])
                k.op("dve", lambda e: e.tensor_scalar(out=mid[:, 0:1], in0=nmid[:, 0:1], scalar1=-1.0, scalar2=None, op0=ALU.mult),
                     r=[B_bis], w=[B_bis])
                for t in range(NIT):
                    k.op("dve", lambda e, t=t: e.tensor_scalar(out=msk[:, 0:n], in0=A[:, 0:n], scalar1=mid[:, t:t + 1], scalar2=None,
                                                               op0=ALU.is_ge, op1=ALU.add, accum_out=bis[:, 3:4]),
                         r=[B_A, B_bis], w=[B_msk, B_bis])
                    k.op("dve", lambda e, t=t: e.tensor_scalar(out=bis[:, 7:8], in0=bis[:, 3:4], scalar1=255.5, scalar2=whs2[:, t + 1:t + 2],
                                                               op0=ALU.is_ge, op1=ALU.mult), r=[B_bis], w=[B_bis])
                    k.op("dve", lambda e, t=t: e.scalar_tensor_tensor(out=mid[:, t + 1:t + 2], in0=bis[:, 7:8], scalar=mid[:, t:t + 1],
                                                                      in1=whs[:, t + 1:t + 2], op0=ALU.add, op1=ALU.subtract),
                         r=[B_bis], w=[B_bis])
                    yield
                k.op("dve", lambda e: e.tensor_tensor(out=bis[:, 0:1], in0=mid[:, NIT:NIT + 1], in1=whs[:, NIT:NIT + 1], op=ALU.subtract),
                     r=[B_bis], w=[B_bis])
            k.op("dve", lambda e: e.tensor_scalar(out=msk[:, 0:n], in0=A[:, 0:n], scalar1=bis[:, 0:1], scalar2=None, op0=ALU.is_ge),
                 r=[B_A, B_bis], w=[B_msk, B_msk2])
            yield
            for g0 in range(0, i + 1, 8):
                g1 = min(g0 + 8, i + 1)
                for j in range(g0, g1):
                    k.op("pe", lambda e, j=j, g0=g0: e.transpose(out=psb[3][:, (j - g0) * 128:(j - g0 + 1) * 128],
                                                                 in_=msk[:, j * 128:(j + 1) * 128], identity=ident[:]),
                         r=[B_msk, B_ident], w=[B_ps[3]], inc=(j == g1 - 1))
                k.op("act", lambda e, g0=g0, g1=g1: e.activation(out=maskT[mp][:, g0:g1, :].rearrange("p j q -> p (j q)"),
                                                                 in_=psb[3][:, 0:(g1 - g0) * 128], func=AF.Identity),
                     r=[B_ps[3]], w=[B_maskT[mp]])
                yield

        def stage_attn(i):
            sl = i % 2
            mp = i % 2
            cntS = 0

            def emit_pv(h, g0, g1, es_):
                obank = 4 + h // 4
                for j in range(g0, g1):
                    k.op("pe", lambda e, j=j: e.matmul(
                        ps[obank][:, (h % 4) * 65:(h % 4) * 65 + 65], lhsT=PT[es_][:, (j - g0) * 128:(j - g0 + 1) * 128],
                        rhs=v_aug[:, j, h, :], start=(j == 0), stop=(j == i)),
                        r=[B_PT[es_], B_v], w=[B_ps[obank]], inc=(j == g1 - 1))

            prev = None
            for h in range(8):
                c = h // 2; pb = 64 * (h % 2)
                for g0 in range(0, i + 1, 4):
                    g1 = min(g0 + 4, i + 1)
                    ng = g1 - g0
                    bank = cntS % 3
                    es_ = cntS % 2
                    cntS += 1
                    for j in range(g0, g1):
                        near = j >= i - 1
                        k.op("pe", lambda e, j=j, g0=g0, bank=bank, near=near: e.matmul(
                            ps[bank][:, (j - g0) * 128:(j - g0 + 1) * 128], lhsT=kT[pb:pb + 64, c, j * 128:(j + 1) * 128],
                            rhs=qT_t[sl][pb:pb + 64, c, :], start=True, stop=(not near)),
                            r=[B_kc, B_ld[sl]], w=[B_ps[bank]], inc=(j == g1 - 1 and not near))
                        if near:
                            t = 0 if j == i else 1
                            k.op("pe", lambda e, j=j, g0=g0, bank=bank, t=t: e.matmul(
                                ps[bank][:, (j - g0) * 128:(j - g0 + 1) * 128], lhsT=ident[:, :], rhs=NBTh[:, t, h, :],
                                start=False, stop=False), r=[B_ident, B_NBT], w=[B_ps[bank]], inc=False)
                            k.op("pe", lambda e, j=j, g0=g0, bank=bank, t=t: e.matmul(
                                ps[bank][:, (j - g0) * 128:(j - g0 + 1) * 128], lhsT=ident[:, :], rhs=NBTl[:, t, h, :],
                                start=False, stop=True), r=[B_ident, B_NBT], w=[B_ps[bank]], inc=(j == g1 - 1))
                    k.op("act", lambda e, bank=bank, es_=es_, ng=ng: e.activation(
                        out=expS[es_][:, 0:ng * 128], in_=ps[bank][:, 0:ng * 128], func=AF.Exp, scale=0.125, bias=cB[:, h:h + 1]),
                        r=[B_ps[bank], B_NBT], w=[B_expS[es_]])
                    k.op("pool", lambda e, es_=es_, ng=ng, g0=g0, g1=g1: e.tensor_tensor(
                        out=PT[es_][:, 0:ng * 128], in0=expS[es_][:, 0:ng * 128],
                        in1=maskT[mp][:, g0:g1, :].rearrange("p j q -> p (j q)"), op=ALU.mult),
                        r=[B_expS[es_], B_maskT[mp]], w=[B_PT[es_]])
                    if prev is not None:
                        emit_pv(*prev)
                    prev = (h, g0, g1, es_)
                    yield
            emit_pv(*prev)
            for hb in range(2):
                pv = ps[4 + hb][:, 0:260].rearrange("p (h d) -> p h d", h=4)
                k.op("dve", lambda e, hb=hb, pv=pv: e.reciprocal(out=rec[:, hb * 4:(hb + 1) * 4], in_=pv[:, :, 64]),
                     r=[B_ps[4 + hb]], w=[B_oa])
                k.op("dve", lambda e, hb=hb, pv=pv: e.tensor_tensor(
                    out=o_a[:, hb * 4:(hb + 1) * 4, :], in0=pv[:, :, 0:64],
                    in1=rec[:, hb * 4:(hb + 1) * 4].unsqueeze(2).broadcast_to([128, 4, 64]), op=ALU.mult),
                    r=[B_ps[4 + hb], B_oa], w=[B_oa])
            oa2 = o_a[:].rearrange("p h d -> p (h d)")
            for c in range(4):
                k.op("pe", lambda e, c=c: e.transpose(out=psb[3][:, c * 128:(c + 1) * 128], in_=oa2[:, c * 128:(c + 1) * 128],
                                                      identity=ident[:]), r=[B_oa, B_ident], w=[B_ps[3]], inc=(c == 3))
            k.op("act", lambda e: e.activation(out=o_aT[sl][:].rearrange("p c t -> p (c t)"), in_=psb[3][:, 0:512], func=AF.Identity),
                 r=[B_ps[3]], w=[B_oaT[sl]])
            k.dma("pool", oaT_d[:, :, i * 128:(i + 1) * 128].rearrange("c p t -> p c t"), o_aT[sl][:], s_oaT[sl],
                  r=[B_oaT[sl]], w=[B_oaTd])
            yield

        def stage_gla(i):
            sl = i % 2
            tk = tok_t[sl]
            gv_v = tk[:, 512:1024]; gr_v = tk[:, 1024:1536]; gk_v = tk[:, 1536:1792]
            gq_v = gT_t[sl][:, 0:2, :]
            gkT_v = gT_t[sl][:, 2:4, :]
            k.op("pe", lambda e: e.matmul(ps[6][:, 0:256], lhsT=glr_t[sl][:, :], rhs=wg[:, :], start=True, stop=False),
                 r=[B_ld[sl], B_gc], w=[B_ps[6]], inc=False)
            k.op("pe", lambda e: e.matmul(ps[6][:, 0:256], lhsT=ones1[:, :], rhs=bg[:, :], start=False, stop=True),
                 r=[B_gc], w=[B_ps[6]])
            k.op("act", lambda e: e.activation(out=ge_[:], in_=ps[6][:, 0:256], func=AF.Exp, scale=-1.0), r=[B_ps[6]], w=[B_g1])
            k.op("act", lambda e: e.activation(out=ge_[:], in_=ge_[:], func=AF.Ln, bias=1.0), r=[B_g1], w=[B_g1])
            k.op("dve", lambda e: e.tensor_scalar(out=lgf[:], in0=ge_[:], scalar1=-1.0 / 16.0, scalar2=None, op0=ALU.mult),
                 r=[B_g1], w=[B_lg])
            k.op("dve", lambda e: e.tensor_copy(out=lgh[:], in_=lgf[:]), r=[B_lg], w=[B_lg])
            k.op("dve", lambda e: e.tensor_tensor(out=lgl[:], in0=lgf[:], in1=lgh[:], op=ALU.subtract), r=[B_lg], w=[B_lg])
            yield
            for fc in range(2):
                for pi, part in enumerate((lgh, lgl)):
                    k.op("pe", lambda e, fc=fc, part=part, pi=pi: e.matmul(
                        ps[7][:, fc * 128:(fc + 1) * 128], lhsT=part[:, fc * 128:(fc + 1) * 128], rhs=tri_in[:, :],
                        start=(pi == 0), stop=(pi == 1)), r=[B_lg, B_tri], w=[B_ps[7]], inc=False)
                for pi, part in enumerate((lgh, lgl)):
                    k.op("pe", lambda e, fc=fc, part=part, pi=pi: e.matmul(
                        ps[7][:, 256 + fc * 128:256 + (fc + 1) * 128], lhsT=part[:, fc * 128:(fc + 1) * 128], rhs=tri_st[:, :],
                        start=(pi == 0), stop=(pi == 1)), r=[B_lg, B_tri], w=[B_ps[7]], inc=False)
            for pi, part in enumerate((lgh, lgl)):
                k.op("pe", lambda e, part=part, pi=pi: e.matmul(
                    ps[6][:, 256:512], lhsT=tri_st[:, :], rhs=part[:, :], start=(pi == 0), stop=(pi == 1)),
                    r=[B_lg, B_tri], w=[B_ps[6], B_ps[7]], inc=(pi == 1))
            k.op("act", lambda e: e.activation(out=ET[:, 0, :], in_=ps[7][:, 0:256], func=AF.Exp), r=[B_ps[7]], w=[B_ET])
            k.op("act", lambda e: e.activation(out=ET[:, 1, :], in_=ps[7][:, 256:512], func=AF.Exp), r=[B_ps[7]], w=[B_ET])
            k.op("act", lambda e: e.activation(out=ET[:, 2, :], in_=ps[7][:, 256:512], func=AF.Exp, scale=-1.0), r=[B_ps[7]], w=[B_ET])
            k.op("act", lambda e: e.activation(out=E2[:], in_=ps[6][:, 256:512], func=AF.Exp), r=[B_ps[6]], w=[B_ET])
            yield
            gq2 = gq_v.rearrange("p c t -> p (c t)")
            gk2 = gkT_v.rearrange("p c t -> p (c t)")
            k.op("dve", lambda e: e.scalar_tensor_tensor(out=qin[:].rearrange("p c t -> p (c t)"), in0=gq2, scalar=0.125, in1=ET[:, 0, :],
                                                         op0=ALU.mult, op1=ALU.mult), r=[B_ld[sl], B_ET], w=[B_gq])
            k.op("dve", lambda e: e.tensor_tensor(out=kst[:].rearrange("p c t -> p (c t)"), in0=gk2, in1=ET[:, 1, :], op=ALU.mult),
                 r=[B_ld[sl], B_ET], w=[B_gq])
            k.op("dve", lambda e: e.scalar_tensor_tensor(out=qrl[:].rearrange("p c t -> p (c t)"), in0=gq2, scalar=0.125, in1=ET[:, 2, :],
                                                         op0=ALU.mult, op1=ALU.mult), r=[B_ld[sl], B_ET], w=[B_gq])
            k.op("dve", lambda e: e.tensor_tensor(out=kstk[:], in0=gk_v, in1=E2[:], op=ALU.mult), r=[B_ld[sl], B_ET], w=[B_gq])
            yield
            for h in range(4):
                fc = h // 2; pb = 64 * (h % 2)
                abank = 6 if h % 2 == 0 else 7
                k.op("pe", lambda e, h=h, fc=fc, pb=pb, abank=abank: e.matmul(
                    ps[abank][:, fc * 128:(fc + 1) * 128], lhsT=kst[pb:pb + 64, fc, :],
                    rhs=qrl[pb:pb + 64, fc, :], start=True, stop=True),
                    r=[B_gq], w=[B_ps[abank]])
            for h in range(4):
                fc = h // 2
                abank = 6 if h % 2 == 0 else 7
                k.op("dve", lambda e, h=h, fc=fc, abank=abank: e.tensor_tensor(
                    out=attT[:, h, :], in0=ps[abank][:, fc * 128:(fc + 1) * 128], in1=tri_in[:, :], op=ALU.mult),
                    r=[B_ps[abank], B_tri], w=[B_att])
            yield
            for h in range(4):
                fc = h // 2; pb = 64 * (h % 2)
                k.op("pe", lambda e, h=h: e.matmul(ps[7][:, h * 128:(h + 1) * 128], lhsT=attT[:, h, :], rhs=gv_v[:, h * 128:(h + 1) * 128],
                                                   start=True, stop=False), r=[B_att, B_ld[sl]], w=[B_ps[7]], inc=False)
                k.op("pe", lambda e, h=h, fc=fc, pb=pb: e.matmul(ps[7][:, h * 128:(h + 1) * 128], lhsT=qin[pb:pb + 64, fc, :],
                                                                 rhs=Sb[pb:pb + 64, fc, :], start=False, stop=True),
                     r=[B_gq, B_Sb], w=[B_ps[7]], inc=(h == 3))
            yield
            for fc in range(2):
                k.op("pe", lambda e, fc=fc: e.matmul(ps[6][:, fc * 256:(fc + 1) * 256], lhsT=kstk[:, fc * 128:(fc + 1) * 128],
                                                     rhs=gv_v[:, fc * 256:(fc + 1) * 256], start=True, stop=True),
                     r=[B_gq, B_ld[sl]], w=[B_ps[6]], inc=(fc == 1))
            for fc in range(2):
                for hh in range(2):
                    pb = 64 * hh
                    k.op("dve", lambda e, fc=fc, hh=hh, pb=pb: e.scalar_tensor_tensor(
                        out=Sst[pb:pb + 64, fc, :], in0=Sst[pb:pb + 64, fc, :], scalar=ET[pb:pb + 64, 0, fc * 128 + 127:fc * 128 + 128],
                        in1=ps[6][pb:pb + 64, fc * 256 + hh * 128:fc * 256 + (hh + 1) * 128], op0=ALU.mult, op1=ALU.add),
                        r=[B_ET, B_ps[6], B_S], w=[B_S])
            k.op("act", lambda e: e.activation(out=Sb[:].rearrange("p c t -> p (c t)"), in_=Sst[:].rearrange("p c t -> p (c t)"),
                                               func=AF.Identity), r=[B_S], w=[B_Sb])
            yield
            for h in range(4):
                k.op("dve", lambda e, h=h: e.bn_stats(out=gst[:, h, :], in_=ps[7][:, h * 128:(h + 1) * 128]), r=[B_ps[7]], w=[B_gs])
            for h in range(4):
                k.op("dve", lambda e, h=h: e.bn_aggr(out=gmv[:, h, :], in_=gst[:, h, :]), r=[B_gs], w=[B_gs])
            k.op("act", lambda e: e.activation(out=grs[:, 0:4], in_=gmv[:, :, 1], func=AF.Sqrt, bias=EPS), r=[B_gs], w=[B_gs])
            k.op("dve", lambda e: e.reciprocal(out=grs[:, 0:4], in_=grs[:, 0:4]), r=[B_gs], w=[B_gs])
            k.op("dve", lambda e: e.scalar_tensor_tensor(out=grs[:, 4:8], in0=gmv[:, :, 0], scalar=-1.0, in1=grs[:, 0:4],
                                                         op0=ALU.mult, op1=ALU.mult), r=[B_gs], w=[B_gs])
            for h in range(4):
                k.op("act", lambda e, h=h: e.activation(out=on[:, h * 128:(h + 1) * 128], in_=ps[7][:, h * 128:(h + 1) * 128],
                                                        func=AF.Identity, scale=grs[:, h:h + 1], bias=grs[:, 4 + h:5 + h]),
                     r=[B_ps[7], B_gs], w=[B_on])
            k.op("act", lambda e: e.activation(out=sg[:], in_=gr_v, func=AF.Silu), r=[B_ld[sl]], w=[B_ob])
            k.op("dve", lambda e: e.tensor_tensor(out=on[:], in0=on[:], in1=gB[:], op=ALU.mult), r=[B_on, B_gc], w=[B_on])
            k.op("dve", lambda e: e.tensor_tensor(out=ob[:], in0=on[:], in1=sg[:], op=ALU.mult), r=[B_on, B_ob], w=[B_ob])
            for c in range(4):
                k.op("pe", lambda e, c=c: e.transpose(out=psb[3][:, 512 + c * 128:512 + (c + 1) * 128], in_=ob[:, c * 128:(c + 1) * 128],
                                                      identity=ident[:]), r=[B_ob, B_ident], w=[B_ps[3]], inc=(c == 3))
            k.op("act", lambda e: e.activation(out=o_bT[sl][:].rearrange("p c t -> p (c t)"), in_=psb[3][:, 512:1024], func=AF.Identity),
                 r=[B_ps[3]], w=[B_obT[sl]])
            k.dma("pool", obT_d[:, :, i * 128:(i + 1) * 128].rearrange("c p t -> p c t"), o_bT[sl][:], s_obT[sl],
                  r=[B_obT[sl]], w=[B_obTd])

            yield

        def run_interleaved(gens, weights):
            live = [[g, w] for g, w in zip(gens, weights)]
            while live:
                for ent in list(live):
                    g, w = ent
                    for _ in range(w):
                        try:
                            next(g)
                        except StopIteration:
                            live.remove(ent)
                            break

        for step in range(nt3 + 1):
            if step < nt3:
                stage_idx(step)
            gens = []; wts = []
            if step < nt3:
                gens.append(stage_bis(step)); wts.append(1)
                gens.append(stage_gla(step)); wts.append(1)
            if step >= 1:
                gens.append(stage_attn(step - 1)); wts.append(3)
            run_interleaved(gens, wts)
        phase_end()
        vstack.close()
        if debug:
            s_dbg2 = k.new_sem("dbg2")
            k.dma("sp", dbg["oaT"][:, :, :], oaT_d[:, :, :], s_dbg2)
            k.dma("sp", dbg["obT"][:, :, :], obT_d[:, :, :], s_dbg2)
            k.final_wait("sp", [s_dbg2])
        if STOP == 20:
            return nc
        phase_begin()
        h2T = hT
        B_h2T = [Buf("h2T%d" % i) for i in range(NT)]
        w_ba = lsb("w_ba_s", [128, 4, 1024], BF16)
        w_bb = lsb("w_bb_s", [128, 4, 1024], BF16)
        w_o = lsb("w_o_s", [128, 8, 1024], BF16)
        g1B = lsb("g1B", [128, 1024], F32)
        b1B = lsb("b1B", [128, 1024], F32)
        B_w3b = Buf("w3b")
        s_w3b = k.new_sem("w3b")
        k.dma("pool", w_ba[:], w_ba_d.rearrange("(k p) j -> p k j", p=128), s_w3b, w=[B_w3b])
        k.dma("pool", w_bb[:], w_bb_d.rearrange("(k p) j -> p k j", p=128), s_w3b, w=[B_w3b])
        k.dma("pool", w_o[:], w_o_d.rearrange("(k p) j -> p k j", p=128), s_w3b, w=[B_w3b])
        s_w3c = k.new_sem("w3c")
        k.dma("sp", g1B[:], ln1_g_d[0:1, :].broadcast_to([128, 1024]), s_w3c, w=[B_w3b])
        k.dma("sp", b1B[:], ln1_b_d[0:1, :].broadcast_to([128, 1024]), s_w3c, w=[B_w3b])
        k.barrier()
        xs = [lsb("xs%d" % i, [128, D], F32) for i in range(2)]
        B_xs = [Buf("xs0"), Buf("xs1")]
        s_xs = [k.new_sem("xsb0"), k.new_sem("xsb1")]
        alloc_ln(4, nxn=2)
        gts = [lsb("gts%d" % i, [128, 2048], BF16) for i in range(2)]
        oaL = [lsb("oaL%d" % i, [128, 4, 128], BF16) for i in range(2)]
        obL = [lsb("obL%d" % i, [128, 4, 128], BF16) for i in range(2)]
        B_l3 = [Buf("l3_0"), Buf("l3_1")]
        s_l3 = [k.new_sem("l3_0"), k.new_sem("l3_1")]
        sga = lsb("sga", [128, 1024], BF16)
        sgb = lsb("sgb", [128, 1024], BF16)
        m1 = lsb("m1", [128, 1024], F32)
        m2 = lsb("m2", [128, 1024], F32)
        mrg = lsb("mrg", [128, 1024], BF16)
        mrgT = lsb("mrgT", [128, 8, 128], BF16)
        vv = lsb("vv", [128, 1024], F32)
        x1t = [lsb("x1t%d" % i, [128, 1024], F32) for i in range(3)]
        B_sg = Buf("sg"); B_m1 = Buf("m1"); B_m2 = Buf("m2"); B_mrg = Buf("mrg"); B_mrgT = Buf("mrgT"); B_vv = Buf("vv")
        B_x1t = [Buf("x1t%d" % i) for i in range(3)]
        s_x1t = [k.new_sem("x1t%d" % i) for i in range(3)]
        B_x1d = [Buf("x1d%d" % i) for i in range(NT)]
        mrgT2 = [mrgT, lsb("mrgT1", [128, 8, 128], BF16)]
        B_mrgT2 = [B_mrgT, Buf("mrgT1")]

        def p3b_front(i):
            sl = i % 2
            k.dma("sp", gts[sl][:], tok_d[i * 128:(i + 1) * 128, 2320:4368], s_l3[sl], r=[B_tok], w=[B_l3[sl]])
            k.dma("sp", oaL[sl][:], oaT_d[:, :, i * 128:(i + 1) * 128].rearrange("c p t -> p c t"), s_l3[sl], r=[B_oaTd], w=[B_l3[sl]])
            k.dma("sp", obL[sl][:], obT_d[:, :, i * 128:(i + 1) * 128].rearrange("c p t -> p c t"), s_l3[sl], r=[B_obTd], w=[B_l3[sl]])
            k.dma("sp", xs[sl][:], x_d[i * 128:(i + 1) * 128, :], s_xs[sl], w=[B_xs[sl]])
            for br, (src, wt) in enumerate(((oaL[sl], w_ba), (obL[sl], w_bb))):
                for half in range(2):
                    bank = br * 2 + half
                    for kc in range(4):
                        k.op("pe", lambda e, src=src, wt=wt, kc=kc, half=half, bank=bank: e.matmul(
                            ps[bank][:, :], lhsT=src[:, kc, :], rhs=wt[:, kc, half * 512:(half + 1) * 512],
                            start=(kc == 0), stop=(kc == 3)), r=[B_l3[sl], B_w3b], w=[B_ps[bank]], inc=(kc == 3))
            k.op("act", lambda e: e.activation(out=sga[:], in_=gts[sl][:, 0:1024], func=AF.Sigmoid), r=[B_l3[sl]], w=[B_sg])
            k.op("act", lambda e: e.activation(out=sgb[:], in_=gts[sl][:, 1024:2048], func=AF.Sigmoid), r=[B_l3[sl]], w=[B_sg])
            yield
            for half in range(2):
                cs = slice(half * 512, (half + 1) * 512)
                k.op("dve", lambda e, half=half, cs=cs: e.tensor_tensor(out=m1[:, cs], in0=ps[half][:, :], in1=sga[:, cs], op=ALU.mult),
                     r=[B_ps[half], B_sg], w=[B_m1])
                k.op("dve", lambda e, half=half, cs=cs: e.tensor_tensor(out=m2[:, cs], in0=ps[2 + half][:, :], in1=sgb[:, cs], op=ALU.mult),
                     r=[B_ps[2 + half], B_sg], w=[B_m2])
            yield
            k.op("pool", lambda e: e.tensor_tensor(out=mrg[:], in0=m1[:], in1=m2[:], op=ALU.add), r=[B_m1, B_m2], w=[B_mrg])
            for kc in range(8):
                k.op("pe", lambda e, kc=kc: e.transpose(out=psb[6][:, kc * 128:(kc + 1) * 128], in_=mrg[:, kc * 128:(kc + 1) * 128],
                                                        identity=ident[:]), r=[B_mrg, B_ident], w=[B_ps[6]], inc=(kc == 7))
            k.op("act", lambda e: e.activation(out=mrgT2[sl][:].rearrange("p c t -> p (c t)"), in_=psb[6][:, :], func=AF.Identity),
                 r=[B_ps[6]], w=[B_mrgT2[sl]])
            yield

        def p3b_back(i):
            sl = i % 2
            for half in range(2):
                bank = 4 + half
                for kc in range(8):
                    k.op("pe", lambda e, kc=kc, half=half, bank=bank: e.matmul(
                        ps[bank][:, :], lhsT=mrgT2[sl][:, kc, :], rhs=w_o[:, kc, half * 512:(half + 1) * 512],
                        start=(kc == 0), stop=(kc == 7)), r=[B_mrgT2[sl], B_w3b], w=[B_ps[bank]], inc=(kc == 7))
            for half in range(2):
                cs = slice(half * 512, (half + 1) * 512)
                k.op("dve", lambda e, half=half, cs=cs: e.tensor_tensor(out=vv[:, cs], in0=ps[4 + half][:, :], in1=gtB[:, cs], op=ALU.mult),
                     r=[B_ps[4 + half], B_gtB], w=[B_vv])
            k.op("dve", lambda e: e.scalar_tensor_tensor(out=vv[:], in0=xs[sl][:], scalar=ALPHA, in1=vv[:], op0=ALU.mult, op1=ALU.add),
                 r=[B_xs[sl], B_vv], w=[B_vv])
            yield
            s3 = i % 3
            mv = LN["mv"]; B_mv = LN["B_mv"]
            ln_stats(vv, B_vv, sl)
            k.op("act", lambda e: e.activation(out=x1t[s3][:], in_=vv[:], func=AF.Identity, scale=mv[sl][:, 2:3], bias=mv[sl][:, 3:4]),
                 r=[B_vv, B_mv[sl]], w=[B_x1t[s3]])
            yield
            k.op("pool", lambda e: e.tensor_tensor(out=x1t[s3][:], in0=x1t[s3][:], in1=g1B[:], op=ALU.mult), r=[B_x1t[s3], B_w3b], w=[B_x1t[s3]])
            k.op("pool", lambda e: e.tensor_tensor(out=x1t[s3][:], in0=x1t[s3][:], in1=b1B[:], op=ALU.add), r=[B_x1t[s3], B_w3b], w=[B_x1t[s3]])
            k.dma("pool", x1_d[i * 128:(i + 1) * 128, :], x1t[s3][:], s_x1t[s3], r=[B_x1t[s3]], w=[B_x1d[i]])
            yield

        def p3b_feat(i):
            s3 = i % 3
            yield from to_featT(i, x1t[s3], B_x1t[s3], h2T, B_h2T[i], i * 128, 24, 16, bank=7, slo=2)
            yield

        def run_il(gens):
            live = list(gens)
            while live:
                for g in list(live):
                    try:
                        next(g)
                    except StopIteration:
                        live.remove(g)

        for step in range(NT + 2):
            gens = []
            if step < NT:
                gens.append(p3b_front(step))
            if 1 <= step <= NT:
                gens.append(p3b_back(step - 1))
            if 2 <= step:
                gens.append(p3b_feat(step - 2))
            run_il(gens)
        s_h2 = k.new_sem("h2d")
        B_h2d = Buf("h2d")
        k.dma("sp", h2T_d.rearrange("k p t -> p k t"), h2T[:], s_h2, r=B_h2T, w=[B_h2d])
        if debug:
            dbg["x1"] = nc.dram_tensor("dbg_x1", [S, D], F32, kind="ExternalOutput").ap()
            k.barrier()
            k.dma("sp", dbg["x1"][:, :], x1_d[:, :], s_h2, r=B_x1d)
        phase_end()
        if STOP == 21:
            return nc
        phase_begin()
        HT = 2048
        h2h = lsb("h2h", [128, 8, HT], BF16)
        B_h2h = Buf("h2h")
        s_h2h = k.new_sem("h2h")
        acc = hT[:].rearrange("p a b -> p (a b)").bitcast(F32).rearrange("p (t c) -> p t c", c=1024)
        B_acc = [Buf("acc%d" % i) for i in range(HT // 128)]
        wr = lsb("wr", [128, 8, 36], BF16)
        brr = lsb("brr", [1, 36], BF16)
        brf = lsb("brf", [1, 36], F32)
        g2B = lsb("g2B", [128, 1024], F32)
        b2B = lsb("b2B", [128, 1024], F32)
        B_wr = Buf("wr")
        s_wr = k.new_sem("wr")
        k.dma("pool", wr[:], w_r_d.rearrange("(k p) j -> p k j", p=128), s_wr, w=[B_wr])
        s_wr2 = k.new_sem("wr2")
        k.dma("sp", brf[:], b_r_d[:, :], s_wr2, w=[B_wr])
        k.dma("sp", g2B[:], ln2_g_d[0:1, :].broadcast_to([128, 1024]), s_wr2, w=[B_wr])
        k.dma("sp", b2B[:], ln2_b_d[0:1, :].broadcast_to([128, 1024]), s_wr2, w=[B_wr])
        k.op("dve", lambda e: e.tensor_copy(out=brr[:], in_=brf[:]), r=[B_wr], w=[B_wr])
        k.barrier()
        gates = lsb("gates", [128, HT // 128, 32], F32)
        B_gates = Buf("gates")
        B_rt = Buf("rt")
        TH_ = HT // 128
        L3 = lsb("r_L3", [128, TH_, 36], F32)
        r_mx = lsb("r_mx", [128, TH_], F32); r_gw = lsb("r_gw", [128, TH_], F32)
        r_m1 = lsb("r_m1", [128, TH_], F32); r_m2 = lsb("r_m2", [128, TH_], F32)
        r_d = lsb("r_d", [128, TH_], F32); r_w1 = lsb("r_w1", [128, TH_], F32); r_w2 = lsb("r_w2", [128, TH_], F32)
        r_oh4 = lsb("r_oh4", [128, TH_, 4], F32); r_ex4 = lsb("r_ex4", [128, TH_, 4], F32)
        r_t32 = lsb("r_t32", [128, TH_, 4, 8], F32)
        r_eig = lsb("r_eig", [128, TH_, 8], F32); r_e2 = lsb("r_e2", [128, TH_, 8], F32)
        r_eq1 = lsb("r_eq1", [128, TH_, 8], F32); r_eq2 = lsb("r_eq2", [128, TH_, 8], F32)
        w1s = [lsb("w1s%d" % i, [128, 8, 256], BF16) for i in range(2)]
        w3s = [lsb("w3s%d" % i, [128, 8, 256], BF16) for i in range(2)]
        w2s = [lsb("w2s%d" % i, [128, 2, 1024], BF16) for i in range(2)]
        B_ws = [Buf("ws0"), Buf("ws1")]
        s_ws = [k.new_sem("ws0"), k.new_sem("ws1")]
        sgl = [lsb("sgl%d" % i, [128, 2, 512], BF16) for i in range(2)]
        hid = [lsb("hid%d" % i, [128, 2, 512], BF16) for i in range(2)]
        B_sgl = [Buf("sgl0"), Buf("sgl1")]
        B_hid = [Buf("hid0"), Buf("hid1")]
        NTL = 3
        st6b = lsb("st6b", [128, HT // 128, 2, 6], F32)
        mvb = lsb("mvb", [128, HT // 128, 4], F32)
        B_mvt = [Buf("mvt%d" % i) for i in range(HT // 128)]
        x1l = [lsb("x1l%d" % i, [128, 1024], F32) for i in range(NTL)]
        B_x1l = [Buf("x1l%d" % i) for i in range(NTL)]
        s_x1l = [k.new_sem("x1l%d" % i) for i in range(NTL)]
        fo = [lsb("fo%d" % i, [128, 1024], F32) for i in range(NTL)]
        B_fo = [Buf("fo%d" % i) for i in range(NTL)]
        s_fo = [k.new_sem("fo%d" % i) for i in range(NTL)]
        NTH = HT // 128
        k.dma("sp", h2h[:], h2T_d[:, :, 0:HT].rearrange("k p t -> p k t"), s_h2h, r=[B_h2d], w=[B_h2h])
        for hf in range(S // HT):
            for ti in range(NTH):
                bank = ti // 8
                co = (ti % 8) * 36
                k.op("pe", lambda e, bank=bank, co=co: e.matmul(ps[bank][:, co:co + 36], lhsT=ones1[:, :], rhs=brr[:, :], start=True, stop=False),
                     r=[B_wr], w=[B_ps[bank]], inc=False)
                for kc in range(8):
                    k.op("pe", lambda e, kc=kc, ti=ti, bank=bank, co=co: e.matmul(
                        ps[bank][:, co:co + 36], lhsT=h2h[:, kc, ti * 128:(ti + 1) * 128], rhs=wr[:, kc, :],
                        start=False, stop=(kc == 7)), r=[B_h2h, B_wr], w=[B_ps[bank]], inc=(kc == 7))
            T_ = NTH
            for bank in range(2):
                k.op("dve", lambda e, bank=bank: e.tensor_copy(out=L3[:, bank * 8:(bank + 1) * 8, :].rearrange("p t c -> p (t c)"),
                                                               in_=ps[bank][:, 0:288]), r=[B_ps[bank]], w=[B_rt])
            lg4 = L3[:, :, 0:4]
            le = L3[:, :, 4:36].rearrange("p t (g e) -> p t g e", g=4)
            k.op("dve", lambda e: e.tensor_reduce(out=r_mx[:], in_=lg4, axis=AX.X, op=ALU.max), r=[B_rt], w=[B_rt])
            k.op("dve", lambda e: e.tensor_tensor(out=r_oh4[:], in0=lg4, in1=r_mx[:].unsqueeze(2).broadcast_to([128, T_, 4]), op=ALU.is_equal),
                 r=[B_rt], w=[B_rt])
            k.op("dve", lambda e: e.tensor_tensor(out=r_ex4[:], in0=lg4, in1=r_mx[:].unsqueeze(2).broadcast_to([128, T_, 4]), op=ALU.subtract),
                 r=[B_rt], w=[B_rt])
            k.op("act", lambda e: e.activation(out=r_ex4[:], in_=r_ex4[:], func=AF.Exp), r=[B_rt], w=[B_rt])
            k.op("dve", lambda e: e.tensor_reduce(out=r_gw[:], in_=r_ex4[:], axis=AX.X, op=ALU.add), r=[B_rt], w=[B_rt])
            k.op("dve", lambda e: e.reciprocal(out=r_gw[:], in_=r_gw[:]), r=[B_rt], w=[B_rt])
            k.op("dve", lambda e: e.tensor_tensor(out=r_t32[:], in0=le, in1=r_oh4[:].unsqueeze(3).broadcast_to([128, T_, 4, 8]), op=ALU.mult),
                 r=[B_rt], w=[B_rt])
            k.op("dve", lambda e: e.tensor_reduce(out=r_eig[:], in_=r_t32[:].rearrange("p t g e -> p t e g"), axis=AX.X, op=ALU.add),
                 r=[B_rt], w=[B_rt])
            k.op("dve", lambda e: e.tensor_reduce(out=r_m1[:], in_=r_eig[:], axis=AX.X, op=ALU.max), r=[B_rt], w=[B_rt])
            k.op("dve", lambda e: e.tensor_tensor(out=r_eq1[:], in0=r_eig[:], in1=r_m1[:].unsqueeze(2).broadcast_to([128, T_, 8]), op=ALU.is_equal),
                 r=[B_rt], w=[B_rt])
            k.op("dve", lambda e: e.scalar_tensor_tensor(out=r_e2[:], in0=r_eq1[:], scalar=-1.0e30, in1=r_eig[:], op0=ALU.mult, op1=ALU.add),
                 r=[B_rt], w=[B_rt])
            k.op("dve", lambda e: e.tensor_reduce(out=r_m2[:], in_=r_e2[:], axis=AX.X, op=ALU.max), r=[B_rt], w=[B_rt])
            k.op("dve", lambda e: e.tensor_tensor(out=r_eq2[:], in0=r_eig[:], in1=r_m2[:].unsqueeze(2).broadcast_to([128, T_, 8]), op=ALU.is_equal),
                 r=[B_rt], w=[B_rt])
            k.op("dve", lambda e: e.tensor_tensor(out=r_d[:], in0=r_m2[:], in1=r_m1[:], op=ALU.subtract), r=[B_rt], w=[B_rt])
            k.op("act", lambda e: e.activation(out=r_d[:], in_=r_d[:], func=AF.Exp), r=[B_rt], w=[B_rt])
            k.op("dve", lambda e: e.tensor_scalar(out=r_w1[:], in0=r_d[:], scalar1=1.0, scalar2=None, op0=ALU.add), r=[B_rt], w=[B_rt])
            k.op("dve", lambda e: e.reciprocal(out=r_w1[:], in_=r_w1[:]), r=[B_rt], w=[B_rt])
            k.op("dve", lambda e: e.tensor_tensor(out=r_w1[:], in0=r_w1[:], in1=r_gw[:], op=ALU.mult), r=[B_rt], w=[B_rt])
            k.op("dve", lambda e: e.tensor_tensor(out=r_w2[:], in0=r_w1[:], in1=r_d[:], op=ALU.mult), r=[B_rt], w=[B_rt])
            k.op("dve", lambda e: e.tensor_tensor(out=r_eq1[:], in0=r_eq1[:], in1=r_w1[:].unsqueeze(2).broadcast_to([128, T_, 8]), op=ALU.mult),
                 r=[B_rt], w=[B_rt])
            k.op("dve", lambda e: e.tensor_tensor(out=r_eq2[:], in0=r_eq2[:], in1=r_w2[:].unsqueeze(2).broadcast_to([128, T_, 8]), op=ALU.mult),
                 r=[B_rt], w=[B_rt])
            k.op("dve", lambda e: e.tensor_tensor(out=r_eq1[:], in0=r_eq1[:], in1=r_eq2[:], op=ALU.add), r=[B_rt], w=[B_rt])
            k.op("dve", lambda e: e.tensor_tensor(out=gates[:].rearrange("p t (g e) -> p t g e", g=4),
                                                  in0=r_oh4[:].unsqueeze(3).broadcast_to([128, T_, 4, 8]),
                                                  in1=r_eq1[:].unsqueeze(2).broadcast_to([128, T_, 4, 8]), op=ALU.mult),
                 r=[B_rt], w=[B_gates])
            ycnt = [0]

            def emit_H(ex, tb, ws, hs):
                for which, wsrc in enumerate((w1s[ws], w3s[ws])):
                    for fcn in range(2):
                        bank = which * 2 + fcn
                        for kc in range(8):
                            k.op("pe", lambda e, wsrc=wsrc, fcn=fcn, kc=kc, bank=bank: e.matmul(
                                ps[bank][:, :], lhsT=wsrc[:, kc, fcn * 128:(fcn + 1) * 128], rhs=h2h[:, kc, tb * 512:(tb + 1) * 512],
                                start=(kc == 0), stop=(kc == 7)), r=[B_ws[ws], B_h2h], w=[B_ps[bank]], inc=(kc == 7))
                for fcn in range(2):
                    k.op("act", lambda e, fcn=fcn: e.activation(out=sgl[hs][:, fcn, :], in_=ps[fcn][:, :], func=AF.Silu),
                         r=[B_ps[fcn]], w=[B_sgl[hs]])
                for fcn in range(2):
                    k.op("dve", lambda e, fcn=fcn: e.tensor_tensor(out=hid[hs][:, fcn, :], in0=ps[2 + fcn][:, :], in1=sgl[hs][:, fcn, :],
                                                                   op=ALU.mult), r=[B_ps[2 + fcn], B_sgl[hs]], w=[B_hid[hs]])

            def emit_Y(ex, tb, ws, hs):
                for t4 in range(4):
                    ti = tb * 4 + t4
                    yb = 4 + 2 * (ycnt[0] % 2)
                    ycnt[0] += 1
                    for h2_ in range(2):
                        for fcn in range(2):
                            k.op("pe", lambda e, h2_=h2_, fcn=fcn, t4=t4, yb=yb: e.matmul(
                                ps[yb + h2_][:, :], lhsT=hid[hs][:, fcn, t4 * 128:(t4 + 1) * 128], rhs=w2s[ws][:, fcn, h2_ * 512:(h2_ + 1) * 512],
                                start=(fcn == 0), stop=(fcn == 1)), r=[B_hid[hs], B_ws[ws]], w=[B_ps[yb + h2_]], inc=(fcn == 1))
                    for h2_ in range(2):
                        cs = slice(h2_ * 512, (h2_ + 1) * 512)
                        if ex == 0:
                            k.op("dve", lambda e, h2_=h2_, cs=cs, ti=ti, yb=yb: e.tensor_scalar(
                                out=acc[:, ti, cs], in0=ps[yb + h2_][:, :], scalar1=gates[:, ti, ex:ex + 1], scalar2=None, op0=ALU.mult),
                                r=[B_ps[yb + h2_], B_gates], w=[B_acc[ti]])
                        else:
                            k.op("dve", lambda e, h2_=h2_, cs=cs, ti=ti, yb=yb: e.scalar_tensor_tensor(
                                out=acc[:, ti, cs], in0=ps[yb + h2_][:, :], scalar=gates[:, ti, ex:ex + 1], in1=acc[:, ti, cs],
                                op0=ALU.mult, op1=ALU.add), r=[B_ps[yb + h2_], B_gates, B_acc[ti]], w=[B_acc[ti]])

            prev = None
            hcnt = 0
            for ex in range(32):
                ws = ex % 2
                k.dma("pool", w1s[ws][:], w_eg_d[ex].rearrange("(k p) f -> p k f", p=128), s_ws[ws], w=[B_ws[ws]])
                k.dma("pool", w3s[ws][:], w_eu_d[ex].rearrange("(k p) f -> p k f", p=128), s_ws[ws], w=[B_ws[ws]])
                k.dma("pool", w2s[ws][:], w_ed_d[ex].rearrange("(k p) j -> p k j", p=128), s_ws[ws], w=[B_ws[ws]])
                for tb in range(HT // 512):
                    hs = hcnt % 2
                    hcnt += 1
                    emit_H(ex, tb, ws, hs)
                    if prev is not None:
                        emit_Y(*prev)
                    prev = (ex, tb, ws, hs)
            emit_Y(*prev)
            if hf + 1 < S // HT:
                k.dma("sp", h2h[:], h2T_d[:, :, (hf + 1) * HT:(hf + 2) * HT].rearrange("k p t -> p k t"), s_h2h, r=[B_h2d], w=[B_h2h])
            for ti in range(NTH):
                i = hf * NTH + ti
                sl = ti % NTL
                k.dma("sp", x1l[sl][:], x1_d[i * 128:(i + 1) * 128, :], s_x1l[sl], r=[B_x1d[i]], w=[B_x1l[sl]])
                k.op("pool", lambda e, ti=ti: e.tensor_tensor(out=acc[:, ti, :], in0=acc[:, ti, :], in1=gtB[:, 1024:2048], op=ALU.mult),
                     r=[B_acc[ti], B_gtB], w=[B_acc[ti]])
                k.op("dve", lambda e, ti=ti, sl=sl: e.scalar_tensor_tensor(out=acc[:, ti, :], in0=x1l[sl][:], scalar=ALPHA, in1=acc[:, ti, :],
                                                                           op0=ALU.mult, op1=ALU.add), r=[B_x1l[sl], B_acc[ti]], w=[B_acc[ti]])
                for hh in range(2):
                    k.op("dve", lambda e, ti=ti, hh=hh: e.bn_stats(out=st6b[:, ti, hh, :], in_=acc[:, ti, hh * 512:(hh + 1) * 512]),
                         r=[B_acc[ti]], w=[B_mvt[ti]])
                k.op("dve", lambda e, ti=ti: e.bn_aggr(out=mvb[:, ti, 0:2], in_=st6b[:, ti, :, :].rearrange("p a b -> p (a b)")),
                     r=[B_mvt[ti]], w=[B_mvt[ti]])
            k.op("act", lambda e: e.activation(out=mvb[:, :, 2], in_=mvb[:, :, 1], func=AF.Sqrt, bias=EPS), r=B_mvt, w=B_mvt)
            k.op("dve", lambda e: e.reciprocal(out=mvb[:, :, 2], in_=mvb[:, :, 2]), r=B_mvt, w=B_mvt)
            k.op("dve", lambda e: e.scalar_tensor_tensor(out=mvb[:, :, 3], in0=mvb[:, :, 0], scalar=-1.0, in1=mvb[:, :, 2],
                                                         op0=ALU.mult, op1=ALU.mult), r=B_mvt, w=B_mvt)
            for ti in range(NTH):
                i = hf * NTH + ti
                k.op("act", lambda e, ti=ti: e.activation(out=acc[:, ti, :], in_=acc[:, ti, :], func=AF.Identity, scale=mvb[:, ti, 2:3],
                                                          bias=mvb[:, ti, 3:4]), r=[B_acc[ti], B_mvt[ti]], w=[B_acc[ti]])
                k.op("dve", lambda e, ti=ti: e.tensor_tensor(out=acc[:, ti, :], in0=acc[:, ti, :], in1=g2B[:], op=ALU.mult),
                     r=[B_acc[ti], B_wr], w=[B_acc[ti]])
                k.op("pool", lambda e, ti=ti: e.tensor_tensor(out=acc[:, ti, :], in0=acc[:, ti, :], in1=b2B[:], op=ALU.add),
                     r=[B_acc[ti], B_wr], w=[B_acc[ti]])
                k.dma("pool", out_d[i * 128:(i + 1) * 128, :], acc[:, ti, :], s_fo[ti % NTL], r=[B_acc[ti]])
        k.barrier()
        k.final_wait("sp", s_fo)
        cur[0].close()
        cur[0] = None
    return nc


def t5_bucket_np(d):
    d = np.asarray(d)
    max_exact = 16
    d_f = np.maximum(d, 1).astype(np.float32)
    large = max_exact + (np.log(d_f / max_exact) / np.log(128 / max_exact) * (32 - max_exact)).astype(np.int32)
    large = np.minimum(large, 31)
    return np.where(d < max_exact, d, large)


def make_ohpad():
    oh = np.zeros((32, 384), np.float32)
    d = np.arange(256)
    b = t5_bucket_np(d)
    oh[b, 128 + d] = 8.0
    return oh


def prep_inputs(inputs, b):
    f = np.float32
    c = np.ascontiguousarray(inputs["c"][b].reshape(8, 128).T.astype(f))
    b_ada = inputs["b_ada"][0]
    m = {
        "x": np.ascontiguousarray(inputs["x"][b]),
        "c_pl": c,
        "w_ada": np.ascontiguousarray(inputs["w_ada"][0]),
        "b_ada_pl": np.ascontiguousarray(b_ada.reshape(48, 128).T),
        "b_ada_row": np.ascontiguousarray(b_ada.reshape(1, -1)),
        "w_in": np.ascontiguousarray(inputs["w_in"][0]),
        "rel_bias": np.ascontiguousarray(inputs["rel_bias"].astype(f)),
        "ohpad": make_ohpad(),
        "gla_wg": np.ascontiguousarray(inputs["gla_w_gate"][0]),
        "gla_bg": np.ascontiguousarray(inputs["gla_b_gate"][0].reshape(1, -1)),
        "gla_g": np.ascontiguousarray(inputs["gla_norm_g"][0].reshape(1, -1)),
        "w_ba": np.ascontiguousarray(inputs["w_branch_a"][0]),
        "w_bb": np.ascontiguousarray(inputs["w_branch_b"][0]),
        "w_o": np.ascontiguousarray(inputs["w_out"][0]),
        "ln1_g": np.ascontiguousarray(inputs["ln1_g"][0].reshape(1, -1)),
        "ln1_b": np.ascontiguousarray(inputs["ln1_b"][0].reshape(1, -1)),
        "ln2_g": np.ascontiguousarray(inputs["ln2_g"][0].reshape(1, -1)),
        "ln2_b": np.ascontiguousarray(inputs["ln2_b"][0].reshape(1, -1)),
        "w_r": np.ascontiguousarray(np.concatenate([inputs["w_router_group"][0], inputs["w_router_expert"][0]], axis=1)),
        "b_r": np.ascontiguousarray(np.concatenate([inputs["b_router_group"][0], inputs["b_router_expert"][0]]).reshape(1, -1)),
        "w_eg": np.ascontiguousarray(inputs["w_exp_gate"][0]),
        "w_eu": np.ascontiguousarray(inputs["w_exp_up"][0]),
        "w_ed": np.ascontiguousarray(inputs["w_exp_down"][0]),
    }
    return m


def kernel(**inputs):
    nc = build_nc()
    in_maps = [prep_inputs(inputs, b) for b in range(8)]
    res = run_bass_kernel_spmd(nc, in_maps, core_ids=list(range(8)))
    return np.stack([r["out"] for r in res.results], axis=0)
```

```python
import numpy as np
from contextlib import ExitStack
import concourse.bass as bass
import concourse.mybir as mybir
from concourse.bass_utils import run_bass_kernel_spmd

F32 = mybir.dt.float32
BF16 = mybir.dt.bfloat16
AF = mybir.ActivationFunctionType
ALU = mybir.AluOpType
AX = mybir.AxisListType

S = 4096
D = 1024
NT = S // 128
DP = 5696
O_AQ, O_AK, O_AV, O_IQ, O_IK, O_IW = 0, 512, 1024, 1536, 2048, 2080
O_GQ, O_GK, O_GV, O_GR, O_GLR, O_GA, O_GB = 2096, 2352, 2608, 3120, 3632, 3648, 4672
ALPHA = 2.0 ** 0.25
EPS = 1e-5
NEG = -1.0e30
TOKW = 4368
NFT = 14
NIT = 16
NT3 = NT
STOP = 0


class Sem:
    def __init__(self, h, is_dma=True):
        self.h = h
        self.val = 0
        self.is_dma = is_dma


class Buf:
    __slots__ = ("name", "last_w", "reads")

    def __init__(self, name):
        self.name = name
        self.last_w = None
        self.reads = {}


class Eng:
    def __init__(self, name, obj, sem):
        self.name = name
        self.obj = obj
        self.sem = sem
        self.seen = {}


class K:
    def __init__(self, nc, es):
        self.nc = nc
        self.es = es
        self.nsem = 0
        self.sems = []
        self.engs = {}
        for name, obj in (("pe", nc.tensor), ("act", nc.scalar), ("dve", nc.vector),
                          ("pool", nc.gpsimd), ("sp", nc.sync)):
            self.engs[name] = Eng(name, obj, self.new_sem("e_" + name))
            self.engs[name].sem.is_dma = False

    def new_sem(self, name):
        h = self.es.enter_context(self.nc.semaphore("s%d_%s" % (self.nsem, name)))
        self.nsem += 1
        s = Sem(h)
        self.sems.append(s)
        return s

    def _needs(self, E, r, w, skip_self=False):
        needs = {}

        def need(dep):
            if dep is None:
                return
            s, v = dep
            if skip_self and s is E.sem:
                return
            if s.is_dma:
                v = s.val
            if E.seen.get(s, 0) >= v:
                return
            if needs.get(s, 0) < v:
                needs[s] = v

        for b in r:
            need(b.last_w)
        for b in w:
            need(b.last_w)
            for s, v in b.reads.items():
                need((s, v))
        return needs

    def op(self, eng, fn, r=(), w=(), inc=True):
        E = self.engs[eng]
        needs = self._needs(E, r, w, skip_self=(eng == "pe"))
        items = list(needs.items())
        for s, v in items[:-1]:
            E.obj.wait_ge(s.h, v)
        ins = fn(E.obj)
        if items:
            s, v = items[-1]
            ins._wait_ge(s.h, v)
        for s, v in items:
            E.seen[s] = v
        if inc:
            E.sem.val += 1
            ins.then_inc(E.sem.h, 1)
            stamp = E.sem.val
        else:
            stamp = E.sem.val + 1
        for b in r:
            if b.reads.get(E.sem, 0) < stamp:
                b.reads[E.sem] = stamp
        for b in w:
            b.last_w = (E.sem, stamp)
            b.reads = {}
        return ins

    def dma(self, q, out, in_, sem, r=(), w=()):
        E = self.engs[q]
        needs = self._needs(E, r, w)
        for s, v in needs.items():
            E.obj.wait_ge(s.h, v)
            E.seen[s] = v
        ins = E.obj.dma_start(out=out, in_=in_)
        sem.val += 16
        ins.then_inc(sem.h, 16)
        for b in r:
            if b.reads.get(sem, 0) < sem.val:
                b.reads[sem] = sem.val
        for b in w:
            b.last_w = (sem, sem.val)
            b.reads = {}
        return ins

    def barrier(self):
        for E in self.engs.values():
            for s in self.sems:
                if s is E.sem:
                    continue
                if s.val > E.seen.get(s, 0):
                    E.obj.wait_ge(s.h, s.val)
                    E.seen[s] = s.val

    def final_wait(self, q, sems):
        E = self.engs[q]
        for s in sems:
            E.obj.wait_ge(s.h, s.val)


def build_nc(debug=None):
    nc = bass.Bass("TRN2", target_bir_lowering=False)
    dbg = {}

    def din(name, shape, dt=F32):
        return nc.dram_tensor(name, list(shape), dt, kind="ExternalInput").ap()

    x_d = din("x", [S, D])
    c_d = din("c_pl", [128, 8])
    w_ada_d = din("w_ada", [D, 6 * D])
    b_ada_pl_d = din("b_ada_pl", [128, 48])
    b_ada_row_d = din("b_ada_row", [1, 6 * D])
    w_in_d = din("w_in", [D, DP])
    rel_bias_d = din("rel_bias", [32, 8])
    ohpad_d = din("ohpad", [32, 384])
    gla_wg_d = din("gla_wg", [16, 256])
    gla_bg_d = din("gla_bg", [1, 256])
    gla_g_d = din("gla_g", [1, 512])
    out_d = nc.dram_tensor("out", [S, D], F32, kind="ExternalOutput").ap()
    w_ba_d = din("w_ba", [512, D])
    w_bb_d = din("w_bb", [512, D])
    w_o_d = din("w_o", [D, D])
    ln1_g_d = din("ln1_g", [1, D]); ln1_b_d = din("ln1_b", [1, D])
    ln2_g_d = din("ln2_g", [1, D]); ln2_b_d = din("ln2_b", [1, D])
    w_r_d = din("w_r", [D, 36]); b_r_d = din("b_r", [1, 36])
    w_eg_d = din("w_eg", [32, D, 256]); w_eu_d = din("w_eu", [32, D, 256]); w_ed_d = din("w_ed", [32, 256, D])
    x1_d = nc.dram_tensor("x1s", [S, D], F32, kind="Internal").ap()
    h2T_d = nc.dram_tensor("h2Ts", [8, 128, S], BF16, kind="Internal").ap()
    a2_d = nc.dram_tensor("a2", [8, 128, 384], F32, kind="Internal").ap()
    oaT_d = nc.dram_tensor("oaT", [4, 128, S], BF16, kind="Internal").ap()
    obT_d = nc.dram_tensor("obT", [4, 128, S], BF16, kind="Internal").ap()

    featT_d = nc.dram_tensor("featT", [NFT, 128, S], BF16, kind="Internal").ap()
    tok_d = nc.dram_tensor("tokm", [S, TOKW], BF16, kind="Internal").ap()

    if debug:
        dbg["hT"] = nc.dram_tensor("dbg_hT", [128, 8, S], BF16, kind="ExternalOutput").ap()
        dbg["featT"] = nc.dram_tensor("dbg_featT", [NFT, 128, S], BF16, kind="ExternalOutput").ap()
        dbg["tok"] = nc.dram_tensor("dbg_tok", [S, TOKW], BF16, kind="ExternalOutput").ap()
        dbg["ada"] = nc.dram_tensor("dbg_ada", [128, 32], F32, kind="ExternalOutput").ap()
        dbg["gt"] = nc.dram_tensor("dbg_gt", [128, 2048], F32, kind="ExternalOutput").ap()
        dbg["oaT"] = nc.dram_tensor("dbg_oaT", [4, 128, S], BF16, kind="ExternalOutput").ap()
        dbg["obT"] = nc.dram_tensor("dbg_obT", [4, 128, S], BF16, kind="ExternalOutput").ap()

    es = ExitStack()
    with es:
        k = K(nc, es)

        SB_LIMIT = 208 * 1024

        def _chk(name, t):
            m = nc.lookup_mloc(name)
            sz = 1
            for d_ in list(m.dims)[1:]:
                sz *= d_
            assert m.addr + sz <= SB_LIMIT, ("SBUF overflow", name, m.addr, sz)
            return t

        uid = [0]

        def sb(name, shape, dt):
            uid[0] += 1
            name = "%s_u%d" % (name, uid[0])
            return _chk(name, es.enter_context(nc.sbuf_tensor(name, list(shape), dt)))

        cur = [None]

        def lsb(name, shape, dt):
            uid[0] += 1
            name = "%s_u%d" % (name, uid[0])
            return _chk(name, cur[0].enter_context(nc.sbuf_tensor(name, list(shape), dt)))

        def phase_begin():
            cur[0] = ExitStack()

        def phase_end():
            k.barrier()
            cur[0].close()
            cur[0] = None

        def pst(name, shape, dt):
            return es.enter_context(nc.psum_tensor(name, list(shape), dt))

        ident = sb("ident", [128, 128], BF16)
        identf = sb("identf", [128, 128], F32)
        hT = sb("hT", [128, 8, S], BF16)
        adaP = sb("adaP", [128, 32], F32)
        gtB = sb("gtB", [128, 2048], F32)
        B_ident = Buf("ident")
        B_hT = [Buf("hT%d" % i) for i in range(NT)]
        B_adaP = Buf("adaP")
        B_gtB = Buf("gtB")

        ps = [pst("ps%d" % i, [128, 512], F32) for i in range(8)]
        B_ps = [Buf("ps%d" % i) for i in range(8)]

        k.op("pool", lambda e: e.memset(identf[:], 0.0), w=[B_ident])
        k.op("pool", lambda e: e.affine_select(out=identf[:], in_=identf[:], pattern=[[-1, 128]],
                                               compare_op=ALU.not_equal, fill=1.0, base=0,
                                               channel_multiplier=1), r=[B_ident], w=[B_ident])
        k.op("pool", lambda e: e.tensor_copy(out=ident[:], in_=identf[:]), r=[B_ident], w=[B_ident])

        ones1 = sb("ones1", [1, 128], BF16)
        phase_begin()
        c_sb = lsb("c_sb", [128, 8], F32)
        cond = lsb("cond", [128, 8], BF16)
        condB = lsb("condB", [128, 8, 128], BF16)
        bpl = lsb("bpl", [128, 48], F32)
        brow = lsb("brow", [1, 6 * D], F32)
        browb = lsb("browb", [1, 6 * D], BF16)
        wada = [lsb("wada%d" % i, [128, 8, 1024], BF16) for i in range(2)]
        B_c = Buf("c"); B_cond = Buf("cond"); B_bpl = Buf("bpl"); B_brow = Buf("brow")
        B_wada = [Buf("wada0"), Buf("wada1")]
        s_misc = k.new_sem("misc")
        s_wada = [k.new_sem("wada0"), k.new_sem("wada1")]
        k.dma("sp", c_sb[:], c_d[:, :], s_misc, w=[B_c])
        k.dma("sp", bpl[:], b_ada_pl_d[:, :], k.new_sem("misc2"), w=[B_bpl])
        k.dma("sp", brow[:], b_ada_row_d[:, :], k.new_sem("misc3"), w=[B_brow])
        k.op("act", lambda e: e.activation(out=cond[:], in_=c_sb[:], func=AF.Silu), r=[B_c], w=[B_cond])
        k.op("dve", lambda e: e.tensor_copy(out=condB[:], in_=cond[:].unsqueeze(2).broadcast_to([128, 8, 128])),
             r=[B_cond], w=[B_cond])
        k.op("dve", lambda e: e.tensor_copy(out=browb[:], in_=brow[:]), r=[B_brow], w=[B_brow])
        k.op("dve", lambda e: e.memset(ones1[:], 1.0), w=[B_brow])
        w_ada_v = w_ada_d.rearrange("(k p) j -> p k j", p=128)
        for pc in range(6):
            sl = pc % 2
            k.dma("pool", wada[sl][:], w_ada_v[:, :, pc * 1024:(pc + 1) * 1024], s_wada[sl], w=[B_wada[sl]])
            if pc in (2, 5):
                g = 0 if pc == 2 else 1
                for half in range(2):
                    bank = 2 + half
                    for kk in range(8):
                        k.op("pe", lambda e, kk=kk, half=half, bank=bank: e.matmul(
                            ps[bank][:], lhsT=condB[:, kk, :], rhs=wada[sl][:, kk, half * 512:(half + 1) * 512],
                            start=(kk == 0), stop=False), r=[B_cond, B_wada[sl]], w=[B_ps[bank]], inc=False)
                    k.op("pe", lambda e, half=half, bank=bank: e.matmul(
                        ps[bank][:], lhsT=ones1[:, :], rhs=browb[:, pc * 1024 + half * 512: pc * 1024 + (half + 1) * 512],
                        start=False, stop=True), r=[B_brow], w=[B_ps[bank]])
                    k.op("act", lambda e, half=half, bank=bank, g=g: e.activation(
                        out=gtB[:, g * 1024 + half * 512: g * 1024 + (half + 1) * 512], in_=ps[bank][:], func=AF.Identity),
                        r=[B_ps[bank]], w=[B_gtB])
            else:
                slot = {0: 0, 1: 1, 3: 2, 4: 3}[pc]
                for jc in range(8):
                    col = slot * 8 + jc
                    for kk in range(8):
                        k.op("pe", lambda e, kk=kk, jc=jc, col=col: e.matmul(
                            ps[0][:, col:col + 1], lhsT=wada[sl][:, kk, jc * 128:(jc + 1) * 128], rhs=cond[:, kk:kk + 1],
                            start=(kk == 0), stop=(kk == 7)), r=[B_cond, B_wada[sl]], w=[B_ps[0]], inc=(kk == 7))
                add1 = 1.0 if pc in (1, 4) else 0.0
                k.op("dve", lambda e, slot=slot, pc=pc, add1=add1: e.scalar_tensor_tensor(
                    out=adaP[:, slot * 8:(slot + 1) * 8], in0=ps[0][:, slot * 8:(slot + 1) * 8], scalar=add1,
                    in1=bpl[:, pc * 8:(pc + 1) * 8], op0=ALU.add, op1=ALU.add),
                    r=[B_ps[0], B_bpl], w=[B_adaP])

        phase_end()
        LN = {}

        def alloc_ln(n=2, nxn=None):
            nxn = n if nxn is None else nxn
            LN["xn"] = [lsb("xn%d" % i, [128, D], BF16) for i in range(nxn)]
            LN["st6"] = [lsb("st6_%d" % i, [128, 2, 6], F32) for i in range(n)]
            LN["mv"] = [lsb("mv%d" % i, [128, 4], F32) for i in range(n)]
            LN["B_xn"] = [Buf("xn%d" % i) for i in range(nxn)]
            LN["B_st"] = [Buf("st%d" % i) for i in range(n)]
            LN["B_mv"] = [Buf("mv%d" % i) for i in range(n)]

        phase_begin()
        NXS = 4
        xs = [lsb("xs%d" % i, [128, D], F32) for i in range(NXS)]
        B_xs = [Buf("xs%d" % i) for i in range(NXS)]
        s_xs = [k.new_sem("xs%d" % i) for i in range(NXS)]
        alloc_ln()
        psT = [ps[4][:].bitcast(BF16), ps[5][:].bitcast(BF16)]

        def ln_stats(xt, Bx, sl):
            st6, mv, B_st, B_mv = LN["st6"], LN["mv"], LN["B_st"], LN["B_mv"]
            for hh in range(2):
                k.op("dve", lambda e, hh=hh: e.bn_stats(out=st6[sl][:, hh, :], in_=xt[:, hh * 512:(hh + 1) * 512]),
                     r=[Bx], w=[B_st[sl]])
            k.op("dve", lambda e: e.bn_aggr(out=mv[sl][:, 0:2], in_=st6[sl][:].rearrange("p a b -> p (a b)")),
                 r=[B_st[sl]], w=[B_mv[sl]])
            k.op("act", lambda e: e.activation(out=mv[sl][:, 2:3], in_=mv[sl][:, 1:2], func=AF.Sqrt, bias=EPS),
                 r=[B_mv[sl]], w=[B_mv[sl]])
            k.op("dve", lambda e: e.reciprocal(out=mv[sl][:, 2:3], in_=mv[sl][:, 2:3]), r=[B_mv[sl]], w=[B_mv[sl]])
            k.op("dve", lambda e: e.scalar_tensor_tensor(out=mv[sl][:, 3:4], in0=mv[sl][:, 0:1], scalar=-1.0,
                                                         in1=mv[sl][:, 2:3], op0=ALU.mult, op1=ALU.mult),
                 r=[B_mv[sl]], w=[B_mv[sl]])

        def to_featT(i, xt, Bx, dst, Bdst, col0, scol, bcol, bank=None, slo=0):
            sl = i % 2 + slo
            xn, mv, B_xn, B_mv = LN["xn"], LN["mv"], LN["B_xn"], LN["B_mv"]
            ln_stats(xt, Bx, sl)
            yield
            k.op("act", lambda e: e.activation(out=xn[i % 2][:], in_=xt[:], func=AF.Identity,
                                               scale=mv[sl][:, 2:3], bias=mv[sl][:, 3:4]),
                 r=[Bx, B_mv[sl]], w=[B_xn[i % 2]])
            yield
            if bank is None:
                bank = 4 + i % 2
            psT_ = ps[bank][:].bitcast(BF16)
            for kk in range(8):
                k.op("pe", lambda e, kk=kk: e.transpose(out=psT_[:, kk * 128:(kk + 1) * 128],
                                                        in_=xn[i % 2][:, kk * 128:(kk + 1) * 128], identity=ident[:]),
                     r=[B_xn[i % 2], B_ident], w=[B_ps[bank]], inc=(kk == 7))
            for kk in range(8):
                if kk % 2 == 0:
                    k.op("act", lambda e, kk=kk: e.activation(
                        out=dst[:, kk, col0:col0 + 128], in_=psT_[:, kk * 128:(kk + 1) * 128], func=AF.Identity,
                        scale=adaP[:, scol + kk:scol + kk + 1], bias=adaP[:, bcol + kk:bcol + kk + 1]),
                        r=[B_ps[bank], B_adaP], w=[Bdst])
                else:
                    k.op("dve", lambda e, kk=kk: e.tensor_scalar(
                        out=dst[:, kk, col0:col0 + 128], in0=psT_[:, kk * 128:(kk + 1) * 128],
                        scalar1=adaP[:, scol + kk:scol + kk + 1], scalar2=adaP[:, bcol + kk:bcol + kk + 1],
                        op0=ALU.mult, op1=ALU.add), r=[B_ps[bank], B_adaP], w=[Bdst])

        def run_il(gens):
            live = list(gens)
            while live:
                for g in list(live):
                    try:
                        next(g)
                    except StopIteration:
                        live.remove(g)

        gs_ = []
        for i in range(NT + 2):
            if i < NT:
                sl = i % NXS
                k.dma("sp", xs[sl][:], x_d[i * 128:(i + 1) * 128, :], s_xs[sl], w=[B_xs[sl]])
                g = to_featT(i, xs[sl], B_xs[sl], hT, B_hT[i], i * 128, 8, 0)
                next(g)
                gs_.append(g)
            if 1 <= i <= NT:
                next(gs_[i - 1])
            if i >= 2:
                for _ in gs_[i - 2]:
                    pass

        if debug:
            s_dbg = k.new_sem("dbg")
            k.dma("sp", dbg["hT"][:, :, :], hT[:], s_dbg, r=B_hT)
            k.dma("sp", dbg["ada"][:, :], adaP[:], s_dbg, r=[B_adaP])
            k.dma("sp", dbg["gt"][:, :], gtB[:], s_dbg, r=[B_gtB])

        phase_end()
        fm = []
        for j in range(4): fm.append((j, O_AQ + j * 128, 128))
        for j in range(4): fm.append((4 + j, O_AK + j * 128, 128))
        fm.append((8, O_IK, 32))
        for j in range(2): fm.append((9 + j, O_GQ + j * 128, 128))
        for j in range(2): fm.append((11 + j, O_GK + j * 128, 128))
        fm.append((13, O_GLR, 16))
        tm = [(0, O_AV, 512), (512, O_IQ, 512), (1024, O_GV, 512), (1536, O_GR, 512), (2048, (O_GK, O_IW), 272),
              (2320, O_GA, 512), (2832, O_GA + 512, 512), (3344, O_GB, 512), (3856, O_GB + 512, 512)]
        w_in_v = w_in_d.rearrange("(k p) j -> p k j", p=128)
        vstack = ExitStack()
        v_aug = _chk("v_aug_p", vstack.enter_context(nc.sbuf_tensor("v_aug_p", [128, NT, 8, 65], BF16)))
        B_vt = [Buf("v_aug%d" % i) for i in range(NT)]
        k.op("pool", lambda e: e.memset(v_aug[:], 1.0), w=B_vt)
        phase_begin()
        wfm = [lsb("wfm%d" % i, [128, 8, 128], BF16) for i in range(2)]
        B_wfm = [Buf("wfm0"), Buf("wfm1")]
        s_wfm = [k.new_sem("wfm0"), k.new_sem("wfm1")]
        stg = [lsb("stg%d" % i, [128, S], BF16) for i in range(2)]
        B_stg = [Buf("stg0"), Buf("stg1")]
        s_stg = [k.new_sem("stg0"), k.new_sem("stg1")]
        B_featT = [Buf("featT%d" % i) for i in range(NFT)]
        ev = 0
        for n, (fi, c0, ncol) in enumerate(fm):
            sl = n % 2
            if ncol == 128:
                k.dma("pool", wfm[sl][:], w_in_v[:, :, c0:c0 + 128], s_wfm[sl], w=[B_wfm[sl]])
                M = 128
            elif ncol == 32:
                for rep in range(4):
                    k.dma("pool", wfm[sl][:, :, rep * 32:(rep + 1) * 32], w_in_v[:, :, c0:c0 + 32], s_wfm[sl], w=[B_wfm[sl]])
                M = 128
            else:
                k.dma("pool", wfm[sl][:, :, 0:16], w_in_v[:, :, c0:c0 + 16], s_wfm[sl], w=[B_wfm[sl]])
                M = 16
            for tb in range(8):
                bank = tb % 4
                for kk in range(8):
                    k.op("pe", lambda e, kk=kk, tb=tb, bank=bank, M=M: e.matmul(
                        ps[bank][0:M, :], lhsT=wfm[sl][:, kk, 0:M], rhs=hT[:, kk, tb * 512:(tb + 1) * 512],
                        start=(kk == 0), stop=(kk == 7)),
                        r=[B_wfm[sl]] + B_hT[tb * 4:(tb + 1) * 4], w=[B_ps[bank]], inc=(kk == 7))
                if ev % 2 == 0:
                    k.op("act", lambda e, tb=tb, bank=bank, M=M: e.activation(
                        out=stg[sl][0:M, tb * 512:(tb + 1) * 512], in_=ps[bank][0:M, :], func=AF.Identity),
                        r=[B_ps[bank]], w=[B_stg[sl]])
                else:
                    k.op("dve", lambda e, tb=tb, bank=bank, M=M: e.tensor_copy(
                        out=stg[sl][0:M, tb * 512:(tb + 1) * 512], in_=ps[bank][0:M, :]),
                        r=[B_ps[bank]], w=[B_stg[sl]])
                ev += 1
            k.dma("sp", featT_d[fi, 0:M, :], stg[sl][0:M, :], s_stg[sl], r=[B_stg[sl]], w=[B_featT[fi]])

        tm2 = [[(O_AV, 512)],
               [(O_IQ, 512), (O_GV, 512)],
               [(O_GR, 512), ((O_GK, O_IW), 272)],
               [(O_GA, 512), (O_GA + 512, 512)],
               [(O_GB, 512), (O_GB + 512, 512)]]
        tm2_t0 = [None, 512, 1536, 2320, 3344]
        wtm = [lsb("wtm%d" % i, [128, 8, 1024], BF16) for i in range(2)]
        B_wtm = [Buf("wtm0"), Buf("wtm1")]
        s_wtm = [k.new_sem("wtm0"), k.new_sem("wtm1")]
        NTS = 8
        tstg = [lsb("tstg%d" % i, [128, 1024], BF16) for i in range(NTS)]
        B_tstgA = [Buf("tstgA%d" % i) for i in range(NTS)]
        B_tstgB = [Buf("tstgB%d" % i) for i in range(NTS)]
        s_tstg = [k.new_sem("tstg%d" % i) for i in range(NTS)]
        B_tok = Buf("tok")
        cnt = 0

        def load_wtm(n):
            for pi_, (c0_, ncol_) in enumerate(tm2[n]):
                o_ = pi_ * 512
                if isinstance(c0_, tuple):
                    k.dma("pool", wtm[n % 2][:, :, o_:o_ + 256], w_in_v[:, :, c0_[0]:c0_[0] + 256], s_wtm[n % 2], w=[B_wtm[n % 2]])
                    k.dma("pool", wtm[n % 2][:, :, o_ + 256:o_ + 272], w_in_v[:, :, c0_[1]:c0_[1] + 16], s_wtm[n % 2], w=[B_wtm[n % 2]])
                else:
                    k.dma("pool", wtm[n % 2][:, :, o_:o_ + ncol_], w_in_v[:, :, c0_:c0_ + ncol_], s_wtm[n % 2], w=[B_wtm[n % 2]])

        load_wtm(0)
        for n, parts in enumerate(tm2):
            sl = n % 2
            if n + 1 < len(tm2):
                load_wtm(n + 1)
            for i in range(NT):
                b0 = 2 * (i % 4)
                for pi_, (c0, ncol) in enumerate(parts):
                    bank = b0 + pi_
                    o_ = pi_ * 512
                    for kk in range(8):
                        k.op("pe", lambda e, kk=kk, i=i, bank=bank, o_=o_, ncol=ncol: e.matmul(
                            ps[bank][:, 0:ncol], lhsT=hT[:, kk, i * 128:(i + 1) * 128], rhs=wtm[sl][:, kk, o_:o_ + ncol],
                            start=(kk == 0), stop=(kk == 7)),
                            r=[B_wtm[sl], B_hT[i]], w=[B_ps[bank]], inc=(kk == 7))
                if n == 0:
                    v_out = v_aug[:, i, :, 0:64]
                    v_in = ps[b0][:, 0:512].rearrange("p (h d) -> p h d", h=8)
                    if cnt % 2 == 0:
                        k.op("act", lambda e, v_out=v_out, v_in=v_in: e.activation(out=v_out, in_=v_in, func=AF.Identity),
                             r=[B_ps[b0]], w=[B_vt[i]])
                    else:
                        k.op("dve", lambda e, v_out=v_out, v_in=v_in: e.tensor_copy(out=v_out, in_=v_in),
                             r=[B_ps[b0]], w=[B_vt[i]])
                    cnt += 1
                    continue
                ts = cnt % NTS
                nc1 = parts[1][1]
                k.op("act", lambda e, b0=b0, ts=ts: e.activation(out=tstg[ts][:, 0:512], in_=ps[b0][:, 0:512], func=AF.Identity),
                     r=[B_ps[b0]], w=[B_tstgA[ts]])
                k.op("dve", lambda e, b0=b0, ts=ts, nc1=nc1: e.tensor_copy(out=tstg[ts][:, 512:512 + nc1], in_=ps[b0 + 1][:, 0:nc1]),
                     r=[B_ps[b0 + 1]], w=[B_tstgB[ts]])
                t0 = tm2_t0[n]
                k.dma("sp" if cnt % 2 == 0 else "pool", tok_d[i * 128:(i + 1) * 128, t0:t0 + 512 + nc1], tstg[ts][:, 0:512 + nc1], s_tstg[ts],
                      r=[B_tstgA[ts], B_tstgB[ts]], w=[B_tok])
                cnt += 1

        phase_end()
        phase_begin()
        BIG = hT
        kT = BIG[:, 0:4, :]
        ikT = BIG[:, 4, :]
        A = BIG[:, 5:7, :].rearrange("p a b -> p (a b)").bitcast(F32)
        msk = BIG[:, 7, :]
        B_kc = Buf("kcache"); B_A = Buf("A"); B_msk = Buf("msk")
        B_v = Buf("v_aug")
        maskT0 = lsb("maskT", [128, NT, 128], BF16)
        NBTh = lsb("NBTh", [128, 2, 8, 128], BF16)
        NBTl = lsb("NBTl", [128, 2, 8, 128], BF16)
        cB = lsb("cB", [128, 8], F32)
        c8 = lsb("c8", [128, 8], F32)
        B_NBT = Buf("NBT")
        tri_in = lsb("tri_in", [128, 128], BF16)
        tri_st = lsb("tri_st", [128, 128], BF16)
        trif = lsb("trif", [128, 128], F32)
        B_tri = Buf("tri")
        wg = lsb("wg", [16, 256], BF16)
        bg = lsb("bg", [1, 256], BF16)
        wgf = lsb("wgf", [16, 256], F32)
        bgf = lsb("bgf", [1, 256], F32)
        gB = lsb("gB", [128, 512], F32)
        B_gc = Buf("glaconst")
        pw = lsb("pw", [128, NIT + 1], F32)
        B_pw = Buf("pw")
        s_c = k.new_sem("p3c")
        s_c2 = k.new_sem("p3c2")
        psb = [p[:].bitcast(BF16) for p in ps]

        s_cp = k.new_sem("p3cp")
        for j in range(4):
            k.dma(("sp", "act", "pool", "act")[j], kT[:, j, :], featT_d[4 + j, :, :], s_cp if j == 2 else s_c, r=[B_featT[4 + j]], w=[B_kc])
        k.dma("sp", ikT, featT_d[8, :, :], s_c, r=[B_featT[8]], w=[B_kc])
        k.op("pool", lambda e: e.memset(trif[:], 1.0), w=[B_tri])
        k.op("pool", lambda e: e.affine_select(out=trif[:], in_=trif[:], pattern=[[1, 128]], compare_op=ALU.is_ge,
                                               fill=0.0, base=0, channel_multiplier=-1), r=[B_tri], w=[B_tri])
        k.op("pool", lambda e: e.tensor_copy(out=tri_in[:], in_=trif[:]), r=[B_tri], w=[B_tri])
        k.op("pool", lambda e: e.memset(trif[:], 1.0), r=[B_tri], w=[B_tri])
        k.op("pool", lambda e: e.affine_select(out=trif[:], in_=trif[:], pattern=[[-1, 128]], compare_op=ALU.is_gt,
                                               fill=0.0, base=0, channel_multiplier=1), r=[B_tri], w=[B_tri])
        k.op("pool", lambda e: e.tensor_copy(out=tri_st[:], in_=trif[:]), r=[B_tri], w=[B_tri])
        for t in range(NIT + 1):
            k.op("pool", lambda e, t=t: e.memset(pw[:, t:t + 1], 2.0 ** -(t + 1)), w=[B_pw])
        k.dma("sp", wgf[:], gla_wg_d[:, :], s_c, w=[B_gc])
        k.dma("sp", bgf[:], gla_bg_d[:, :], s_c, w=[B_gc])
        k.dma("sp", gB[:], gla_g_d[0:1, :].broadcast_to([128, 512]), s_c, w=[B_gc])
        k.op("dve", lambda e: e.tensor_copy(out=wg[:], in_=wgf[:]), r=[B_gc], w=[B_gc])
        k.op("dve", lambda e: e.tensor_copy(out=bg[:], in_=bgf[:]), r=[B_gc], w=[B_gc])
        ph2 = ExitStack()
        rb = ph2.enter_context(nc.sbuf_tensor("rb", [32, 8], F32))
        rbB = ph2.enter_context(nc.sbuf_tensor("rbB", [32, 8, 128], F32))
        oh = ph2.enter_context(nc.sbuf_tensor("oh", [32, 384], F32))
        Rrep = ph2.enter_context(nc.sbuf_tensor("Rrep", [128, 8, 384], F32))
        NBT = ph2.enter_context(nc.sbuf_tensor("NBTf", [128, 2, 8, 128], F32))
        B_rb = Buf("rb"); B_Rrep = Buf("Rrep"); B_A2 = Buf("A2")
        k.dma("sp", rb[:], rel_bias_d[:, :], s_c2, w=[B_rb])
        k.dma("sp", oh[:], ohpad_d[:, :], s_c2, w=[B_rb])
        k.op("dve", lambda e: e.tensor_copy(out=rbB[:], in_=rb[:].unsqueeze(2).broadcast_to([32, 8, 128])), r=[B_rb], w=[B_rb])
        for h in range(8):
            bank = h % 2
            k.op("pe", lambda e, h=h, bank=bank: e.matmul(ps[bank][:, 0:384], lhsT=rbB[:, h, :], rhs=oh[:, :], start=True, stop=True),
                 r=[B_rb], w=[B_ps[bank]])
            k.op("act", lambda e, h=h, bank=bank: e.activation(out=Rrep[:, h, :], in_=ps[bank][:, 0:384], func=AF.Identity),
                 r=[B_ps[bank]], w=[B_Rrep])
        k.dma("sp", a2_d.rearrange("h p m -> p h m"), Rrep[:], s_c2, r=[B_Rrep], w=[B_A2])
        for t in range(2):
            src = bass.AP(tensor=a2_d.tensor, offset=128 + 128 * t, ap=[[383, 128], [128 * 384, 8], [1, 128]])
            k.dma("sp", NBT[:, t, :, :], src, s_c2, r=[B_A2], w=[B_NBT])
        k.op("dve", lambda e: e.tensor_copy(out=c8[:], in_=Rrep[:, :, 383]), r=[B_Rrep], w=[B_NBT])
        k.op("dve", lambda e: e.tensor_scalar(out=cB[:], in0=c8[:], scalar1=0.125, scalar2=None, op0=ALU.mult), r=[B_NBT], w=[B_NBT])
        for t in range(2):
            for h in range(8):
                k.op("dve", lambda e, t=t, h=h: e.tensor_scalar(out=NBT[:, t, h, :], in0=NBT[:, t, h, :], scalar1=c8[:, h:h + 1],
                                                                scalar2=None, op0=ALU.subtract), r=[B_NBT], w=[B_NBT])
        k.op("dve", lambda e: e.tensor_copy(out=NBTh[:].rearrange("p a b c -> p (a b c)"), in_=NBT[:].rearrange("p a b c -> p (a b c)")),
             r=[B_NBT], w=[B_NBT])
        k.op("dve", lambda e: e.tensor_tensor(out=NBTl[:].rearrange("p a b c -> p (a b c)"), in0=NBT[:].rearrange("p a b c -> p (a b c)"),
                                              in1=NBTh[:].rearrange("p a b c -> p (a b c)"), op=ALU.subtract), r=[B_NBT], w=[B_NBT])
        k.barrier()
        ph2.close()

        nt3 = NT3
        maskT = [maskT0, lsb("maskT1", [128, NT, 128], BF16)]
        B_maskT = [Buf("maskT0"), Buf("maskT1")]
        qT_t = [lsb("qT_t%d" % i, [128, 4, 128], BF16) for i in range(2)]
        gT_t = [lsb("gT_t%d" % i, [128, 4, 128], BF16) for i in range(2)]
        glr_t = [lsb("glr_t%d" % i, [16, 128], BF16) for i in range(2)]
        tok_t = [lsb("tok_t%d" % i, [128, 1808], BF16) for i in range(2)]
        B_ld = [Buf("ld0"), Buf("ld1")]
        s_ld = [k.new_sem("ld0"), k.new_sem("ld1")]
        wabs = lsb("wabs", [128, 16], F32)
        sgn = lsb("sgn", [128, 16], F32)
        sgnD = lsb("sgnD", [128, 16, 128], BF16)
        iqs = lsb("iqs", [128, 512], BF16)
        iqT = lsb("iqT", [128, 4, 128], BF16)
        B_iq = Buf("iq"); B_iqT = Buf("iqT"); B_sgnD = Buf("sgnD")
        NRH = 8
        Rh = [lsb("Rh%d" % i, [128, 512], BF16) for i in range(NRH)]
        B_Rh = [Buf("Rh%d" % i) for i in range(NRH)]
        bis = lsb("bis", [128, 8], F32)
        nmid = lsb("nmid", [128, NIT + 1], F32)
        whs = lsb("whs", [128, NIT + 1], F32)
        whs2 = lsb("whs2", [128, NIT + 1], F32)
        mid = lsb("mid", [128, NIT + 1], F32)
        B_bis = Buf("bis"); B_bisA = Buf("bisA"); B_bisD = Buf("bisD"); B_msk2 = Buf("msk2")
        expS = [lsb("expS%d" % i, [128, 512], BF16) for i in range(2)]
        B_expS = [Buf("expS0"), Buf("expS1")]
        PT = [lsb("PT%d" % i, [128, 512], BF16) for i in range(2)]
        B_PT = [Buf("PT0"), Buf("PT1")]
        rec = lsb("rec", [128, 8], F32)
        o_a = lsb("o_a", [128, 8, 64], BF16)
        o_aT = [lsb("o_aT%d" % i, [128, 4, 128], BF16) for i in range(2)]
        B_oa = Buf("o_a"); B_oaT = [Buf("o_aT0"), Buf("o_aT1")]
        s_oaT = [k.new_sem("oaT0"), k.new_sem("oaT1")]
        B_oaTd = Buf("oaTd"); B_obTd = Buf("obTd")
        ge_ = lsb("g_e", [128, 256], F32)
        lgf = lsb("g_lgf", [128, 256], F32)
        lgh = lsb("g_lgh", [128, 256], BF16)
        lgl = lsb("g_lgl", [128, 256], BF16)
        ET = lsb("g_ET", [128, 3, 256], F32)
        E2 = lsb("g_E2", [128, 256], F32)
        qin = lsb("g_qin", [128, 2, 128], BF16)
        kst = lsb("g_kst", [128, 2, 128], BF16)
        qrl = lsb("g_qrl", [128, 2, 128], BF16)
        kstk = lsb("g_kstk", [128, 256], BF16)
        attT = lsb("g_attT", [128, 4, 128], BF16)
        Sst = lsb("g_S", [128, 2, 128], F32)
        Sb = lsb("g_Sb", [128, 2, 128], BF16)
        gst = lsb("g_st", [128, 4, 6], F32)
        gmv = lsb("g_mv", [128, 4, 2], F32)
        grs = lsb("g_rs", [128, 8], F32)
        on = lsb("g_on", [128, 512], F32)
        sg = lsb("g_sg", [128, 512], F32)
        ob = lsb("g_ob", [128, 512], BF16)
        o_bT = [lsb("o_bT%d" % i, [128, 4, 128], BF16) for i in range(2)]
        B_g1 = Buf("g1"); B_lg = Buf("lg"); B_ET = Buf("ET"); B_gq = Buf("gq"); B_att = Buf("att")
        B_S = Buf("S"); B_Sb = Buf("Sb"); B_gs = Buf("gs"); B_on = Buf("on"); B_ob = Buf("ob")
        B_obT = [Buf("o_bT0"), Buf("o_bT1")]
        s_obT = [k.new_sem("obT0"), k.new_sem("obT1")]
        k.op("dve", lambda e: e.memset(Sst[:], 0.0), w=[B_S])
        k.op("dve", lambda e: e.memset(Sb[:], 0.0), w=[B_Sb])
        WC = (16.0 ** -0.5) * (32.0 ** -0.5)
        evc = [0]

        def stage_idx(i):
            sl = i % 2
            n = (i + 1) * 128
            k.dma("sp", qT_t[sl][:], featT_d[0:4, :, i * 128:(i + 1) * 128].rearrange("c p t -> p c t"), s_ld[sl],
                  r=B_featT[0:4], w=[B_ld[sl]])
            k.dma("sp", gT_t[sl][:], featT_d[9:13, :, i * 128:(i + 1) * 128].rearrange("c p t -> p c t"), s_ld[sl],
                  r=B_featT[9:13], w=[B_ld[sl]])
            k.dma("sp", glr_t[sl][:], featT_d[13, 0:16, i * 128:(i + 1) * 128], s_ld[sl], r=[B_featT[13]], w=[B_ld[sl]])
            k.dma("sp", tok_t[sl][:], tok_d[i * 128:(i + 1) * 128, 512:2320], s_ld[sl], r=[B_tok], w=[B_ld[sl]])
            tk = tok_t[sl]
            iq_v = tk[:, 0:512]; iw_v = tk[:, 1792:1808]
            k.op("act", lambda e: e.activation(out=wabs[:], in_=iw_v, func=AF.Abs, scale=WC), r=[B_ld[sl]], w=[B_iq])
            k.op("dve", lambda e: e.tensor_scalar(out=sgn[:], in0=iw_v, scalar1=0.0, scalar2=2.0, op0=ALU.is_gt, op1=ALU.mult),
                 r=[B_ld[sl]], w=[B_iq])
            k.op("dve", lambda e: e.tensor_scalar(out=sgn[:], in0=sgn[:], scalar1=-1.0, scalar2=None, op0=ALU.add), r=[B_iq], w=[B_iq])
            k.op("dve", lambda e: e.tensor_tensor(out=sgnD[:], in0=ident[:].unsqueeze(1).broadcast_to([128, 16, 128]),
                                                  in1=sgn[:].unsqueeze(2).broadcast_to([128, 16, 128]), op=ALU.mult),
                 r=[B_iq, B_ident], w=[B_sgnD])
            k.op("dve", lambda e: e.tensor_tensor(out=iqs[:].rearrange("p (h d) -> p h d", h=16),
                                                  in0=iq_v.rearrange("p (h d) -> p h d", h=16),
                                                  in1=wabs[:].unsqueeze(2).broadcast_to([128, 16, 32]), op=ALU.mult),
                 r=[B_ld[sl], B_iq], w=[B_iq])
            for c in range(4):
                k.op("pe", lambda e, c=c: e.transpose(out=psb[3][:, c * 128:(c + 1) * 128], in_=iqs[:, c * 128:(c + 1) * 128],
                                                      identity=ident[:]), r=[B_iq, B_ident], w=[B_ps[3]], inc=(c == 3))
            k.op("act", lambda e: e.activation(out=iqT[:].rearrange("p c t -> p (c t)"), in_=psb[3][:, 0:512], func=AF.Identity),
                 r=[B_ps[3]], w=[B_iqT])
            nkb = (i + 4) // 4
            for kb in range(nkb):
                k0 = kb * 512
                nk = min(512, n - k0)
                sbank = 4 + kb % 2
                pend = []
                for c in range(4):
                    for hh in range(4):
                        pb = 32 * hh
                        k.op("pe", lambda e, c=c, pb=pb, hh=hh: e.matmul(
                            ps[hh][:, 0:nk], lhsT=iqT[pb:pb + 32, c, :], rhs=ikT[pb:pb + 32, k0:k0 + nk], start=True, stop=True,
                            tile_position=(pb, 0)), r=[B_iqT, B_kc], w=[B_ps[hh]])
                    for (ph_, prs) in pend:
                        k.op("pe", lambda e, ph_=ph_, prs=prs: e.matmul(ps[sbank][:, 0:nk], lhsT=sgnD[:, ph_, :], rhs=Rh[prs][:, 0:nk],
                                                                        start=(ph_ == 0), stop=False),
                             r=[B_sgnD, B_Rh[prs]], w=[B_ps[sbank]], inc=False)
                    pend = []
                    for hh in range(4):
                        h = c * 4 + hh
                        rs = (c % 2) * 4 + hh
                        if hh < 2:
                            k.op("act", lambda e, hh=hh, rs=rs: e.activation(out=Rh[rs][:, 0:nk], in_=ps[hh][:, 0:nk], func=AF.Relu),
                                 r=[B_ps[hh]], w=[B_Rh[rs]])
                        else:
                            k.op("dve", lambda e, hh=hh, rs=rs: e.tensor_scalar(out=Rh[rs][:, 0:nk], in0=ps[hh][:, 0:nk], scalar1=0.0,
                                                                                scalar2=None, op0=ALU.max), r=[B_ps[hh]], w=[B_Rh[rs]])
                        pend.append((h, rs))
                for (ph_, prs) in pend:
                    k.op("pe", lambda e, ph_=ph_, prs=prs: e.matmul(ps[sbank][:, 0:nk], lhsT=sgnD[:, ph_, :], rhs=Rh[prs][:, 0:nk],
                                                                    start=False, stop=(ph_ == 15)),
                         r=[B_sgnD, B_Rh[prs]], w=[B_ps[sbank]], inc=(ph_ == 15))
                if kb % 2 == 0:
                    k.op("act", lambda e, sbank=sbank: e.activation(out=A[:, k0:k0 + nk], in_=ps[sbank][:, 0:nk], func=AF.Identity),
                         r=[B_ps[sbank]], w=[B_A])
                else:
                    k.op("dve", lambda e, sbank=sbank: e.tensor_copy(out=A[:, k0:k0 + nk], in_=ps[sbank][:, 0:nk]),
                         r=[B_ps[sbank]], w=[B_A])

        def stage_bis(i):
            n = (i + 1) * 128
            mp = i % 2
            k.op("pool", lambda e: e.affine_select(out=A[:, i * 128:n], in_=A[:, i * 128:n], pattern=[[-1, 128]],
                                                   compare_op=ALU.is_ge, fill=NEG, base=0, channel_multiplier=1),
                 r=[B_A], w=[B_A])
            if i < 2:
                k.op("dve", lambda e: e.memset(bis[:, 0:1], -1.0e29), w=[B_bis])
            else:
                k.op("dve", lambda e: e.tensor_reduce(out=bis[:, 5:6], in_=A[:, 0:i * 128], axis=AX.X, op=ALU.max,
                                                      apply_absolute_value=True), r=[B_A], w=[B_bis])
                k.op("dve", lambda e: e.tensor_reduce(out=bis[:, 4:5], in_=A[:, 0:n], axis=AX.X, op=ALU.max),
                     r=[B_A], w=[B_bis])
                yield
                k.op("dve", lambda e: e.scalar_tensor_tensor(out=bis[:, 4:5], in0=bis[:, 4:5], scalar=2.0, in1=bis[:, 5:6],
                                                             op0=ALU.add, op1=ALU.add), r=[B_bis], w=[B_bis])
                k.op("dve", lambda e: e.tensor_scalar(out=whs[:], in0=pw[:], scalar1=bis[:, 4:5], scalar2=None, op0=ALU.mult),
                     r=[B_bis, B_pw], w=[B_bis])
                k.op("dve", lambda e: e.tensor_scalar(out=whs2[:], in0=whs[:], scalar1=2.0, scalar2=None, op0=ALU.mult),
                     r=[B_bis], w=[B_bis])
                k.op("dve", lambda e: e.scalar_tensor_tensor(out=nmid[:, 0:1], in0=bis[:, 5:6], scalar=1.0, in1=whs[:, 0:1],
                                                             op0=ALU.add, op1=ALU.subtract), r=[B_bis], w=[B_bis])
                k.op("dve", lambda e: e.tensor_scalar(out=mid[:, 0:1], in0=nmid[:, 0:1], scalar1=-1.0, scalar2=None, op0=ALU.mult),
                     r=[B_bis], w=[B_bis])
                for t in range(NIT):
                    k.op("dve", lambda e, t=t: e.tensor_scalar(out=msk[:, 0:n], in0=A[:, 0:n], scalar1=mid[:, t:t + 1], scalar2=None,
                                                               op0=ALU.is_ge, op1=ALU.add, accum_out=bis[:, 3:4]),
                         r=[B_A, B_bis], w=[B_msk, B_bis])
                    k.op("dve", lambda e, t=t: e.tensor_scalar(out=bis[:, 7:8], in0=bis[:, 3:4], scalar1=255.5, scalar2=whs2[:, t + 1:t + 2],
                                                               op0=ALU.is_ge, op1=ALU.mult), r=[B_bis], w=[B_bis])
                    k.op("dve", lambda e, t=t: e.scalar_tensor_tensor(out=mid[:, t + 1:t + 2], in0=bis[:, 7:8], scalar=mid[:, t:t + 1],
                                                                      in1=whs[:, t + 1:t + 2], op0=ALU.add, op1=ALU.subtract),
                         r=[B_bis], w=[B_bis])
                    yield
                k.op("dve", lambda e: e.tensor_tensor(out=bis[:, 0:1], in0=mid[:, NIT:NIT + 1], in1=whs[:, NIT:NIT + 1], op=ALU.subtract),
                     r=[B_bis], w=[B_bis])
            k.op("dve", lambda e: e.tensor_scalar(out=msk[:, 0:n], in0=A[:, 0:n], scalar1=bis[:, 0:1], scalar2=None, op0=ALU.is_ge),
                 r=[B_A, B_bis], w=[B_msk, B_msk2])
            yield
            for g0 in range(0, i + 1, 8):
                g1 = min(g0 + 8, i + 1)
                for j in range(g0, g1):
                    k.op("pe", lambda e, j=j, g0=g0: e.transpose(out=psb[3][:, (j - g0) * 128:(j - g0 + 1) * 128],
                                                                 in_=msk[:, j * 128:(j + 1) * 128], identity=ident[:]),
                         r=[B_msk, B_ident], w=[B_ps[3]], inc=(j == g1 - 1))
                k.op("act", lambda e, g0=g0, g1=g1: e.activation(out=maskT[mp][:, g0:g1, :].rearrange("p j q -> p (j q)"),
                                                                 in_=psb[3][:, 0:(g1 - g0) * 128], func=AF.Identity),
                     r=[B_ps[3]], w=[B_maskT[mp]])
                yield

        def stage_attn(i):
            sl = i % 2
            mp = i % 2
            cntS = 0

            def emit_pv(h, g0, g1, es_):
                obank = 4 + h // 4
                for j in range(g0, g1):
                    k.op("pe", lambda e, j=j: e.matmul(
                        ps[obank][:, (h % 4) * 65:(h % 4) * 65 + 65], lhsT=PT[es_][:, (j - g0) * 128:(j - g0 + 1) * 128],
                        rhs=v_aug[:, j, h, :], start=(j == 0), stop=(j == i)),
                        r=[B_PT[es_], B_v], w=[B_ps[obank]], inc=(j == g1 - 1))

            prev = None
            for h in range(8):
                c = h // 2; pb = 64 * (h % 2)
                for g0 in range(0, i + 1, 4):
                    g1 = min(g0 + 4, i + 1)
                    ng = g1 - g0
                    bank = cntS % 3
                    es_ = cntS % 2
                    cntS += 1
                    for j in range(g0, g1):
                        near = j >= i - 1
                        k.op("pe", lambda e, j=j, g0=g0, bank=bank, near=near: e.matmul(
                            ps[bank][:, (j - g0) * 128:(j - g0 + 1) * 128], lhsT=kT[pb:pb + 64, c, j * 128:(j + 1) * 128],
                            rhs=qT_t[sl][pb:pb + 64, c, :], start=True, stop=(not near)),
                            r=[B_kc, B_ld[sl]], w=[B_ps[bank]], inc=(j == g1 - 1 and not near))
                        if near:
                            t = 0 if j == i else 1
                            k.op("pe", lambda e, j=j, g0=g0, bank=bank, t=t: e.matmul(
                                ps[bank][:, (j - g0) * 128:(j - g0 + 1) * 128], lhsT=ident[:, :], rhs=NBTh[:, t, h, :],
                                start=False, stop=False), r=[B_ident, B_NBT], w=[B_ps[bank]], inc=False)
                            k.op("pe", lambda e, j=j, g0=g0, bank=bank, t=t: e.matmul(
                                ps[bank][:, (j - g0) * 128:(j - g0 + 1) * 128], lhsT=ident[:, :], rhs=NBTl[:, t, h, :],
                                start=False, stop=True), r=[B_ident, B_NBT], w=[B_ps[bank]], inc=(j == g1 - 1))
                    k.op("act", lambda e, bank=bank, es_=es_, ng=ng: e.activation(
                        out=expS[es_][:, 0:ng * 128], in_=ps[bank][:, 0:ng * 128], func=AF.Exp, scale=0.125, bias=cB[:, h:h + 1]),
                        r=[B_ps[bank], B_NBT], w=[B_expS[es_]])
                    k.op("dve" if (i == nt3 - 1 and cntS % 2 == 0) else "pool", lambda e, es_=es_, ng=ng, g0=g0, g1=g1: e.tensor_tensor(
                        out=PT[es_][:, 0:ng * 128], in0=expS[es_][:, 0:ng * 128],
                        in1=maskT[mp][:, g0:g1, :].rearrange("p j q -> p (j q)"), op=ALU.mult),
                        r=[B_expS[es_], B_maskT[mp]], w=[B_PT[es_]])
                    if prev is not None:
                        emit_pv(*prev)
                    prev = (h, g0, g1, es_)
                    yield
            emit_pv(*prev)
            for hb in range(2):
                pv = ps[4 + hb][:, 0:260].rearrange("p (h d) -> p h d", h=4)
                k.op("dve", lambda e, hb=hb, pv=pv: e.reciprocal(out=rec[:, hb * 4:(hb + 1) * 4], in_=pv[:, :, 64]),
                     r=[B_ps[4 + hb]], w=[B_oa])
                k.op("dve", lambda e, hb=hb, pv=pv: e.tensor_tensor(
                    out=o_a[:, hb * 4:(hb + 1) * 4, :], in0=pv[:, :, 0:64],
                    in1=rec[:, hb * 4:(hb + 1) * 4].unsqueeze(2).broadcast_to([128, 4, 64]), op=ALU.mult),
                    r=[B_ps[4 + hb], B_oa], w=[B_oa])
            oa2 = o_a[:].rearrange("p h d -> p (h d)")
            for c in range(4):
                k.op("pe", lambda e, c=c: e.transpose(out=psb[3][:, c * 128:(c + 1) * 128], in_=oa2[:, c * 128:(c + 1) * 128],
                                                      identity=ident[:]), r=[B_oa, B_ident], w=[B_ps[3]], inc=(c == 3))
            k.op("act", lambda e: e.activation(out=o_aT[sl][:].rearrange("p c t -> p (c t)"), in_=psb[3][:, 0:512], func=AF.Identity),
                 r=[B_ps[3]], w=[B_oaT[sl]])
            k.dma("pool", oaT_d[:, :, i * 128:(i + 1) * 128].rearrange("c p t -> p c t"), o_aT[sl][:], s_oaT[sl],
                  r=[B_oaT[sl]], w=[B_oaTd])
            yield

        def stage_gla(i):
            sl = i % 2
            tk = tok_t[sl]
            gv_v = tk[:, 512:1024]; gr_v = tk[:, 1024:1536]; gk_v = tk[:, 1536:1792]
            gq_v = gT_t[sl][:, 0:2, :]
            gkT_v = gT_t[sl][:, 2:4, :]
            k.op("pe", lambda e: e.matmul(ps[6][:, 0:256], lhsT=glr_t[sl][:, :], rhs=wg[:, :], start=True, stop=False),
                 r=[B_ld[sl], B_gc], w=[B_ps[6]], inc=False)
            k.op("pe", lambda e: e.matmul(ps[6][:, 0:256], lhsT=ones1[:, :], rhs=bg[:, :], start=False, stop=True),
                 r=[B_gc], w=[B_ps[6]])
            k.op("act", lambda e: e.activation(out=ge_[:], in_=ps[6][:, 0:256], func=AF.Exp, scale=-1.0), r=[B_ps[6]], w=[B_g1])
            k.op("act", lambda e: e.activation(out=ge_[:], in_=ge_[:], func=AF.Ln, bias=1.0), r=[B_g1], w=[B_g1])
            k.op("dve", lambda e: e.tensor_scalar(out=lgf[:], in0=ge_[:], scalar1=-1.0 / 16.0, scalar2=None, op0=ALU.mult),
                 r=[B_g1], w=[B_lg])
            k.op("dve", lambda e: e.tensor_copy(out=lgh[:], in_=lgf[:]), r=[B_lg], w=[B_lg])
            k.op("dve", lambda e: e.tensor_tensor(out=lgl[:], in0=lgf[:], in1=lgh[:], op=ALU.subtract), r=[B_lg], w=[B_lg])
            yield
            for fc in range(2):
                for pi, part in enumerate((lgh, lgl)):
                    k.op("pe", lambda e, fc=fc, part=part, pi=pi: e.matmul(
                        ps[7][:, fc * 128:(fc + 1) * 128], lhsT=part[:, fc * 128:(fc + 1) * 128], rhs=tri_in[:, :],
                        start=(pi == 0), stop=(pi == 1)), r=[B_lg, B_tri], w=[B_ps[7]], inc=False)
                for pi, part in enumerate((lgh, lgl)):
                    k.op("pe", lambda e, fc=fc, part=part, pi=pi: e.matmul(
                        ps[7][:, 256 + fc * 128:256 + (fc + 1) * 128], lhsT=part[:, fc * 128:(fc + 1) * 128], rhs=tri_st[:, :],
                        start=(pi == 0), stop=(pi == 1)), r=[B_lg, B_tri], w=[B_ps[7]], inc=False)
            for pi, part in enumerate((lgh, lgl)):
                k.op("pe", lambda e, part=part, pi=pi: e.matmul(
                    ps[6][:, 256:512], lhsT=tri_st[:, :], rhs=part[:, :], start=(pi == 0), stop=(pi == 1)),
                    r=[B_lg, B_tri], w=[B_ps[6], B_ps[7]], inc=(pi == 1))
            k.op("act", lambda e: e.activation(out=ET[:, 0, :], in_=ps[7][:, 0:256], func=AF.Exp), r=[B_ps[7]], w=[B_ET])
            k.op("act", lambda e: e.activation(out=ET[:, 1, :], in_=ps[7][:, 256:512], func=AF.Exp), r=[B_ps[7]], w=[B_ET])
            k.op("act", lambda e: e.activation(out=ET[:, 2, :], in_=ps[7][:, 256:512], func=AF.Exp, scale=-1.0), r=[B_ps[7]], w=[B_ET])
            k.op("act", lambda e: e.activation(out=E2[:], in_=ps[6][:, 256:512], func=AF.Exp), r=[B_ps[6]], w=[B_ET])
            yield
            gq2 = gq_v.rearrange("p c t -> p (c t)")
            gk2 = gkT_v.rearrange("p c t -> p (c t)")
            k.op("dve", lambda e: e.scalar_tensor_tensor(out=qin[:].rearrange("p c t -> p (c t)"), in0=gq2, scalar=0.125, in1=ET[:, 0, :],
                                                         op0=ALU.mult, op1=ALU.mult), r=[B_ld[sl], B_ET], w=[B_gq])
            k.op("dve", lambda e: e.tensor_tensor(out=kst[:].rearrange("p c t -> p (c t)"), in0=gk2, in1=ET[:, 1, :], op=ALU.mult),
                 r=[B_ld[sl], B_ET], w=[B_gq])
            k.op("dve", lambda e: e.scalar_tensor_tensor(out=qrl[:].rearrange("p c t -> p (c t)"), in0=gq2, scalar=0.125, in1=ET[:, 2, :],
                                                         op0=ALU.mult, op1=ALU.mult), r=[B_ld[sl], B_ET], w=[B_gq])
            k.op("dve", lambda e: e.tensor_tensor(out=kstk[:], in0=gk_v, in1=E2[:], op=ALU.mult), r=[B_ld[sl], B_ET], w=[B_gq])
            yield
            for h in range(4):
                fc = h // 2; pb = 64 * (h % 2)
                abank = 6 if h % 2 == 0 else 7
                k.op("pe", lambda e, h=h, fc=fc, pb=pb, abank=abank: e.matmul(
                    ps[abank][:, fc * 128:(fc + 1) * 128], lhsT=kst[pb:pb + 64, fc, :],
                    rhs=qrl[pb:pb + 64, fc, :], start=True, stop=True),
                    r=[B_gq], w=[B_ps[abank]])
            for h in range(4):
                fc = h // 2
                abank = 6 if h % 2 == 0 else 7
                k.op("dve", lambda e, h=h, fc=fc, abank=abank: e.tensor_tensor(
                    out=attT[:, h, :], in0=ps[abank][:, fc * 128:(fc + 1) * 128], in1=tri_in[:, :], op=ALU.mult),
                    r=[B_ps[abank], B_tri], w=[B_att])
            yield
            for h in range(4):
                fc = h // 2; pb = 64 * (h % 2)
                k.op("pe", lambda e, h=h: e.matmul(ps[7][:, h * 128:(h + 1) * 128], lhsT=attT[:, h, :], rhs=gv_v[:, h * 128:(h + 1) * 128],
                                                   start=True, stop=False), r=[B_att, B_ld[sl]], w=[B_ps[7]], inc=False)
                k.op("pe", lambda e, h=h, fc=fc, pb=pb: e.matmul(ps[7][:, h * 128:(h + 1) * 128], lhsT=qin[pb:pb + 64, fc, :],
                                                                 rhs=Sb[pb:pb + 64, fc, :], start=False, stop=True),
                     r=[B_gq, B_Sb], w=[B_ps[7]], inc=(h == 3))
            yield
            for fc in range(2):
                k.op("pe", lambda e, fc=fc: e.matmul(ps[6][:, fc * 256:(fc + 1) * 256], lhsT=kstk[:, fc * 128:(fc + 1) * 128],
                                                     rhs=gv_v[:, fc * 256:(fc + 1) * 256], start=True, stop=True),
                     r=[B_gq, B_ld[sl]], w=[B_ps[6]], inc=(fc == 1))
            for fc in range(2):
                for hh in range(2):
                    pb = 64 * hh
                    k.op("dve", lambda e, fc=fc, hh=hh, pb=pb: e.scalar_tensor_tensor(
                        out=Sst[pb:pb + 64, fc, :], in0=Sst[pb:pb + 64, fc, :], scalar=ET[pb:pb + 64, 0, fc * 128 + 127:fc * 128 + 128],
                        in1=ps[6][pb:pb + 64, fc * 256 + hh * 128:fc * 256 + (hh + 1) * 128], op0=ALU.mult, op1=ALU.add),
                        r=[B_ET, B_ps[6], B_S], w=[B_S])
            k.op("act", lambda e: e.activation(out=Sb[:].rearrange("p c t -> p (c t)"), in_=Sst[:].rearrange("p c t -> p (c t)"),
                                               func=AF.Identity), r=[B_S], w=[B_Sb])
            yield
            for h in range(4):
                k.op("dve", lambda e, h=h: e.bn_stats(out=gst[:, h, :], in_=ps[7][:, h * 128:(h + 1) * 128]), r=[B_ps[7]], w=[B_gs])
            for h in range(4):
                k.op("dve", lambda e, h=h: e.bn_aggr(out=gmv[:, h, :], in_=gst[:, h, :]), r=[B_gs], w=[B_gs])
            k.op("act", lambda e: e.activation(out=grs[:, 0:4], in_=gmv[:, :, 1], func=AF.Sqrt, bias=EPS), r=[B_gs], w=[B_gs])
            k.op("dve", lambda e: e.reciprocal(out=grs[:, 0:4], in_=grs[:, 0:4]), r=[B_gs], w=[B_gs])
            k.op("dve", lambda e: e.scalar_tensor_tensor(out=grs[:, 4:8], in0=gmv[:, :, 0], scalar=-1.0, in1=grs[:, 0:4],
                                                         op0=ALU.mult, op1=ALU.mult), r=[B_gs], w=[B_gs])
            for h in range(4):
                k.op("act", lambda e, h=h: e.activation(out=on[:, h * 128:(h + 1) * 128], in_=ps[7][:, h * 128:(h + 1) * 128],
                                                        func=AF.Identity, scale=grs[:, h:h + 1], bias=grs[:, 4 + h:5 + h]),
                     r=[B_ps[7], B_gs], w=[B_on])
            k.op("act", lambda e: e.activation(out=sg[:], in_=gr_v, func=AF.Silu), r=[B_ld[sl]], w=[B_ob])
            k.op("dve", lambda e: e.tensor_tensor(out=on[:], in0=on[:], in1=gB[:], op=ALU.mult), r=[B_on, B_gc], w=[B_on])
            k.op("dve", lambda e: e.tensor_tensor(out=ob[:], in0=on[:], in1=sg[:], op=ALU.mult), r=[B_on, B_ob], w=[B_ob])
            for c in range(4):
                k.op("pe", lambda e, c=c: e.transpose(out=psb[3][:, 512 + c * 128:512 + (c + 1) * 128], in_=ob[:, c * 128:(c + 1) * 128],
                                                      identity=ident[:]), r=[B_ob, B_ident], w=[B_ps[3]], inc=(c == 3))
            k.op("act", lambda e: e.activation(out=o_bT[sl][:].rearrange("p c t -> p (c t)"), in_=psb[3][:, 512:1024], func=AF.Identity),
                 r=[B_ps[3]], w=[B_obT[sl]])
            k.dma("pool", obT_d[:, :, i * 128:(i + 1) * 128].rearrange("c p t -> p c t"), o_bT[sl][:], s_obT[sl],
                  r=[B_obT[sl]], w=[B_obTd])

            yield

        def run_interleaved(gens, weights):
            live = [[g, w] for g, w in zip(gens, weights)]
            while live:
                for ent in list(live):
                    g, w = ent
                    for _ in range(w):
                        try:
                            next(g)
                        except StopIteration:
                            live.remove(ent)
                            break

        for step in range(nt3 + 1):
            if step < nt3:
                stage_idx(step)
            gens = []; wts = []
            if step < nt3:
                gens.append(stage_bis(step)); wts.append(1)
                gens.append(stage_gla(step)); wts.append(1)
            if step >= 1:
                gens.append(stage_attn(step - 1)); wts.append(3)
            run_interleaved(gens, wts)
        phase_end()
        vstack.close()
        if debug:
            s_dbg2 = k.new_sem("dbg2")
            k.dma("sp", dbg["oaT"][:, :, :], oaT_d[:, :, :], s_dbg2)
            k.dma("sp", dbg["obT"][:, :, :], obT_d[:, :, :], s_dbg2)
            k.final_wait("sp", [s_dbg2])
        if STOP == 20:
            return nc
        phase_begin()
        h2T = hT
        B_h2T = [Buf("h2T%d" % i) for i in range(NT)]
        w_ba = lsb("w_ba_s", [128, 4, 1024], BF16)
        w_bb = lsb("w_bb_s", [128, 4, 1024], BF16)
        w_o = lsb("w_o_s", [128, 8, 1024], BF16)
        g1B = lsb("g1B", [128, 1024], F32)
        b1B = lsb("b1B", [128, 1024], F32)
        B_w3b = Buf("w3b")
        s_w3b = k.new_sem("w3b")
        k.dma("pool", w_ba[:], w_ba_d.rearrange("(k p) j -> p k j", p=128), s_w3b, w=[B_w3b])
        k.dma("pool", w_bb[:], w_bb_d.rearrange("(k p) j -> p k j", p=128), s_w3b, w=[B_w3b])
        k.dma("pool", w_o[:], w_o_d.rearrange("(k p) j -> p k j", p=128), s_w3b, w=[B_w3b])
        s_w3c = k.new_sem("w3c")
        k.dma("sp", g1B[:], ln1_g_d[0:1, :].broadcast_to([128, 1024]), s_w3c, w=[B_w3b])
        k.dma("sp", b1B[:], ln1_b_d[0:1, :].broadcast_to([128, 1024]), s_w3c, w=[B_w3b])
        k.barrier()
        xs = [lsb("xs%d" % i, [128, D], F32) for i in range(2)]
        B_xs = [Buf("xs0"), Buf("xs1")]
        s_xs = [k.new_sem("xsb0"), k.new_sem("xsb1")]
        alloc_ln(4, nxn=2)
        gts = [lsb("gts%d" % i, [128, 2048], BF16) for i in range(2)]
        oaL = [lsb("oaL%d" % i, [128, 4, 128], BF16) for i in range(2)]
        obL = [lsb("obL%d" % i, [128, 4, 128], BF16) for i in range(2)]
        B_l3 = [Buf("l3_0"), Buf("l3_1")]
        s_l3 = [k.new_sem("l3_0"), k.new_sem("l3_1")]
        sga = lsb("sga", [128, 1024], BF16)
        sgb = lsb("sgb", [128, 1024], BF16)
        m1 = lsb("m1", [128, 1024], F32)
        m2 = lsb("m2", [128, 1024], F32)
        mrg = lsb("mrg", [128, 1024], BF16)
        mrgT = lsb("mrgT", [128, 8, 128], BF16)
        vv = lsb("vv", [128, 1024], F32)
        x1t = [lsb("x1t%d" % i, [128, 1024], F32) for i in range(3)]
        B_sg = Buf("sg"); B_m1 = Buf("m1"); B_m2 = Buf("m2"); B_mrg = Buf("mrg"); B_mrgT = Buf("mrgT"); B_vv = Buf("vv")
        B_x1t = [Buf("x1t%d" % i) for i in range(3)]
        s_x1t = [k.new_sem("x1t%d" % i) for i in range(3)]
        B_x1d = [Buf("x1d%d" % i) for i in range(NT)]
        mrgT2 = [mrgT, lsb("mrgT1", [128, 8, 128], BF16)]
        B_mrgT2 = [B_mrgT, Buf("mrgT1")]

        def p3b_front(i):
            sl = i % 2
            k.dma("sp", gts[sl][:], tok_d[i * 128:(i + 1) * 128, 2320:4368], s_l3[sl], r=[B_tok], w=[B_l3[sl]])
            k.dma("sp", oaL[sl][:], oaT_d[:, :, i * 128:(i + 1) * 128].rearrange("c p t -> p c t"), s_l3[sl], r=[B_oaTd], w=[B_l3[sl]])
            k.dma("sp", obL[sl][:], obT_d[:, :, i * 128:(i + 1) * 128].rearrange("c p t -> p c t"), s_l3[sl], r=[B_obTd], w=[B_l3[sl]])
            k.dma("sp", xs[sl][:], x_d[i * 128:(i + 1) * 128, :], s_xs[sl], w=[B_xs[sl]])
            for br, (src, wt) in enumerate(((oaL[sl], w_ba), (obL[sl], w_bb))):
                for half in range(2):
                    bank = br * 2 + half
                    for kc in range(4):
                        k.op("pe", lambda e, src=src, wt=wt, kc=kc, half=half, bank=bank: e.matmul(
                            ps[bank][:, :], lhsT=src[:, kc, :], rhs=wt[:, kc, half * 512:(half + 1) * 512],
                            start=(kc == 0), stop=(kc == 3)), r=[B_l3[sl], B_w3b], w=[B_ps[bank]], inc=(kc == 3))
            k.op("act", lambda e: e.activation(out=sga[:], in_=gts[sl][:, 0:1024], func=AF.Sigmoid), r=[B_l3[sl]], w=[B_sg])
            k.op("act", lambda e: e.activation(out=sgb[:], in_=gts[sl][:, 1024:2048], func=AF.Sigmoid), r=[B_l3[sl]], w=[B_sg])
            yield
            for half in range(2):
                cs = slice(half * 512, (half + 1) * 512)
                k.op("dve", lambda e, half=half, cs=cs: e.tensor_tensor(out=m1[:, cs], in0=ps[half][:, :], in1=sga[:, cs], op=ALU.mult),
                     r=[B_ps[half], B_sg], w=[B_m1])
                k.op("dve", lambda e, half=half, cs=cs: e.tensor_tensor(out=m2[:, cs], in0=ps[2 + half][:, :], in1=sgb[:, cs], op=ALU.mult),
                     r=[B_ps[2 + half], B_sg], w=[B_m2])
            yield
            k.op("pool", lambda e: e.tensor_tensor(out=mrg[:], in0=m1[:], in1=m2[:], op=ALU.add), r=[B_m1, B_m2], w=[B_mrg])
            for kc in range(8):
                k.op("pe", lambda e, kc=kc: e.transpose(out=psb[6][:, kc * 128:(kc + 1) * 128], in_=mrg[:, kc * 128:(kc + 1) * 128],
                                                        identity=ident[:]), r=[B_mrg, B_ident], w=[B_ps[6]], inc=(kc == 7))
            k.op("act", lambda e: e.activation(out=mrgT2[sl][:].rearrange("p c t -> p (c t)"), in_=psb[6][:, :], func=AF.Identity),
                 r=[B_ps[6]], w=[B_mrgT2[sl]])
            yield

        def p3b_back(i):
            sl = i % 2
            for half in range(2):
                bank = 4 + half
                for kc in range(8):
                    k.op("pe", lambda e, kc=kc, half=half, bank=bank: e.matmul(
                        ps[bank][:, :], lhsT=mrgT2[sl][:, kc, :], rhs=w_o[:, kc, half * 512:(half + 1) * 512],
                        start=(kc == 0), stop=(kc == 7)), r=[B_mrgT2[sl], B_w3b], w=[B_ps[bank]], inc=(kc == 7))
            for half in range(2):
                cs = slice(half * 512, (half + 1) * 512)
                k.op("dve", lambda e, half=half, cs=cs: e.tensor_tensor(out=vv[:, cs], in0=ps[4 + half][:, :], in1=gtB[:, cs], op=ALU.mult),
                     r=[B_ps[4 + half], B_gtB], w=[B_vv])
            k.op("dve", lambda e: e.scalar_tensor_tensor(out=vv[:], in0=xs[sl][:], scalar=ALPHA, in1=vv[:], op0=ALU.mult, op1=ALU.add),
                 r=[B_xs[sl], B_vv], w=[B_vv])
            yield
            s3 = i % 3
            mv = LN["mv"]; B_mv = LN["B_mv"]
            ln_stats(vv, B_vv, sl)
            k.op("act", lambda e: e.activation(out=x1t[s3][:], in_=vv[:], func=AF.Identity, scale=mv[sl][:, 2:3], bias=mv[sl][:, 3:4]),
                 r=[B_vv, B_mv[sl]], w=[B_x1t[s3]])
            yield
            k.op("pool", lambda e: e.tensor_tensor(out=x1t[s3][:], in0=x1t[s3][:], in1=g1B[:], op=ALU.mult), r=[B_x1t[s3], B_w3b], w=[B_x1t[s3]])
            k.op("pool", lambda e: e.tensor_tensor(out=x1t[s3][:], in0=x1t[s3][:], in1=b1B[:], op=ALU.add), r=[B_x1t[s3], B_w3b], w=[B_x1t[s3]])
            k.dma("pool", x1_d[i * 128:(i + 1) * 128, :], x1t[s3][:], s_x1t[s3], r=[B_x1t[s3]], w=[B_x1d[i]])
            yield

        def p3b_feat(i):
            s3 = i % 3
            yield from to_featT(i, x1t[s3], B_x1t[s3], h2T, B_h2T[i], i * 128, 24, 16, bank=7, slo=2)
            yield

        def run_il(gens):
            live = list(gens)
            while live:
                for g in list(live):
                    try:
                        next(g)
                    except StopIteration:
                        live.remove(g)

        for step in range(NT + 2):
            gens = []
            if step < NT:
                gens.append(p3b_front(step))
            if 1 <= step <= NT:
                gens.append(p3b_back(step - 1))
            if 2 <= step:
                gens.append(p3b_feat(step - 2))
            run_il(gens)
        s_h2 = k.new_sem("h2d")
        B_h2d = Buf("h2d")
        k.dma("sp", h2T_d.rearrange("k p t -> p k t"), h2T[:], s_h2, r=B_h2T, w=[B_h2d])
        if debug:
            dbg["x1"] = nc.dram_tensor("dbg_x1", [S, D], F32, kind="ExternalOutput").ap()
            k.barrier()
            k.dma("sp", dbg["x1"][:, :], x1_d[:, :], s_h2, r=B_x1d)
        phase_end()
        if STOP == 21:
            return nc
        phase_begin()
        HT = 2048
        h2h = lsb("h2h", [128, 8, HT], BF16)
        B_h2h = Buf("h2h")
        s_h2h = k.new_sem("h2h")
        acc = hT[:].rearrange("p a b -> p (a b)").bitcast(F32).rearrange("p (t c) -> p t c", c=1024)
        B_acc = [Buf("acc%d" % i) for i in range(HT // 128)]
        wr = lsb("wr", [128, 8, 36], BF16)
        brr = lsb("brr", [1, 36], BF16)
        brf = lsb("brf", [1, 36], F32)
        g2B = lsb("g2B", [128, 1024], F32)
        b2B = lsb("b2B", [128, 1024], F32)
        B_wr = Buf("wr")
        s_wr = k.new_sem("wr")
        k.dma("pool", wr[:], w_r_d.rearrange("(k p) j -> p k j", p=128), s_wr, w=[B_wr])
        s_wr2 = k.new_sem("wr2")
        k.dma("sp", brf[:], b_r_d[:, :], s_wr2, w=[B_wr])
        k.dma("sp", g2B[:], ln2_g_d[0:1, :].broadcast_to([128, 1024]), s_wr2, w=[B_wr])
        k.dma("sp", b2B[:], ln2_b_d[0:1, :].broadcast_to([128, 1024]), s_wr2, w=[B_wr])
        k.op("dve", lambda e: e.tensor_copy(out=brr[:], in_=brf[:]), r=[B_wr], w=[B_wr])
        k.barrier()
        gates = lsb("gates", [128, HT // 128, 32], F32)
        B_gates = Buf("gates")
        B_rt = Buf("rt")
        TH_ = HT // 128
        L3 = lsb("r_L3", [128, TH_, 36], F32)
        r_mx = lsb("r_mx", [128, TH_], F32); r_gw = lsb("r_gw", [128, TH_], F32)
        r_m1 = lsb("r_m1", [128, TH_], F32); r_m2 = lsb("r_m2", [128, TH_], F32)
        r_d = lsb("r_d", [128, TH_], F32); r_w1 = lsb("r_w1", [128, TH_], F32); r_w2 = lsb("r_w2", [128, TH_], F32)
        r_oh4 = lsb("r_oh4", [128, TH_, 4], F32); r_ex4 = lsb("r_ex4", [128, TH_, 4], F32)
        r_t32 = lsb("r_t32", [128, TH_, 4, 8], F32)
        r_eig = lsb("r_eig", [128, TH_, 8], F32); r_e2 = lsb("r_e2", [128, TH_, 8], F32)
        r_eq1 = lsb("r_eq1", [128, TH_, 8], F32); r_eq2 = lsb("r_eq2", [128, TH_, 8], F32)
        w1s = [lsb("w1s%d" % i, [128, 8, 256], BF16) for i in range(2)]
        w3s = [lsb("w3s%d" % i, [128, 8, 256], BF16) for i in range(2)]
        w2s = [lsb("w2s%d" % i, [128, 2, 1024], BF16) for i in range(2)]
        B_ws = [Buf("ws0"), Buf("ws1")]
        s_ws = [k.new_sem("ws0"), k.new_sem("ws1")]
        sgl = [lsb("sgl%d" % i, [128, 2, 512], BF16) for i in range(2)]
        hid = [lsb("hid%d" % i, [128, 2, 512], BF16) for i in range(2)]
        B_sgl = [Buf("sgl0"), Buf("sgl1")]
        B_hid = [Buf("hid0"), Buf("hid1")]
        NTL = 3
        st6b = lsb("st6b", [128, HT // 128, 2, 6], F32)
        mvb = lsb("mvb", [128, HT // 128, 4], F32)
        B_mvt = [Buf("mvt%d" % i) for i in range(HT // 128)]
        x1l = [lsb("x1l%d" % i, [128, 1024], F32) for i in range(NTL)]
        B_x1l = [Buf("x1l%d" % i) for i in range(NTL)]
        s_x1l = [k.new_sem("x1l%d" % i) for i in range(NTL)]
        fo = [lsb("fo%d" % i, [128, 1024], F32) for i in range(NTL)]
        B_fo = [Buf("fo%d" % i) for i in range(NTL)]
        s_fo = [k.new_sem("fo%d" % i) for i in range(NTL)]
        NTH = HT // 128
        k.dma("sp", h2h[:], h2T_d[:, :, 0:HT].rearrange("k p t -> p k t"), s_h2h, r=[B_h2d], w=[B_h2h])
        for hf in range(S // HT):
            for ti in range(NTH):
                bank = ti // 8
                co = (ti % 8) * 36
                k.op("pe", lambda e, bank=bank, co=co: e.matmul(ps[bank][:, co:co + 36], lhsT=ones1[:, :], rhs=brr[:, :], start=True, stop=False),
                     r=[B_wr], w=[B_ps[bank]], inc=False)
                for kc in range(8):
                    k.op("pe", lambda e, kc=kc, ti=ti, bank=bank, co=co: e.matmul(
                        ps[bank][:, co:co + 36], lhsT=h2h[:, kc, ti * 128:(ti + 1) * 128], rhs=wr[:, kc, :],
                        start=False, stop=(kc == 7)), r=[B_h2h, B_wr], w=[B_ps[bank]], inc=(kc == 7))
            T_ = NTH
            for bank in range(2):
                k.op("dve", lambda e, bank=bank: e.tensor_copy(out=L3[:, bank * 8:(bank + 1) * 8, :].rearrange("p t c -> p (t c)"),
                                                               in_=ps[bank][:, 0:288]), r=[B_ps[bank]], w=[B_rt])
            lg4 = L3[:, :, 0:4]
            le = L3[:, :, 4:36].rearrange("p t (g e) -> p t g e", g=4)
            k.op("dve", lambda e: e.tensor_reduce(out=r_mx[:], in_=lg4, axis=AX.X, op=ALU.max), r=[B_rt], w=[B_rt])
            k.op("dve", lambda e: e.tensor_tensor(out=r_oh4[:], in0=lg4, in1=r_mx[:].unsqueeze(2).broadcast_to([128, T_, 4]), op=ALU.is_equal),
                 r=[B_rt], w=[B_rt])
            k.op("dve", lambda e: e.tensor_tensor(out=r_ex4[:], in0=lg4, in1=r_mx[:].unsqueeze(2).broadcast_to([128, T_, 4]), op=ALU.subtract),
                 r=[B_rt], w=[B_rt])
            k.op("act", lambda e: e.activation(out=r_ex4[:], in_=r_ex4[:], func=AF.Exp), r=[B_rt], w=[B_rt])
            k.op("dve", lambda e: e.tensor_reduce(out=r_gw[:], in_=r_ex4[:], axis=AX.X, op=ALU.add), r=[B_rt], w=[B_rt])
            k.op("dve", lambda e: e.reciprocal(out=r_gw[:], in_=r_gw[:]), r=[B_rt], w=[B_rt])
            k.op("dve", lambda e: e.tensor_tensor(out=r_t32[:], in0=le, in1=r_oh4[:].unsqueeze(3).broadcast_to([128, T_, 4, 8]), op=ALU.mult),
                 r=[B_rt], w=[B_rt])
            k.op("dve", lambda e: e.tensor_reduce(out=r_eig[:], in_=r_t32[:].rearrange("p t g e -> p t e g"), axis=AX.X, op=ALU.add),
                 r=[B_rt], w=[B_rt])
            k.op("dve", lambda e: e.tensor_reduce(out=r_m1[:], in_=r_eig[:], axis=AX.X, op=ALU.max), r=[B_rt], w=[B_rt])
            k.op("dve", lambda e: e.tensor_tensor(out=r_eq1[:], in0=r_eig[:], in1=r_m1[:].unsqueeze(2).broadcast_to([128, T_, 8]), op=ALU.is_equal),
                 r=[B_rt], w=[B_rt])
            k.op("dve", lambda e: e.scalar_tensor_tensor(out=r_e2[:], in0=r_eq1[:], scalar=-1.0e30, in1=r_eig[:], op0=ALU.mult, op1=ALU.add),
                 r=[B_rt], w=[B_rt])
            k.op("dve", lambda e: e.tensor_reduce(out=r_m2[:], in_=r_e2[:], axis=AX.X, op=ALU.max), r=[B_rt], w=[B_rt])
            k.op("dve", lambda e: e.tensor_tensor(out=r_eq2[:], in0=r_eig[:], in1=r_m2[:].unsqueeze(2).broadcast_to([128, T_, 8]), op=ALU.is_equal),
                 r=[B_rt], w=[B_rt])
            k.op("dve", lambda e: e.tensor_tensor(out=r_d[:], in0=r_m2[:], in1=r_m1[:], op=ALU.subtract), r=[B_rt], w=[B_rt])
            k.op("act", lambda e: e.activation(out=r_d[:], in_=r_d[:], func=AF.Exp), r=[B_rt], w=[B_rt])
            k.op("dve", lambda e: e.tensor_scalar(out=r_w1[:], in0=r_d[:], scalar1=1.0, scalar2=None, op0=ALU.add), r=[B_rt], w=[B_rt])
            k.op("dve", lambda e: e.reciprocal(out=r_w1[:], in_=r_w1[:]), r=[B_rt], w=[B_rt])
            k.op("dve", lambda e: e.tensor_tensor(out=r_w1[:], in0=r_w1[:], in1=r_gw[:], op=ALU.mult), r=[B_rt], w=[B_rt])
            k.op("dve", lambda e: e.tensor_tensor(out=r_w2[:], in0=r_w1[:], in1=r_d[:], op=ALU.mult), r=[B_rt], w=[B_rt])
            k.op("dve", lambda e: e.tensor_tensor(out=r_eq1[:], in0=r_eq1[:], in1=r_w1[:].unsqueeze(2).broadcast_to([128, T_, 8]), op=ALU.mult),
                 r=[B_rt], w=[B_rt])
            k.op("dve", lambda e: e.tensor_tensor(out=r_eq2[:], in0=r_eq2[:], in1=r_w2[:].unsqueeze(2).broadcast_to([128, T_, 8]), op=ALU.mult),
                 r=[B_rt], w=[B_rt])
            k.op("dve", lambda e: e.tensor_tensor(out=r_eq1[:], in0=r_eq1[:], in1=r_eq2[:], op=ALU.add), r=[B_rt], w=[B_rt])
            k.op("dve", lambda e: e.tensor_tensor(out=gates[:].rearrange("p t (g e) -> p t g e", g=4),
                                                  in0=r_oh4[:].unsqueeze(3).broadcast_to([128, T_, 4, 8]),
                                                  in1=r_eq1[:].unsqueeze(2).broadcast_to([128, T_, 4, 8]), op=ALU.mult),
                 r=[B_rt], w=[B_gates])
            ycnt = [0]

            def emit_H(ex, tb, ws, hs):
                for which, wsrc in enumerate((w1s[ws], w3s[ws])):
                    for fcn in range(2):
                        bank = which * 2 + fcn
                        for kc in range(8):
                            k.op("pe", lambda e, wsrc=wsrc, fcn=fcn, kc=kc, bank=bank: e.matmul(
                                ps[bank][:, :], lhsT=wsrc[:, kc, fcn * 128:(fcn + 1) * 128], rhs=h2h[:, kc, tb * 512:(tb + 1) * 512],
                                start=(kc == 0), stop=(kc == 7)), r=[B_ws[ws], B_h2h], w=[B_ps[bank]], inc=(kc == 7))
                for fcn in range(2):
                    k.op("act", lambda e, fcn=fcn: e.activation(out=sgl[hs][:, fcn, :], in_=ps[fcn][:, :], func=AF.Silu),
                         r=[B_ps[fcn]], w=[B_sgl[hs]])
                for fcn in range(2):
                    k.op("dve", lambda e, fcn=fcn: e.tensor_tensor(out=hid[hs][:, fcn, :], in0=ps[2 + fcn][:, :], in1=sgl[hs][:, fcn, :],
                                                                   op=ALU.mult), r=[B_ps[2 + fcn], B_sgl[hs]], w=[B_hid[hs]])

            def emit_Y(ex, tb, ws, hs):
                for t4 in range(4):
                    ti = tb * 4 + t4
                    yb = 4 + 2 * (ycnt[0] % 2)
                    ycnt[0] += 1
                    for h2_ in range(2):
                        for fcn in range(2):
                            k.op("pe", lambda e, h2_=h2_, fcn=fcn, t4=t4, yb=yb: e.matmul(
                                ps[yb + h2_][:, :], lhsT=hid[hs][:, fcn, t4 * 128:(t4 + 1) * 128], rhs=w2s[ws][:, fcn, h2_ * 512:(h2_ + 1) * 512],
                                start=(fcn == 0), stop=(fcn == 1)), r=[B_hid[hs], B_ws[ws]], w=[B_ps[yb + h2_]], inc=(fcn == 1))
                    for h2_ in range(2):
                        cs = slice(h2_ * 512, (h2_ + 1) * 512)
                        if ex == 0:
                            k.op("dve", lambda e, h2_=h2_, cs=cs, ti=ti, yb=yb: e.tensor_scalar(
                                out=acc[:, ti, cs], in0=ps[yb + h2_][:, :], scalar1=gates[:, ti, ex:ex + 1], scalar2=None, op0=ALU.mult),
                                r=[B_ps[yb + h2_], B_gates], w=[B_acc[ti]])
                        else:
                            k.op("dve", lambda e, h2_=h2_, cs=cs, ti=ti, yb=yb: e.scalar_tensor_tensor(
                                out=acc[:, ti, cs], in0=ps[yb + h2_][:, :], scalar=gates[:, ti, ex:ex + 1], in1=acc[:, ti, cs],
                                op0=ALU.mult, op1=ALU.add), r=[B_ps[yb + h2_], B_gates, B_acc[ti]], w=[B_acc[ti]])

            prev = None
            hcnt = 0
            for ex in range(32):
                ws = ex % 2
                k.dma("pool", w1s[ws][:], w_eg_d[ex].rearrange("(k p) f -> p k f", p=128), s_ws[ws], w=[B_ws[ws]])
                k.dma("pool", w3s[ws][:], w_eu_d[ex].rearrange("(k p) f -> p k f", p=128), s_ws[ws], w=[B_ws[ws]])
                k.dma("pool", w2s[ws][:], w_ed_d[ex].rearrange("(k p) j -> p k j", p=128), s_ws[ws], w=[B_ws[ws]])
                for tb in range(HT // 512):
                    hs = hcnt % 2
                    hcnt += 1
                    emit_H(ex, tb, ws, hs)
                    if prev is not None:
                        emit_Y(*prev)
                    prev = (ex, tb, ws, hs)
            emit_Y(*prev)
            if hf + 1 < S // HT:
                k.dma("sp", h2h[:], h2T_d[:, :, (hf + 1) * HT:(hf + 2) * HT].rearrange("k p t -> p k t"), s_h2h, r=[B_h2d], w=[B_h2h])
            for ti in range(NTH):
                i = hf * NTH + ti
                sl = ti % NTL
                k.dma("sp", x1l[sl][:], x1_d[i * 128:(i + 1) * 128, :], s_x1l[sl], r=[B_x1d[i]], w=[B_x1l[sl]])
                k.op("pool", lambda e, ti=ti: e.tensor_tensor(out=acc[:, ti, :], in0=acc[:, ti, :], in1=gtB[:, 1024:2048], op=ALU.mult),
                     r=[B_acc[ti], B_gtB], w=[B_acc[ti]])
                k.op("dve", lambda e, ti=ti, sl=sl: e.scalar_tensor_tensor(out=acc[:, ti, :], in0=x1l[sl][:], scalar=ALPHA, in1=acc[:, ti, :],
                                                                           op0=ALU.mult, op1=ALU.add), r=[B_x1l[sl], B_acc[ti]], w=[B_acc[ti]])
                for hh in range(2):
                    k.op("dve", lambda e, ti=ti, hh=hh: e.bn_stats(out=st6b[:, ti, hh, :], in_=acc[:, ti, hh * 512:(hh + 1) * 512]),
                         r=[B_acc[ti]], w=[B_mvt[ti]])
                k.op("dve", lambda e, ti=ti: e.bn_aggr(out=mvb[:, ti, 0:2], in_=st6b[:, ti, :, :].rearrange("p a b -> p (a b)")),
                     r=[B_mvt[ti]], w=[B_mvt[ti]])
            k.op("act", lambda e: e.activation(out=mvb[:, :, 2], in_=mvb[:, :, 1], func=AF.Sqrt, bias=EPS), r=B_mvt, w=B_mvt)
            k.op("dve", lambda e: e.reciprocal(out=mvb[:, :, 2], in_=mvb[:, :, 2]), r=B_mvt, w=B_mvt)
            k.op("dve", lambda e: e.scalar_tensor_tensor(out=mvb[:, :, 3], in0=mvb[:, :, 0], scalar=-1.0, in1=mvb[:, :, 2],
                                                         op0=ALU.mult, op1=ALU.mult), r=B_mvt, w=B_mvt)
            for ti in range(NTH):
                i = hf * NTH + ti
                k.op("act", lambda e, ti=ti: e.activation(out=acc[:, ti, :], in_=acc[:, ti, :], func=AF.Identity, scale=mvb[:, ti, 2:3],
                                                          bias=mvb[:, ti, 3:4]), r=[B_acc[ti], B_mvt[ti]], w=[B_acc[ti]])
                k.op("dve", lambda e, ti=ti: e.tensor_tensor(out=acc[:, ti, :], in0=acc[:, ti, :], in1=g2B[:], op=ALU.mult),
                     r=[B_acc[ti], B_wr], w=[B_acc[ti]])
                k.op("pool", lambda e, ti=ti: e.tensor_tensor(out=acc[:, ti, :], in0=acc[:, ti, :], in1=b2B[:], op=ALU.add),
                     r=[B_acc[ti], B_wr], w=[B_acc[ti]])
                k.dma("pool", out_d[i * 128:(i + 1) * 128, :], acc[:, ti, :], s_fo[ti % NTL], r=[B_acc[ti]])
        k.barrier()
        k.final_wait("sp", s_fo)
        cur[0].close()
        cur[0] = None
    return nc


def t5_bucket_np(d):
    d = np.asarray(d)
    max_exact = 16
    d_f = np.maximum(d, 1).astype(np.float32)
    large = max_exact + (np.log(d_f / max_exact) / np.log(128 / max_exact) * (32 - max_exact)).astype(np.int32)
    large = np.minimum(large, 31)
    return np.where(d < max_exact, d, large)


def make_ohpad():
    oh = np.zeros((32, 384), np.float32)
    d = np.arange(256)
    b = t5_bucket_np(d)
    oh[b, 128 + d] = 8.0
    return oh


def prep_inputs(inputs, b):
    f = np.float32
    c = np.ascontiguousarray(inputs["c"][b].reshape(8, 128).T.astype(f))
    b_ada = inputs["b_ada"][0]
    m = {
        "x": np.ascontiguousarray(inputs["x"][b]),
        "c_pl": c,
        "w_ada": np.ascontiguousarray(inputs["w_ada"][0]),
        "b_ada_pl": np.ascontiguousarray(b_ada.reshape(48, 128).T),
        "b_ada_row": np.ascontiguousarray(b_ada.reshape(1, -1)),
        "w_in": np.ascontiguousarray(inputs["w_in"][0]),
        "rel_bias": np.ascontiguousarray(inputs["rel_bias"].astype(f)),
        "ohpad": make_ohpad(),
        "gla_wg": np.ascontiguousarray(inputs["gla_w_gate"][0]),
        "gla_bg": np.ascontiguousarray(inputs["gla_b_gate"][0].reshape(1, -1)),
        "gla_g": np.ascontiguousarray(inputs["gla_norm_g"][0].reshape(1, -1)),
        "w_ba": np.ascontiguousarray(inputs["w_branch_a"][0]),
        "w_bb": np.ascontiguousarray(inputs["w_branch_b"][0]),
        "w_o": np.ascontiguousarray(inputs["w_out"][0]),
        "ln1_g": np.ascontiguousarray(inputs["ln1_g"][0].reshape(1, -1)),
        "ln1_b": np.ascontiguousarray(inputs["ln1_b"][0].reshape(1, -1)),
        "ln2_g": np.ascontiguousarray(inputs["ln2_g"][0].reshape(1, -1)),
        "ln2_b": np.ascontiguousarray(inputs["ln2_b"][0].reshape(1, -1)),
        "w_r": np.ascontiguousarray(np.concatenate([inputs["w_router_group"][0], inputs["w_router_expert"][0]], axis=1)),
        "b_r": np.ascontiguousarray(np.concatenate([inputs["b_router_group"][0], inputs["b_router_expert"][0]]).reshape(1, -1)),
        "w_eg": np.ascontiguousarray(inputs["w_exp_gate"][0]),
        "w_eu": np.ascontiguousarray(inputs["w_exp_up"][0]),
        "w_ed": np.ascontiguousarray(inputs["w_exp_down"][0]),
    }
    return m


def kernel(**inputs):
    nc = build_nc()
    in_maps = [prep_inputs(inputs, b) for b in range(8)]
    res = run_bass_kernel_spmd(nc, in_maps, core_ids=list(range(8)))
    return np.stack([r["out"] for r in res.results], axis=0)
```
